# Optimizing a Trainium2 kernel written in Bass

```python
import jax, jax.numpy as jnp
from jax import lax
import numpy as np

D_MODEL = 2048
BATCH = 1
SEQ = 8192
DEPTH = 4

N_MIXERS = 3
EPS = 1e-6
ROPE_THETA = 10000.0
N_HEADS = 16
HEAD_DIM = 128
N_KV_HEADS = 4
IDX_HEADS = 16
IDX_DIM = 128
IDX_ROPE_DIM = 64
TOPK_MAX = 256
Q_BLOCK = 128
A_SPLITS = [N_HEADS * HEAD_DIM, N_KV_HEADS * HEAD_DIM, N_KV_HEADS * HEAD_DIM, IDX_HEADS * IDX_DIM, IDX_DIM]
A_IN = sum(A_SPLITS) + IDX_HEADS
A_CUTS = [int(c) for c in np.cumsum(A_SPLITS)]
LRU_WIDTH = D_MODEL
LRU_BLOCKS = 8
LRU_BLOCK = LRU_WIDTH // LRU_BLOCKS
CONV_WIDTH = 4
LRU_C = 8.0
POOL_WINDOWS = (2, 4, 8, 16)
POOL_GROUPS = len(POOL_WINDOWS)
POOL_GROUP = D_MODEL // POOL_GROUPS
D_FF = 4 * D_MODEL

kernel_name = 'hybrid_dsa_rglru_pool_trunk'

F32 = jnp.float32


def rms_norm(x, g):
    xf = x.astype(F32)
    y = xf * lax.rsqrt(jnp.mean(xf * xf, axis=-1, keepdims=True) + EPS)
    return (y * g.astype(F32)).astype(x.dtype)


def rope_tables(pos, dim):
    inv = 1.0 / (ROPE_THETA ** (jnp.arange(0, dim, 2, dtype=F32) / dim))
    ang = pos.astype(F32)[..., None] * inv
    return jnp.cos(ang), jnp.sin(ang)


def apply_rope(x, cos, sin):
    xf = x.astype(F32)
    x1, x2 = jnp.split(xf, 2, axis=-1)
    c = cos[:, :, None, :]
    s = sin[:, :, None, :]
    return jnp.concatenate([x1 * c - x2 * s, x2 * c + x1 * s], axis=-1).astype(x.dtype)


def partial_rope(x, cos, sin):
    return jnp.concatenate([apply_rope(x[..., :IDX_ROPE_DIM], cos, sin), x[..., IDX_ROPE_DIM:]], axis=-1)


def dsa_mixer(h, pos, w_in, q_norm, k_norm, w_out):
    B, T, _ = h.shape
    topk = min(TOPK_MAX, T // 4)
    proj = h @ w_in
    q, k, v, iq, ik, iw = jnp.split(proj, A_CUTS, axis=-1)
    q = rms_norm(q.reshape(B, T, N_HEADS, HEAD_DIM), q_norm)
    k = rms_norm(k.reshape(B, T, N_KV_HEADS, HEAD_DIM), k_norm)
    v = v.reshape(B, T, N_KV_HEADS, HEAD_DIM)
    cos, sin = rope_tables(pos, HEAD_DIM)
    q = apply_rope(q, cos, sin)
    k = apply_rope(k, cos, sin)
    icos, isin = rope_tables(pos, IDX_ROPE_DIM)
    iq = partial_rope(iq.reshape(B, T, IDX_HEADS, IDX_DIM), icos, isin)
    ik = partial_rope(ik.reshape(B, T, 1, IDX_DIM), icos, isin)[:, :, 0]
    iw = iw * IDX_HEADS ** -0.5
    n_blk = T // Q_BLOCK
    key_idx = jnp.arange(T, dtype=jnp.int32)
    ik_f = ik.astype(F32)
    group = N_HEADS // N_KV_HEADS

    def to_blocks(a):
        return jnp.moveaxis(a.reshape((B, n_blk, Q_BLOCK) + a.shape[2:]), 1, 0)

    def block_fn(args):
        qb, iqb, iwb, tb = args
        logits = jnp.einsum('bqhd,bsd->bqhs', iqb.astype(F32), ik_f) * IDX_DIM ** -0.5
        score = jnp.einsum('bqh,bqhs->bqs', iwb.astype(F32), jax.nn.relu(logits))
        causal = key_idx[None, None, :] <= tb[None, :, None]
        score = jnp.where(causal, score, -jnp.inf)
        _, sel = lax.top_k(score, topk)
        valid = sel <= tb[None, :, None]
        kg = jax.vmap(lambda kk, ii: kk[ii])(k, sel)
        vg = jax.vmap(lambda vv, ii: vv[ii])(v, sel)
        qg = qb.reshape(B, Q_BLOCK, N_KV_HEADS, group, HEAD_DIM)
        s = jnp.einsum('bqgrd,bqkgd->bqgrk', qg, kg).astype(F32) * HEAD_DIM ** -0.5
        s = jnp.where(valid[:, :, None, None, :], s, -jnp.inf)
        p = jax.nn.softmax(s, axis=-1).astype(vg.dtype)
        o = jnp.einsum('bqgrk,bqkgd->bqgrd', p, vg)
        return o.reshape(B, Q_BLOCK, N_HEADS * HEAD_DIM)

    out = lax.map(block_fn, (to_blocks(q), to_blocks(iq), to_blocks(iw), key_idx.reshape(n_blk, Q_BLOCK)))
    out = jnp.moveaxis(out, 0, 1).reshape(B, T, N_HEADS * HEAD_DIM)
    return out @ w_out


def rglru_mixer(h, w_in, conv_w, conv_b, gate_a_w, gate_a_b, gate_x_w, gate_x_b, lam, w_out):
    B, T, _ = h.shape
    proj = h @ w_in
    gate, xr = jnp.split(proj, 2, axis=-1)
    xp = jnp.pad(xr, ((0, 0), (CONV_WIDTH - 1, 0), (0, 0)))
    xc = conv_b + xp[:, 0:T] * conv_w[0]
    for j in range(1, CONV_WIDTH):
        xc = xc + xp[:, j:j + T] * conv_w[j]
    xb = xc.reshape(B, T, LRU_BLOCKS, LRU_BLOCK)
    r = jax.nn.sigmoid(jnp.einsum('btnc,ncd->btnd', xb, gate_a_w).reshape(B, T, LRU_WIDTH) + gate_a_b)
    i = jax.nn.sigmoid(jnp.einsum('btnc,ncd->btnd', xb, gate_x_w).reshape(B, T, LRU_WIDTH) + gate_x_b)
    log_a = -LRU_C * r.astype(F32) * jax.nn.softplus(-lam.astype(F32))
    a = jnp.exp(log_a)
    mult = jnp.sqrt(-jnp.expm1(2.0 * log_a))
    b = xc.astype(F32) * i.astype(F32) * mult

    def combine(left, right):
        a1, b1 = left
        a2, b2 = right
        return a1 * a2, a2 * b1 + b2

    _, hs = lax.associative_scan(combine, (a, b), axis=1)
    y = hs.astype(h.dtype) * jax.nn.gelu(gate)
    return y @ w_out


def pool_mixer(h, w_group, b_group, scale):
    B, T, D = h.shape
    hf = h.astype(F32)
    csum = jnp.concatenate([jnp.zeros((B, 1, D), F32), jnp.cumsum(hf, axis=1)], axis=1)
    t1 = jnp.arange(1, T + 1, dtype=F32)[None, :, None]
    outs = []
    for g, w in enumerate(POOL_WINDOWS):
        sl = slice(g * POOL_GROUP, (g + 1) * POOL_GROUP)
        c = csum[:, :, sl]
        lag = jnp.concatenate([jnp.zeros((B, w - 1, POOL_GROUP), F32), c[:, :T + 1 - w]], axis=1)
        mean = (c[:, 1:] - lag) / jnp.minimum(t1, float(w))
        outs.append(mean - hf[:, :, sl])
    y = jnp.stack(outs, axis=2).astype(h.dtype)
    y = jnp.einsum('btgc,gcd->btgd', y, w_group) + b_group
    return y.reshape(B, T, D) * scale


def channel_mlp(h, w_up, w_down):
    return jnp.square(jax.nn.relu(h @ w_up)) @ w_down


def setup_inputs(seed: int = 0) -> dict:
    key = jax.random.key(seed)
    ks = jax.random.split(key, 32)
    n_a = (DEPTH + 2) // 3
    n_b = (DEPTH + 1) // 3
    n_c = DEPTH // 3
    nrm = jax.random.normal

    def gain(k, n, d):
        return 1.0 + 0.02 * nrm(k, (n, d), F32)

    x = nrm(ks[0], (BATCH, SEQ, D_MODEL), F32)
    positions = jnp.broadcast_to(jnp.arange(SEQ, dtype=jnp.int32)[None, :], (BATCH, SEQ))
    u = jax.random.uniform(ks[14], (n_b, LRU_WIDTH), F32, 0.9, 0.999)
    a0 = u ** (1.0 / LRU_C)
    lam = jnp.log(a0) - jnp.log1p(-a0)
    return {
        'x': x,
        'positions': positions,
        'attn_norm': gain(ks[1], n_a, D_MODEL),
        'attn_w_in': nrm(ks[2], (n_a, D_MODEL, A_IN), F32) * D_MODEL ** -0.5,
        'attn_q_norm': gain(ks[3], n_a, HEAD_DIM),
        'attn_k_norm': gain(ks[4], n_a, HEAD_DIM),
        'attn_w_out': nrm(ks[5], (n_a, N_HEADS * HEAD_DIM, D_MODEL), F32) * (N_HEADS * HEAD_DIM) ** -0.5,
        'rnn_norm': gain(ks[6], n_b, D_MODEL),
        'rnn_w_in': nrm(ks[7], (n_b, D_MODEL, 2 * LRU_WIDTH), F32) * D_MODEL ** -0.5,
        'rnn_conv_w': nrm(ks[8], (n_b, CONV_WIDTH, LRU_WIDTH), F32) * CONV_WIDTH ** -0.5,
        'rnn_conv_b': 0.02 * nrm(ks[9], (n_b, LRU_WIDTH), F32),
        'rnn_gate_a_w': nrm(ks[10], (n_b, LRU_BLOCKS, LRU_BLOCK, LRU_BLOCK), F32) * LRU_BLOCK ** -0.5,
        'rnn_gate_a_b': 0.02 * nrm(ks[11], (n_b, LRU_WIDTH), F32),
        'rnn_gate_x_w': nrm(ks[12], (n_b, LRU_BLOCKS, LRU_BLOCK, LRU_BLOCK), F32) * LRU_BLOCK ** -0.5,
        'rnn_gate_x_b': 0.02 * nrm(ks[13], (n_b, LRU_WIDTH), F32),
        'rnn_lambda': lam,
        'rnn_w_out': nrm(ks[15], (n_b, LRU_WIDTH, D_MODEL), F32) * LRU_WIDTH ** -0.5,
        'pool_norm': gain(ks[16], n_c, D_MODEL),
        'pool_w': nrm(ks[17], (n_c, POOL_GROUPS, POOL_GROUP, POOL_GROUP), F32) * POOL_GROUP ** -0.5,
        'pool_b': 0.02 * nrm(ks[18], (n_c, POOL_GROUPS, POOL_GROUP), F32),
        'pool_scale': 1.0 + 0.1 * nrm(ks[19], (n_c, D_MODEL), F32),
        'mlp_norm': gain(ks[20], DEPTH, D_MODEL),
        'mlp_w_up': nrm(ks[21], (DEPTH, D_MODEL, D_FF), F32) * D_MODEL ** -0.5,
        'mlp_w_down': nrm(ks[22], (DEPTH, D_FF, D_MODEL), F32) * D_FF ** -0.5,
    }


def reference(x, positions, attn_norm, attn_w_in, attn_q_norm, attn_k_norm, attn_w_out,
              rnn_norm, rnn_w_in, rnn_conv_w, rnn_conv_b, rnn_gate_a_w, rnn_gate_a_b,
              rnn_gate_x_w, rnn_gate_x_b, rnn_lambda, rnn_w_out,
              pool_norm, pool_w, pool_b, pool_scale,
              mlp_norm, mlp_w_up, mlp_w_down):
    for i in range(DEPTH):
        kind, j = i % N_MIXERS, i // N_MIXERS
        if kind == 0:
            x = x + dsa_mixer(rms_norm(x, attn_norm[j]), positions, attn_w_in[j],
                              attn_q_norm[j], attn_k_norm[j], attn_w_out[j])
        elif kind == 1:
            x = x + rglru_mixer(rms_norm(x, rnn_norm[j]), rnn_w_in[j], rnn_conv_w[j], rnn_conv_b[j],
                                rnn_gate_a_w[j], rnn_gate_a_b[j], rnn_gate_x_w[j], rnn_gate_x_b[j],
                                rnn_lambda[j], rnn_w_out[j])
        else:
            x = x + pool_mixer(rms_norm(x, pool_norm[j]), pool_w[j], pool_b[j], pool_scale[j])
        x = x + channel_mlp(rms_norm(x, mlp_norm[i]), mlp_w_up[i], mlp_w_down[i])
    return x
```

```python
import numpy as np
from contextlib import ExitStack
import concourse.bass as bass
import concourse.mybir as mybir
from concourse.bass_utils import run_bass_kernel_spmd

F32 = mybir.dt.float32
BF16 = mybir.dt.bfloat16
I32 = mybir.dt.int32
ALU = mybir.AluOpType
AF = mybir.ActivationFunctionType
AX = mybir.AxisListType

NCORES = 8
T = 8192
D = 2048
TL = T // NCORES
NT = TL // 128
KC = D // 128
DFF = 4 * D
EPS = 1e-6
A_IN = 5264
TOPK = 256
NBIS = 21
BIG = 1.0e30


class Sched:
    NDS = 40

    def __init__(self, nc, es):
        self.nc = nc
        self.E = {"pe": nc.tensor, "act": nc.scalar, "dve": nc.vector, "pool": nc.gpsimd, "sp": nc.sync}
        self.csem = {e: es.enter_context(nc.semaphore("c_" + e)) for e in ("pe", "act", "dve", "pool")}
        self.ccnt = {e: 0 for e in self.csem}
        self.dsem = [es.enter_context(nc.semaphore("d%d" % i)) for i in range(self.NDS)]
        self.dcnt = [0] * self.NDS
        self.drr = 0
        self.known = {e: {} for e in self.E}
        self.lastw = {}
        self.readers = {}
        self.nins = 0

    def _wait(self, eng, ev):
        sid, h, val, src, isdma = ev
        if src == "pe" and eng == "pe" and not isdma:
            return
        if self.known[eng].get(sid, 0) >= val:
            return
        self.E[eng].wait_ge(h, val)
        self.known[eng][sid] = val

    def op(self, eng, fn, r=(), w=(), dma=False):
        deps = []
        for k in r:
            if k in self.lastw:
                deps.append(self.lastw[k])
        for k in w:
            if k in self.lastw:
                deps.append(self.lastw[k])
            deps.extend(self.readers.get(k, {}).values())
        for ev in deps:
            self._wait(eng, ev)
        if dma:
            i = self.drr
            self.drr = (self.drr + 1) % self.NDS
            if self.dcnt[i] > 0:
                self._wait(eng, (("d", i), self.dsem[i], self.dcnt[i], eng, True))
        ins = fn(self.E[eng])
        self.nins += 1
        if dma:
            self.dcnt[i] += 16
            ins.then_inc(self.dsem[i], 16)
            ev = (("d", i), self.dsem[i], self.dcnt[i], eng, True)
        else:
            self.ccnt[eng] += 1
            ins.then_inc(self.csem[eng], 1)
            ev = (("c", eng), self.csem[eng], self.ccnt[eng], eng, False)
        for k in w:
            self.lastw[k] = ev
            self.readers[k] = {}
        for k in r:
            self.readers.setdefault(k, {})[ev[0]] = ev
        return ev

    def pe(self, fn, r=(), w=()):
        return self.op("pe", fn, r, w)

    def act(self, fn, r=(), w=()):
        return self.op("act", fn, r, w)

    def dve(self, fn, r=(), w=()):
        return self.op("dve", fn, r, w)

    def pool(self, fn, r=(), w=()):
        return self.op("pool", fn, r, w)

    def dma(self, out, in_, r=(), w=(), q="sp"):
        return self.op(q, lambda e: e.dma_start(out=out, in_=in_), r, w, dma=True)

    def barrier(self):
        evs = []
        for e in self.csem:
            if self.ccnt[e] > 0:
                evs.append((("c", e), self.csem[e], self.ccnt[e], e, False))
        for i in range(self.NDS):
            if self.dcnt[i] > 0:
                evs.append((("d", i), self.dsem[i], self.dcnt[i], "sp", True))
        for eng in self.E:
            for ev in evs:
                if ev[3] == "pe" and eng == "pe" and not ev[4]:
                    continue
                if self.known[eng].get(ev[0], 0) >= ev[2]:
                    continue
                self.E[eng].wait_ge(ev[1], ev[2])
                self.known[eng][ev[0]] = ev[2]
        self.lastw = {}
        self.readers = {}

    def finish(self, keys):
        for k in keys:
            if k in self.lastw:
                self._wait("sp", self.lastw[k])


class Prog:
    def __init__(self):
        self.nc = bass.Bass("TRN2", target_bir_lowering=False)
        self.es = ExitStack()
        self.S = Sched(self.nc, self.es)
        self.ins = {}
        self.outs = {}
        nc = self.nc
        self.ps = [self.es.enter_context(nc.psum_tensor("psf%d" % i, [128, 512], F32)) for i in range(6)]
        self.psb = [self.es.enter_context(nc.psum_tensor("psb%d" % i, [128, 1024], BF16)) for i in range(2)]
        self.uid = 0

    def inp(self, name, shape, dt=F32):
        t = self.nc.dram_tensor(name, list(shape), dt, kind="ExternalInput").ap()
        self.ins[name] = t
        return t

    def out(self, name, shape, dt=F32):
        t = self.nc.dram_tensor(name, list(shape), dt, kind="ExternalOutput").ap()
        self.outs[name] = t
        return t

    def scratch(self, name, shape, dt=F32):
        return self.nc.dram_tensor(name, list(shape), dt).ap()

    def sb(self, es, name, shape, dt=F32):
        self.uid += 1
        return es.enter_context(self.nc.sbuf_tensor("%s_%d" % (name, self.uid), list(shape), dt))

    def close(self, out_keys):
        self.S.finish(out_keys)
        self.es.close()
        return self.nc


def load_consts(P, es):
    S = P.S
    c = {}
    ident_d = P.inp("c_ident", [128, 128])
    c["ident"] = P.sb(es, "ident", [128, 128], BF16)
    c["ones"] = P.sb(es, "ones", [128, 128], BF16)
    S.dma(c["ident"][:], ident_d, w=["ident"], q="pool")
    S.pool(lambda e: e.memset(c["ones"][:], 1.0), w=["ones"])
    return c


def emit_norm_T(P, c, xsrc, xkey, g_bc, hT, hkey, col0, work):
    S = P.S
    junk, ss, rs, hbf = work["junk"], work["ss"], work["rs"], work["hbf"]
    S.dve(lambda e: e.scalar_tensor_tensor(out=junk[:], in0=xsrc, scalar=1.0, in1=xsrc,
                                           op0=ALU.mult, op1=ALU.mult, accum_out=ss[:]),
          r=[xkey], w=["n_junk", "n_ss"])
    S.act(lambda e: e.activation(out=rs[:], in_=ss[:], func=AF.Sqrt, bias=work["eps"][:], scale=1.0 / D),
          r=["n_ss", "n_eps"], w=["n_rs"])
    S.dve(lambda e: e.reciprocal(out=rs[:], in_=rs[:]), r=["n_rs"], w=["n_rs"])
    S.dve(lambda e: e.scalar_tensor_tensor(out=hbf[:], in0=xsrc, scalar=rs[:], in1=g_bc,
                                           op0=ALU.mult, op1=ALU.mult),
          r=[xkey, "n_rs", "gbc"], w=["n_hbf"])
    for q in range(KC // 8):
        pb = P.psb[q % 2]
        for i in range(8):
            kc = q * 8 + i
            S.pe(lambda e, kc=kc, i=i: e.transpose(out=pb[:, i * 128:(i + 1) * 128],
                                                   in_=hbf[:, kc * 128:(kc + 1) * 128],
                                                   identity=c["ident"][:]),
                 r=["n_hbf", "ident"], w=[("psb", q % 2)])
        dst = hT[:, q * 8:(q + 1) * 8, col0:col0 + 128]
        src = pb[:].rearrange("p (a b) -> p a b", b=128)
        if q % 2 == 0:
            S.act(lambda e, dst=dst, src=src: e.copy(out=dst, in_=src), r=[("psb", q % 2)], w=[hkey])
        else:
            S.dve(lambda e, dst=dst, src=src: e.tensor_copy(out=dst, in_=src), r=[("psb", q % 2)], w=[hkey])


def norm_work(P, es):
    w = {
        "junk": P.sb(es, "n_junk", [128, D], BF16),
        "ss": P.sb(es, "n_ss", [128, 1], F32),
        "rs": P.sb(es, "n_rs", [128, 1], F32),
        "hbf": P.sb(es, "n_hbf", [128, D], BF16),
        "eps": P.sb(es, "n_eps", [128, 1], F32),
    }
    P.S.pool(lambda e: e.memset(w["eps"][:], EPS), w=["n_eps"])
    return w


class WStream:
    def __init__(self, P, es, name, shape, nbuf=4):
        self.P = P
        self.name = name
        self.bufs = [P.sb(es, name, shape, BF16) for _ in range(nbuf)]
        self.nbuf = nbuf
        self.pieces = []
        self.issued = 0
        self.used = 0

    def plan(self, pieces):
        self.pieces = list(pieces)
        self.issued = 0
        self.used = 0

    def _issue(self):
        i = self.issued
        b = i % self.nbuf
        dst, src = self.pieces[i](self.bufs[b])
        self.P.S.dma(dst, src, w=[(self.name, b)], q="pool")
        self.issued += 1

    def next(self):
        while self.issued < len(self.pieces) and self.issued < self.used + self.nbuf - 1:
            self._issue()
        if self.issued <= self.used:
            self._issue()
        b = self.used % self.nbuf
        self.used += 1
        return self.bufs[b], (self.name, b)


def emit_mlp(P, c, es_outer, x_res, g_bc_dram, w_up, w_down):
    S = P.S
    with ExitStack() as es:
        gbc = P.sb(es, "gbc", [128, D], F32)
        S.dma(gbc[:], g_bc_dram, w=["gbc"])
        wk = norm_work(P, es)
        hT = P.sb(es, "hT", [128, KC, 512], BF16)
        actT = P.sb(es, "actT", [128, DFF // 128, 512], BF16)
        rl = [P.sb(es, "rl", [128, 512], F32) for _ in range(2)]
        ws = WStream(P, es, "wmlp", [128, 4096], nbuf=4)
        for half in range(2):
            for t in range(4):
                j = half * 4 + t
                emit_norm_T(P, c, x_res[:, j, :], ("x", j), gbc[:], hT, "hT", t * 128, wk)
            npc = DFF // 256

            def up_piece(i):
                def f(buf):
                    dst = buf[:].rearrange("p (k n) -> p k n", n=256)
                    src = w_up[:, i * 256:(i + 1) * 256].rearrange("(k p) n -> p k n", p=128)
                    return dst, src
                return f
            ws.plan([up_piece(i) for i in range(npc)])
            for i in range(npc):
                buf, bkey = ws.next()
                wv = buf[:].rearrange("p (k n) -> p k n", n=256)
                for s in range(2):
                    oc = i * 2 + s
                    pb = P.ps[oc % 4]
                    for kc in range(KC):
                        S.pe(lambda e, kc=kc, s=s, pb=pb, wv=wv: e.matmul(
                            pb[:], lhsT=wv[:, kc, s * 128:(s + 1) * 128], rhs=hT[:, kc, :],
                            start=(kc == 0), stop=(kc == KC - 1)),
                            r=[bkey, "hT"], w=[("ps", oc % 4)])
                    r_ = rl[oc % 2]
                    S.act(lambda e, pb=pb, r_=r_: e.activation(out=r_[:], in_=pb[:], func=AF.Relu),
                          r=[("ps", oc % 4)], w=[("rl", oc % 2)])
                    S.pool(lambda e, oc=oc, r_=r_: e.tensor_tensor(out=actT[:, oc, :], in0=r_[:], in1=r_[:],
                                                                  op=ALU.mult),
                           r=[("rl", oc % 2)], w=["actT"])
            nkp = (DFF // 128) // 8

            def dn_piece(cc, kp):
                def f(buf):
                    dst = buf[:].rearrange("p (k n) -> p k n", n=512)
                    src = w_down[kp * 1024:(kp + 1) * 1024, cc * 512:(cc + 1) * 512].rearrange(
                        "(k p) n -> p k n", p=128)
                    return dst, src
                return f
            ws.plan([dn_piece(cc, kp) for cc in range(4) for kp in range(nkp)])
            for cc in range(4):
                for kp in range(nkp):
                    buf, bkey = ws.next()
                    wv = buf[:].rearrange("p (k n) -> p k n", n=512)
                    for t in range(4):
                        for k in range(8):
                            kc = kp * 8 + k
                            S.pe(lambda e, t=t, k=k, kc=kc, wv=wv: e.matmul(
                                P.ps[t][:], lhsT=actT[:, kc, t * 128:(t + 1) * 128], rhs=wv[:, k, :],
                                start=(kc == 0), stop=(kc == DFF // 128 - 1)),
                                r=[bkey, "actT"], w=[("ps", t)])
                for t in range(4):
                    j = half * 4 + t
                    xs = x_res[:, j, cc * 512:(cc + 1) * 512]
                    S.dve(lambda e, xs=xs, t=t: e.tensor_tensor(out=xs, in0=xs, in1=P.ps[t][:], op=ALU.add),
                          r=[("ps", t), ("x", j)], w=[("x", j)])
        S.barrier()


def build_mlp_only():
    P = Prog()
    S = P.S
    x_in = P.inp("x", [TL, D])
    g = P.inp("g_bc", [128, D])
    w_up = P.inp("w_up", [D, DFF])
    w_down = P.inp("w_down", [DFF, D])
    y = P.out("y", [TL, D])
    with ExitStack() as es:
        c = load_consts(P, es)
        x_res = P.sb(es, "x_res", [128, NT, D], F32)
        for j in range(NT):
            S.dma(x_res[:, j, :], x_in[j * 128:(j + 1) * 128, :], w=[("x", j)])
        emit_mlp(P, c, es, x_res, g, w_up, w_down)
        for j in range(NT):
            S.dma(y[j * 128:(j + 1) * 128, :], x_res[:, j, :], r=[("x", j)], w=[("y", j)])
        nc = P.close([("y", j) for j in range(NT)])
    return nc


def shard_tokens(a):
    b = a.reshape((NT, NCORES, 128) + a.shape[1:])
    return [np.ascontiguousarray(b[:, c].reshape((TL,) + a.shape[1:])) for c in range(NCORES)]


def unshard_tokens(parts):
    a = np.stack([p.reshape((NT, 128) + p.shape[1:]) for p in parts], axis=1)
    return np.ascontiguousarray(a.reshape((T,) + parts[0].shape[1:]))


def bc128(v):
    return np.ascontiguousarray(np.broadcast_to(np.asarray(v, np.float32).reshape(1, -1), (128, v.size)))


IDENT = np.eye(128, dtype=np.float32)


def emit_linear_tm_res(P, ws, AT, atkey, nkc, w_dram, x_res):
    S = P.S
    nkp = nkc // 8
    for half in range(2):
        def piece(cc, kp):
            def f(buf):
                dst = buf[:].rearrange("p (k n) -> p k n", n=512)
                src = w_dram[kp * 1024:(kp + 1) * 1024, cc * 512:(cc + 1) * 512].rearrange(
                    "(k p) n -> p k n", p=128)
                return dst, src
            return f
        ws.plan([piece(cc, kp) for cc in range(4) for kp in range(nkp)])
        for cc in range(4):
            for kp in range(nkp):
                buf, bkey = ws.next()
                wv = buf[:].rearrange("p (k n) -> p k n", n=512)
                for t in range(4):
                    j = half * 4 + t
                    for k in range(8):
                        kc = kp * 8 + k
                        S.pe(lambda e, t=t, j=j, k=k, kc=kc, wv=wv: e.matmul(
                            P.ps[t][:], lhsT=AT[:, kc, j * 128:(j + 1) * 128], rhs=wv[:, k, :],
                            start=(kc == 0), stop=(kc == nkc - 1)),
                            r=[bkey, atkey], w=[("ps", t)])
            for t in range(4):
                j = half * 4 + t
                xs = x_res[:, j, cc * 512:(cc + 1) * 512]
                S.dve(lambda e, xs=xs, t=t: e.tensor_tensor(out=xs, in0=xs, in1=P.ps[t][:], op=ALU.add),
                      r=[("ps", t), ("x", j)], w=[("x", j)])


def load_x(P, x_res, x_in):
    for j in range(NT):
        P.S.dma(x_res[:, j, :], x_in[j * 128:(j + 1) * 128, :], w=[("x", j)])


def store_x(P, y, x_res):
    for j in range(NT):
        P.S.dma(y[j * 128:(j + 1) * 128, :], x_res[:, j, :], r=[("x", j)], w=[("y", j)])
    return [("y", j) for j in range(NT)]


GELU_C = 0.044715
GELU_S = 1.5957691216057308


def build_R1(stage=99):
    P = Prog()
    S = P.S
    x9 = P.inp("x9", [9 * 128, D])
    g = P.inp("g_bc", [128, D])
    w_in = P.inp("w_in", [D, 2 * D])
    cw_d = P.inp("cw", [128, KC * 4])
    vec_d = P.inp("vecs", [128, KC * 4])
    wa_d = P.inp("wa", [8 * 256, 256])
    wx_d = P.inp("wx", [8 * 256, 256])
    gel_o = P.out("gel", [128, KC * TL])
    hl_o = P.out("hloc", [128, KC * TL])
    pp_o = P.out("pp", [128, KC * TL])
    ab_o = P.out("ab", [128, KC * 16])
    with ExitStack() as es:
        c = load_consts(P, es)
        gbc = P.sb(es, "gbc", [128, D], F32)
        S.dma(gbc[:], g, w=["gbc"])
        wk = norm_work(P, es)
        hT = P.sb(es, "hT9", [128, KC, 9 * 128], BF16)
        xt = [P.sb(es, "xt", [128, D], F32) for _ in range(2)]
        for t in range(9):
            S.dma(xt[t % 2][:], x9[t * 128:(t + 1) * 128, :], w=[("xt", t % 2)])
            emit_norm_T(P, c, xt[t % 2][:], ("xt", t % 2), gbc[:], hT, "hT", t * 128, wk)
        cw = P.sb(es, "cw", [128, KC, 4], F32)
        vec = P.sb(es, "vec", [128, KC, 4], F32)
        S.dma(cw[:], cw_d.rearrange("p (k n) -> p k n", n=4), w=["cw"])
        S.dma(vec[:], vec_d.rearrange("p (k n) -> p k n", n=4), w=["vec"])
        wa = P.sb(es, "wa", [128, 8, 2, 256], BF16)
        wx = P.sb(es, "wx", [128, 8, 2, 256], BF16)
        for n in range(8):
            S.dma(wa[:, n], wa_d[n * 256:(n + 1) * 256, :].rearrange("(k p) c -> p k c", p=128), w=["wa"], q="pool")
            S.dma(wx[:, n], wx_d[n * 256:(n + 1) * 256, :].rearrange("(k p) c -> p k c", p=128), w=["wx"], q="pool")
        one = P.sb(es, "one", [128, 1], F32)
        S.pool(lambda e: e.memset(one[:], 1.0), w=["one"])
        cl = P.sb(es, "cl", [128, KC], F32)
        S.act(lambda e: e.activation(out=cl[:], in_=vec[:, :, 3], func=AF.Exp, scale=-1.0), r=["vec"], w=["cl"])
        S.act(lambda e: e.activation(out=cl[:], in_=cl[:], func=AF.Ln, bias=one[:], scale=1.0),
              r=["cl", "one"], w=["cl"])
        S.dve(lambda e: e.tensor_scalar(cl[:], cl[:], -8.0, None, ALU.mult), r=["cl"], w=["cl"])
        if stage == 1:
            return P.close([])
        ws = WStream(P, es, "wrin", [128, 4096], nbuf=3)
        f4 = lambda name: P.sb(es, name, [128, TL], F32)
        gelb = [f4("gelb"), f4("gelb")]
        hlb = [f4("hlb"), f4("hlb")]
        ppb = [f4("ppb"), f4("ppb")]
        rr, ii, aa, a2, bb, ap_, dd = f4("rr"), f4("ii"), f4("aa"), f4("a2"), f4("bb"), f4("ap"), f4("dd")
        t1 = [P.sb(es, "t1", [128, 512], F32) for _ in range(2)]
        xrx = P.sb(es, "xrx", [128, 2, 8, 131], F32)
        xc = P.sb(es, "xc", [128, 2, TL], F32)
        xcb = P.sb(es, "xcb", [128, 2, TL], BF16)
        ab = P.sb(es, "ab", [128, KC, 2, 8], F32)
        S.pool(lambda e: e.memset(dd[:], 0.0), w=["dd"])

        def piece(col0):
            def f(buf):
                dst = buf[:].rearrange("p (k n) -> p k n", n=256)
                src = w_in[:, col0:col0 + 256].rearrange("(k p) n -> p k n", p=128)
                return dst, src
            return f
        pcs = []
        for n in range(8):
            pcs.append(piece(n * 256))
            pcs.append(piece(D + n * 256))
        ws.plan(pcs)
        for n in range(8):
            buf, bkey = ws.next()
            wv = buf[:].rearrange("p (k n) -> p k n", n=256)
            for s in range(2):
                ch = 2 * n + s
                gb = gelb[ch % 2]
                for tc in range(2):
                    pb = P.ps[tc]
                    for kc in range(KC):
                        S.pe(lambda e, kc=kc, s=s, tc=tc, pb=pb, wv=wv: e.matmul(
                            pb[:], lhsT=wv[:, kc, s * 128:(s + 1) * 128], rhs=hT[:, kc, tc * 512:(tc + 1) * 512],
                            start=(kc == 0), stop=(kc == KC - 1)), r=[bkey, "hT"], w=[("ps", tc)])
                    tt = t1[tc]
                    S.act(lambda e, tt=tt, pb=pb: e.activation(out=tt[:], in_=pb[:], func=AF.Square),
                          r=[("ps", tc)], w=[("t1", tc)])
                    S.dve(lambda e, tt=tt: e.tensor_scalar(tt[:], tt[:], GELU_C, 1.0, ALU.mult, ALU.add),
                          r=[("t1", tc)], w=[("t1", tc)])
                    S.dve(lambda e, tt=tt, pb=pb: e.tensor_tensor(out=tt[:], in0=tt[:], in1=pb[:], op=ALU.mult),
                          r=[("t1", tc), ("ps", tc)], w=[("t1", tc)])
                    S.act(lambda e, tt=tt: e.activation(out=tt[:], in_=tt[:], func=AF.Sigmoid, scale=GELU_S),
                          r=[("t1", tc)], w=[("t1", tc)])
                    gs = gb[:, tc * 512:(tc + 1) * 512]
                    S.dve(lambda e, tt=tt, pb=pb, gs=gs: e.tensor_tensor(out=gs, in0=tt[:], in1=pb[:], op=ALU.mult),
                          r=[("t1", tc), ("ps", tc)], w=[("gelb", ch % 2)])
                S.dma(gel_o[:, ch * TL:(ch + 1) * TL], gb[:], r=[("gelb", ch % 2)], w=[("gel_o", ch)])
            if stage == 2:
                return P.close([("gel_o", 0), ("gel_o", 1)])
            buf, bkey = ws.next()
            wv = buf[:].rearrange("p (k n) -> p k n", n=256)
            for s in range(2):
                ch = 2 * n + s
                for tc in range(3):
                    pb = P.ps[2 + tc]
                    rhs_of = (lambda kc, tc=tc: hT[:, kc, tc * 512:(tc + 1) * 512]) if tc < 2 else \
                        (lambda kc: hT[:, kc, 1024:1152])
                    po = pb[:] if tc < 2 else pb[:, 0:128]
                    for kc in range(KC):
                        S.pe(lambda e, kc=kc, s=s, po=po, wv=wv, rhs_of=rhs_of: e.matmul(
                            po, lhsT=wv[:, kc, s * 128:(s + 1) * 128], rhs=rhs_of(kc),
                            start=(kc == 0), stop=(kc == KC - 1)), r=[bkey, "hT"], w=[("ps", 2 + tc)])
                    if tc < 2:
                        S.act(lambda e, s=s, tc=tc, pb=pb: e.copy(
                            out=xrx[:, s, tc * 4:(tc + 1) * 4, 3:131],
                            in_=pb[:].rearrange("p (a b) -> p a b", b=128)),
                            r=[("ps", 2 + tc)], w=["xrx"])
                    else:
                        S.act(lambda e, s=s, pb=pb: e.copy(
                            out=xrx[:, s, :, 0:3],
                            in_=pb[:, 0:128].rearrange("p (a b) -> p a b", b=16)[:, :, 13:16]),
                            r=[("ps", 2 + tc)], w=["xrx"])
                xcv = xc[:, s, :].rearrange("p (a b) -> p a b", b=128)
                S.dve(lambda e, s=s, ch=ch, xcv=xcv: e.tensor_scalar(
                    xcv, xrx[:, s, :, 0:128], cw[:, ch, 0:1], vec[:, ch, 0:1], ALU.mult, ALU.add),
                    r=["xrx", "cw", "vec"], w=["xc"])
                for i in range(1, 4):
                    S.dve(lambda e, s=s, ch=ch, i=i, xcv=xcv: e.scalar_tensor_tensor(
                        out=xcv, in0=xrx[:, s, :, i:i + 128], scalar=cw[:, ch, i:i + 1], in1=xcv,
                        op0=ALU.mult, op1=ALU.add), r=["xrx", "cw", "xc"], w=["xc"])
                S.pool(lambda e, s=s: e.tensor_copy(out=xcb[:, s, :], in_=xc[:, s, :]), r=["xc"], w=["xcb"])
            if stage == 3:
                return P.close([("gel_o", 0), ("gel_o", 1)])
            for s in range(2):
                ch = 2 * n + s
                for (wg, dst, bcol, pbase, nm) in ((wa, rr, 1, 0, "rr"), (wx, ii, 2, 2, "ii")):
                    for tc in range(2):
                        pb = P.ps[pbase + tc]
                        for k in range(2):
                            S.pe(lambda e, k=k, s=s, tc=tc, pb=pb, wg=wg: e.matmul(
                                pb[:], lhsT=wg[:, n, k, s * 128:(s + 1) * 128], rhs=xcb[:, k, tc * 512:(tc + 1) * 512],
                                start=(k == 0), stop=(k == 1)), r=["wa", "wx", "xcb"], w=[("ps", pbase + tc)])
                        S.act(lambda e, tc=tc, pb=pb, dst=dst, bcol=bcol, ch=ch: e.activation(
                            out=dst[:, tc * 512:(tc + 1) * 512], in_=pb[:], func=AF.Sigmoid,
                            bias=vec[:, ch, bcol:bcol + 1], scale=1.0), r=[("ps", pbase + tc), "vec"], w=[nm])
                S.act(lambda e, ch=ch: e.activation(out=aa[:], in_=rr[:], func=AF.Exp, scale=cl[:, ch:ch + 1]),
                      r=["rr", "cl"], w=["aa"])
                S.pool(lambda e: e.tensor_tensor(out=a2[:], in0=aa[:], in1=aa[:], op=ALU.mult), r=["aa"], w=["a2"])
                S.act(lambda e: e.activation(out=a2[:], in_=a2[:], func=AF.Sqrt, bias=one[:], scale=-1.0),
                      r=["a2", "one"], w=["a2"])
                S.dve(lambda e, s=s: e.tensor_tensor(out=bb[:], in0=xc[:, s, :], in1=ii[:], op=ALU.mult),
                      r=["xc", "ii"], w=["bb"])
                S.dve(lambda e: e.tensor_tensor(out=bb[:], in0=bb[:], in1=a2[:], op=ALU.mult),
                      r=["bb", "a2"], w=["bb"])
                a3 = aa[:].rearrange("p (a b) -> p a b", b=128)
                ap3 = ap_[:].rearrange("p (a b) -> p a b", b=128)
                d3 = dd[:].rearrange("p (a b) -> p a b", b=128)
                S.pool(lambda e: e.tensor_copy(out=ap_[:], in_=aa[:]), r=["aa"], w=["ap"])
                S.pool(lambda e, ap3=ap3: e.memset(ap3[:, :, 0:1], 0.0), w=["ap"])
                S.pool(lambda e, d3=d3, a3=a3: e.tensor_copy(out=d3[:, :, 0:1], in_=a3[:, :, 0:1]),
                       r=["aa"], w=["dd"])
                hb, pb_ = hlb[ch % 2], ppb[ch % 2]
                S.dve(lambda e, hb=hb: e.tensor_tensor_scan(out=hb[:], data0=ap_[:], data1=bb[:], initial=0.0,
                                                            op0=ALU.mult, op1=ALU.add),
                      r=["ap", "bb"], w=[("hlb", ch % 2)])
                S.dve(lambda e, pb_=pb_: e.tensor_tensor_scan(out=pb_[:], data0=ap_[:], data1=dd[:], initial=0.0,
                                                              op0=ALU.mult, op1=ALU.add),
                      r=["ap", "dd"], w=[("ppb", ch % 2)])
                h3 = hb[:].rearrange("p (a b) -> p a b", b=128)
                p3 = pb_[:].rearrange("p (a b) -> p a b", b=128)
                S.pool(lambda e, ch=ch, p3=p3: e.tensor_copy(out=ab[:, ch, 0, :], in_=p3[:, :, 127]),
                       r=[("ppb", ch % 2)], w=["ab"])
                S.pool(lambda e, ch=ch, h3=h3: e.tensor_copy(out=ab[:, ch, 1, :], in_=h3[:, :, 127]),
                       r=[("hlb", ch % 2)], w=["ab"])
                S.dma(hl_o[:, ch * TL:(ch + 1) * TL], hb[:], r=[("hlb", ch % 2)], w=[("hl_o", ch)])
                S.dma(pp_o[:, ch * TL:(ch + 1) * TL], pb_[:], r=[("ppb", ch % 2)], w=[("pp_o", ch)])
        S.dma(ab_o, ab[:].rearrange("p a b c -> p (a b c)"), r=["ab"], w=["ab_o"])
        keys = ["ab_o"] + [(k, ch) for k in ("gel_o", "hl_o", "pp_o") for ch in range(KC)]
        nc = P.close(keys)
    return nc


def build_R2(final=False):
    P = Prog()
    S = P.S
    x_in = P.inp("x", [TL, D])
    gel_d = P.inp("gel", [128, KC * TL])
    hl_d = P.inp("hloc", [128, KC * TL])
    pp_d = P.inp("pp", [128, KC * TL])
    abg_d = P.inp("abg", [128, KC * 2 * 64])
    sel_d = P.inp("selb", [128, 8 * 64])
    w_out = P.inp("w_out", [D, D])
    g2 = P.inp("g_bc", [128, D])
    w_up = P.inp("w_up", [D, DFF])
    w_down = P.inp("w_down", [DFF, D])
    y = P.out("y", [TL, D])
    with ExitStack() as es:
        c = load_consts(P, es)
        x_res = P.sb(es, "x_res", [128, NT, D], F32)
        load_x(P, x_res, x_in)
        with ExitStack() as es2:
            yT = P.sb(es2, "yT", [128, KC, TL], BF16)
            abg = P.sb(es2, "abg", [128, KC, 2, 64], F32)
            sel = P.sb(es2, "sel", [128, 8, 64], F32)
            hs = P.sb(es2, "hs", [128, KC, 64], F32)
            carry = P.sb(es2, "carry", [128, KC, 8], F32)
            junk = P.sb(es2, "junk", [128, 64], F32)
            S.dma(abg[:], abg_d.rearrange("p (a b c) -> p a b c", b=2, c=64), w=["abg"])
            S.dma(sel[:], sel_d.rearrange("p (a b) -> p a b", b=64), w=["sel"])
            for ch in range(KC):
                S.dve(lambda e, ch=ch: e.tensor_tensor_scan(out=hs[:, ch, :], data0=abg[:, ch, 0, :],
                                                            data1=abg[:, ch, 1, :], initial=0.0,
                                                            op0=ALU.mult, op1=ALU.add),
                      r=["abg"], w=["hs"])
            for ch in range(KC):
                for j in range(NT):
                    S.dve(lambda e, ch=ch, j=j: e.scalar_tensor_tensor(
                        out=junk[:], in0=hs[:, ch, :], scalar=1.0, in1=sel[:, j, :], op0=ALU.mult, op1=ALU.mult,
                        accum_out=carry[:, ch, j:j + 1]), r=["hs", "sel"], w=["junk", "carry"])
            f4 = lambda name: P.sb(es2, name, [128, TL], F32)
            gb = [f4("gb"), f4("gb")]
            hb = [f4("hb"), f4("hb")]
            pb = [f4("pb"), f4("pb")]
            for ch in range(KC):
                q = ch % 2
                S.dma(gb[q][:], gel_d[:, ch * TL:(ch + 1) * TL], w=[("gb", q)])
                S.dma(hb[q][:], hl_d[:, ch * TL:(ch + 1) * TL], w=[("hb", q)])
                S.dma(pb[q][:], pp_d[:, ch * TL:(ch + 1) * TL], w=[("pb", q)])
                for j in range(NT):
                    sl = slice(j * 128, (j + 1) * 128)
                    S.dve(lambda e, q=q, ch=ch, j=j, sl=sl: e.scalar_tensor_tensor(
                        out=hb[q][:, sl], in0=pb[q][:, sl], scalar=carry[:, ch, j:j + 1], in1=hb[q][:, sl],
                        op0=ALU.mult, op1=ALU.add), r=[("pb", q), ("hb", q), "carry"], w=[("hb", q)])
                S.pool(lambda e, q=q, ch=ch: e.tensor_tensor(out=yT[:, ch, :], in0=hb[q][:], in1=gb[q][:],
                                                             op=ALU.mult),
                       r=[("hb", q), ("gb", q)], w=["yT"])
            ws = WStream(P, es2, "wout", [128, 4096], nbuf=3)
            emit_linear_tm_res(P, ws, yT, "yT", KC, w_out, x_res)
            S.barrier()
        emit_mlp(P, c, es, x_res, g2, w_up, w_down)
        keys = store_x(P, y, x_res)
        nc = P.close(keys)
    return nc


def halo_tile(xg, c):
    out = np.zeros((128, xg.shape[1]), np.float32)
    for j in range(NT):
        t0 = (8 * j + c) * 128 - 16
        if t0 >= 0:
            out[j * 16:(j + 1) * 16] = xg[t0:t0 + 16]
    return out


def pk(v):
    return np.ascontiguousarray(np.asarray(v, np.float32).reshape(KC, 128).T)


def r1_inputs(xg, d, j):
    xs = shard_tokens(xg)
    cw = np.stack([pk(d["rnn_conv_w"][j][i]) for i in range(4)], -1).reshape(128, KC * 4)
    vecs = np.stack([pk(d["rnn_conv_b"][j]), pk(d["rnn_gate_a_b"][j]), pk(d["rnn_gate_x_b"][j]),
                     pk(d["rnn_lambda"][j])], -1).reshape(128, KC * 4)
    common = {
        "g_bc": bc128(d["rnn_norm"][j]), "w_in": np.ascontiguousarray(d["rnn_w_in"][j]),
        "cw": np.ascontiguousarray(cw), "vecs": np.ascontiguousarray(vecs),
        "wa": np.ascontiguousarray(d["rnn_gate_a_w"][j].reshape(8 * 256, 256)),
        "wx": np.ascontiguousarray(d["rnn_gate_x_w"][j].reshape(8 * 256, 256)),
        "c_ident": IDENT,
    }
    return [dict(common, x9=np.concatenate([xs[c], halo_tile(xg, c)], 0)) for c in range(NCORES)]


def r2_inputs(xg, r1, d, j, li):
    xs = shard_tokens(xg)
    ab = np.stack([r1[c]["ab"].reshape(128, KC, 2, NT) for c in range(NCORES)], -1)
    abg = np.ascontiguousarray(ab.reshape(128, KC * 2 * 64))
    common = {
        "abg": abg, "w_out": np.ascontiguousarray(d["rnn_w_out"][j]), "g_bc": bc128(d["mlp_norm"][li]),
        "w_up": np.ascontiguousarray(d["mlp_w_up"][li]), "w_down": np.ascontiguousarray(d["mlp_w_down"][li]),
        "c_ident": IDENT,
    }
    ins = []
    for c in range(NCORES):
        sel = np.zeros((NT, 64), np.float32)
        for jj in range(NT):
            b = 8 * jj + c - 1
            if b >= 0:
                sel[jj, b] = 1.0
        selb = np.ascontiguousarray(np.broadcast_to(sel.reshape(1, NT * 64), (128, NT * 64)))
        ins.append(dict(common, x=xs[c], gel=r1[c]["gel"], hloc=r1[c]["hloc"], pp=r1[c]["pp"], selb=selb))
    return ins


TWO_PI = 6.283185307179586
CW1 = 6.28125
CW2 = TWO_PI - CW1
PI = 3.141592653589793


def emit_rope_tables(P, es, posb_d, invf_d):
    S = P.S
    posi = P.sb(es, "posi", [128, TL], I32)
    posf = P.sb(es, "posf", [128, TL], F32)
    invf = P.sb(es, "invf", [128, 2], F32)
    ang = P.sb(es, "ang", [128, TL], F32)
    kf = P.sb(es, "kf", [128, TL], F32)
    ki = P.sb(es, "ki", [128, TL], I32)
    mm = P.sb(es, "mm", [128, TL], F32)
    cosT = P.sb(es, "cosT", [128, 2, TL], F32)
    sinT = P.sb(es, "sinT", [128, 2, TL], F32)
    S.dma(posi[:], posb_d, w=["posi"])
    S.dma(invf[:], invf_d, w=["invf"])
    S.dve(lambda e: e.tensor_copy(out=posf[:], in_=posi[:]), r=["posi"], w=["posf"])

    def wrap(buf, key):
        S.dve(lambda e: e.tensor_single_scalar(out=mm[:], in_=buf, scalar=PI, op=ALU.is_gt), r=[key], w=["mm"])
        S.dve(lambda e: e.scalar_tensor_tensor(out=buf, in0=mm[:], scalar=-TWO_PI, in1=buf, op0=ALU.mult, op1=ALU.add),
              r=["mm", key], w=[key])
        S.dve(lambda e: e.tensor_single_scalar(out=mm[:], in_=buf, scalar=-PI, op=ALU.is_lt), r=[key], w=["mm"])
        S.dve(lambda e: e.scalar_tensor_tensor(out=buf, in0=mm[:], scalar=TWO_PI, in1=buf, op0=ALU.mult, op1=ALU.add),
              r=["mm", key], w=[key])

    for t in range(2):
        S.dve(lambda e, t=t: e.tensor_scalar(ang[:], posf[:], invf[:, t:t + 1], None, ALU.mult),
              r=["posf", "invf"], w=["ang"])
        S.dve(lambda e: e.tensor_scalar(kf[:], ang[:], 1.0 / TWO_PI, None, ALU.mult), r=["ang"], w=["kf"])
        S.dve(lambda e: e.tensor_copy(out=ki[:], in_=kf[:]), r=["kf"], w=["ki"])
        S.dve(lambda e: e.tensor_copy(out=kf[:], in_=ki[:]), r=["ki"], w=["kf"])
        S.dve(lambda e: e.scalar_tensor_tensor(out=ang[:], in0=kf[:], scalar=-CW1, in1=ang[:], op0=ALU.mult, op1=ALU.add),
              r=["kf", "ang"], w=["ang"])
        S.dve(lambda e: e.scalar_tensor_tensor(out=ang[:], in0=kf[:], scalar=-CW2, in1=ang[:], op0=ALU.mult, op1=ALU.add),
              r=["kf", "ang"], w=["ang"])
        wrap(ang[:], "ang")
        S.act(lambda e, t=t: e.activation(out=sinT[:, t, :], in_=ang[:], func=AF.Sin), r=["ang"], w=["sinT"])
        S.dve(lambda e: e.tensor_scalar(ang[:], ang[:], PI / 2, None, ALU.add), r=["ang"], w=["ang"])
        wrap(ang[:], "ang")
        S.act(lambda e, t=t: e.activation(out=cosT[:, t, :], in_=ang[:], func=AF.Sin), r=["ang"], w=["cosT"])
    return cosT, sinT


def emit_attn_inproj(P, c, es, hT, w_in, d_in, d_out):
    S = P.S
    cosT, sinT = emit_rope_tables(P, es, d_in["posb"], d_in["invf"])
    rt = P.sb(es, "rt", [128, 2, 128], BF16)
    S.dma(rt[:, 0, :], d_in["rt"][0:128, :], w=["rt"], q="pool")
    S.dma(rt[:, 1, :], d_in["rt"][128:256, :], w=["rt"], q="pool")
    qkg = P.sb(es, "qkg", [128, 2], F32)
    S.dma(qkg[:], d_in["qkg"], w=["qkg"])
    epsh = P.sb(es, "epsh", [128, 1], F32)
    S.pool(lambda e: e.memset(epsh[:], EPS), w=["epsh"])
    sqb = P.sb(es, "sqb", [128, 512], BF16)
    rsb = P.sb(es, "rsb", [128, 512], F32)
    xnb = P.sb(es, "xnb", [128, 512], BF16)
    t1 = P.sb(es, "t1", [128, 512], F32)
    t2 = P.sb(es, "t2", [128, 512], F32)
    ob = [P.sb(es, "ob", [128, 512], BF16) for _ in range(2)]
    ws = WStream(P, es, "wain", [128, 4096], nbuf=3)

    def head_chunk(wv, bkey, s, kind, dst_of_tc):
        tab = 0 if kind in ("q", "k") else 1
        for tc in range(2):
            pb = P.ps[tc]
            for kc in range(KC):
                S.pe(lambda e, kc=kc, pb=pb: e.matmul(
                    pb[:], lhsT=wv[:, kc, s * 128:(s + 1) * 128], rhs=hT[:, kc, tc * 512:(tc + 1) * 512],
                    start=(kc == 0), stop=(kc == KC - 1)), r=[bkey, "hT"], w=[("ps", tc)])
            tsl = slice(tc * 512, (tc + 1) * 512)
            if kind in ("q", "k"):
                gcol = 0 if kind == "q" else 1
                S.act(lambda e, pb=pb: e.activation(out=sqb[:], in_=pb[:], func=AF.Square), r=[("ps", tc)], w=["sqb"])
                S.pe(lambda e: e.matmul(P.ps[2][:], lhsT=c["ones"][:], rhs=sqb[:], start=True, stop=True),
                     r=["ones", "sqb"], w=[("ps", 2)])
                S.act(lambda e: e.activation(out=rsb[:], in_=P.ps[2][:], func=AF.Sqrt, bias=epsh[:], scale=1.0 / 128),
                      r=[("ps", 2), "epsh"], w=["rsb"])
                S.dve(lambda e: e.reciprocal(out=rsb[:], in_=rsb[:]), r=["rsb"], w=["rsb"])
                S.dve(lambda e, pb=pb, gcol=gcol: e.scalar_tensor_tensor(
                    out=xnb[:], in0=pb[:], scalar=qkg[:, gcol:gcol + 1], in1=rsb[:], op0=ALU.mult, op1=ALU.mult),
                    r=[("ps", tc), "qkg", "rsb"], w=["xnb"])
            else:
                S.act(lambda e, pb=pb: e.copy(out=xnb[:], in_=pb[:]), r=[("ps", tc)], w=["xnb"])
            S.pe(lambda e, tab=tab: e.matmul(P.ps[3][:], lhsT=rt[:, tab, :], rhs=xnb[:], start=True, stop=True),
                 r=["rt", "xnb"], w=[("ps", 3)])
            S.dve(lambda e, tab=tab, tsl=tsl: e.tensor_tensor(out=t1[:], in0=xnb[:], in1=cosT[:, tab, tsl], op=ALU.mult),
                  r=["xnb", "cosT"], w=["t1"])
            S.dve(lambda e, tab=tab, tsl=tsl: e.tensor_tensor(out=t2[:], in0=P.ps[3][:], in1=sinT[:, tab, tsl], op=ALU.mult),
                  r=[("ps", 3), "sinT"], w=["t2"])
            o = ob[tc]
            S.pool(lambda e, o=o: e.tensor_tensor(out=o[:], in0=t1[:], in1=t2[:], op=ALU.add),
                   r=["t1", "t2"], w=[("ob", tc)])
            S.dma(dst_of_tc(tc), o[:], r=[("ob", tc)], w=[("hp_out", P.uid)])
            P.uid += 1

    def piece(col0, ncols):
        def f(buf):
            dst = buf[:, 0:KC * ncols].rearrange("p (k n) -> p k n", n=ncols)
            src = w_in[:, col0:col0 + ncols].rearrange("(k p) n -> p k n", p=128)
            return dst, src
        return f

    groups = [("q", 0, 16, d_out["qT"]), ("k", 2048, 4, d_out["kT"]), ("iq", 3072, 16, d_out["iqT"])]
    pcs = []
    for kind, col0, nch, _ in groups:
        for i in range(nch // 2):
            pcs.append(piece(col0 + i * 256, 256))
    pcs.append(piece(5120, 144))
    ws.plan(pcs)
    for kind, col0, nch, dst in groups:
        for i in range(nch // 2):
            buf, bkey = ws.next()
            wv = buf[:].rearrange("p (k n) -> p k n", n=256)
            for s in range(2):
                hh = 2 * i + s
                head_chunk(wv, bkey, s, kind,
                           lambda tc, hh=hh, dst=dst: dst[:, hh * TL + tc * 512: hh * TL + (tc + 1) * 512])
    buf, bkey = ws.next()
    wv = buf[:, 0:KC * 144].rearrange("p (k n) -> p k n", n=144)
    head_chunk(wv, bkey, 0, "ik", lambda tc: d_out["ikT"][:, tc * 512:(tc + 1) * 512])
    iwsb = P.sb(es, "iwsb", [128, NT, 16], F32)
    for t in range(NT):
        for kc in range(KC):
            S.pe(lambda e, kc=kc, t=t: e.matmul(P.ps[4][:, t * 16:(t + 1) * 16], lhsT=hT[:, kc, t * 128:(t + 1) * 128],
                                                rhs=wv[:, kc, 128:144], start=(kc == 0), stop=(kc == KC - 1)),
                 r=[bkey, "hT"], w=[("ps", 4)])
    S.act(lambda e: e.copy(out=iwsb[:], in_=P.ps[4][:, 0:NT * 16].rearrange("p (a b) -> p a b", b=16)),
          r=[("ps", 4)], w=["iwsb"])
    S.dma(d_out["iw"], iwsb[:].rearrange("p a b -> p (a b)"), r=["iwsb"], w=["iw_out"])
    vsb = [P.sb(es, "vsb", [128, 512], BF16) for _ in range(2)]

    def vpiece(kp):
        def f(buf):
            dst = buf[:].rearrange("p (k n) -> p k n", n=512)
            src = w_in[kp * 1024:(kp + 1) * 1024, 2560:3072].rearrange("(k p) n -> p k n", p=128)
            return dst, src
        return f
    for half in range(2):
        ws.plan([vpiece(0), vpiece(1)])
        for kp in range(2):
            buf, bkey = ws.next()
            wv = buf[:].rearrange("p (k n) -> p k n", n=512)
            for t in range(4):
                j = half * 4 + t
                for k in range(8):
                    kc = kp * 8 + k
                    S.pe(lambda e, t=t, j=j, k=k, kc=kc, wv=wv: e.matmul(
                        P.ps[t][:], lhsT=hT[:, kc, j * 128:(j + 1) * 128], rhs=wv[:, k, :],
                        start=(kc == 0), stop=(kc == KC - 1)), r=[bkey, "hT"], w=[("ps", t)])
        for t in range(4):
            j = half * 4 + t
            S.act(lambda e, t=t: e.copy(out=vsb[t % 2][:], in_=P.ps[t][:]), r=[("ps", t)], w=[("vsb", t % 2)])
            S.dma(d_out["v"][j * 128:(j + 1) * 128, :], vsb[t % 2][:], r=[("vsb", t % 2)], w=[("v_out", j)])
    keys = ["iw_out"] + [("v_out", j) for j in range(NT)]
    return keys


def attn_a1_io(P):
    d_in = {"posb": P.inp("posb", [128, TL], I32), "invf": P.inp("invf", [128, 2]),
            "rt": P.inp("rt", [256, 128]), "qkg": P.inp("qkg", [128, 2])}
    d_out = {"qT": P.out("qT", [128, 16 * TL], BF16), "kT": P.out("kT", [128, 4 * TL], BF16),
             "iqT": P.out("iqT", [128, 16 * TL], BF16), "ikT": P.out("ikT", [128, TL], BF16),
             "iw": P.out("iw", [128, NT * 16]), "v": P.out("v", [TL, 512], BF16)}
    return d_in, d_out


def build_A1():
    P = Prog()
    S = P.S
    x_in = P.inp("x", [TL, D])
    g = P.inp("g_bc", [128, D])
    w_in = P.inp("w_in", [D, A_IN])
    d_in, d_out = attn_a1_io(P)
    with ExitStack() as es:
        c = load_consts(P, es)
        gbc = P.sb(es, "gbc", [128, D], F32)
        S.dma(gbc[:], g, w=["gbc"])
        wk = norm_work(P, es)
        hT = P.sb(es, "hT", [128, KC, TL], BF16)
        xt = [P.sb(es, "xt", [128, D], F32) for _ in range(2)]
        for t in range(NT):
            S.dma(xt[t % 2][:], x_in[t * 128:(t + 1) * 128, :], w=[("xt", t % 2)])
            emit_norm_T(P, c, xt[t % 2][:], ("xt", t % 2), gbc[:], hT, "hT", t * 128, wk)
        keys = emit_attn_inproj(P, c, es, hT, w_in, d_in, d_out)
        S.barrier()
        nc = P.close(keys)
    return nc


def rope_consts():
    inv_h = (1.0 / (10000.0 ** (np.arange(0, 128, 2, dtype=np.float32) / np.float32(128)))).astype(np.float32)
    inv_i = (1.0 / (10000.0 ** (np.arange(0, 64, 2, dtype=np.float32) / np.float32(64)))).astype(np.float32)
    invf = np.zeros((128, 2), np.float32)
    invf[:, 0] = np.concatenate([inv_h, inv_h])
    invf[:64, 1] = np.concatenate([inv_i, inv_i])
    R = np.zeros((128, 128), np.float32)
    for i in range(64):
        R[i, i + 64] = -1.0
        R[i + 64, i] = 1.0
    R2 = np.zeros((128, 128), np.float32)
    for i in range(32):
        R2[i, i + 32] = -1.0
        R2[i + 32, i] = 1.0
    rt = np.concatenate([R.T, R2.T], 0)
    return invf, np.ascontiguousarray(rt)


def a1_inputs(xs, pos, d, j, with_x=True):
    invf, rt = rope_consts()
    ps = shard_tokens(np.asarray(pos).reshape(T))
    qkg = np.ascontiguousarray(np.stack([d["attn_q_norm"][j], d["attn_k_norm"][j]], -1).astype(np.float32))
    ins = []
    for c in range(NCORES):
        m = {"posb": np.ascontiguousarray(np.broadcast_to(ps[c].astype(np.int32).reshape(1, TL), (128, TL))),
             "invf": invf, "rt": rt, "qkg": qkg, "w_in": np.ascontiguousarray(d["attn_w_in"][j]), "c_ident": IDENT}
        if with_x:
            m["x"] = xs[c]
            m["g_bc"] = bc128(d["attn_norm"][j])
        ins.append(m)
    return ins


def emit_attention(P, c, es, d, oT_s):
    S = P.S
    ikT = P.sb(es, "ikT", [128, T], BF16)
    for q in range(4):
        S.dma(ikT[:, q * 2048:(q + 1) * 2048], d["ikTa"][:, q * 2048:(q + 1) * 2048], w=["ikT"])
    iwsb = P.sb(es, "iwsb", [128, NT, 16], F32)
    S.dma(iwsb[:], d["iw"].rearrange("p (a b) -> p a b", b=16), w=["iwsb"])
    cm = P.sb(es, "cm", [128, 1024], F32)
    pen = P.sb(es, "pen", [128, 1024], F32)
    S.dma(cm[:], d["cm"], w=["cm"])
    S.dma(pen[:], d["pen"], w=["pen"])
    score = P.sb(es, "score", [128, T], F32)
    junk = P.sb(es, "junkb", [128, T], BF16)
    maskT = P.sb(es, "maskT", [128, T // 128, 128], BF16)
    mkb = [P.sb(es, "mkb", [128, 512], BF16) for _ in range(2)]
    rl = [P.sb(es, "rl", [128, 512], F32) for _ in range(2)]
    iqtb = [P.sb(es, "iqt", [128, 16, 128], BF16) for _ in range(2)]
    qt = [P.sb(es, "qt", [128, 4, 128], BF16) for _ in range(2)]
    kTb = [P.sb(es, "kTg", [128, T], BF16) for _ in range(2)]
    vgb = [P.sb(es, "vg", [128, T // 128, 128], BF16) for _ in range(2)]
    ptb = [P.sb(es, "ptb", [128, 512], BF16) for _ in range(3)]
    rden = P.sb(es, "rden", [128, 512], F32)
    ot = [P.sb(es, "ot", [128, 512], F32) for _ in range(2)]
    sm = {n: P.sb(es, n, [128, 1], F32) for n in ("M", "lo", "hi", "mid", "cnt", "pred", "dl")}
    iq3 = d["iqT"].rearrange("p (h t) -> p h t", t=TL)
    q3 = d["qT"].rearrange("p (h t) -> p h t", t=TL)
    o3 = oT_s.rearrange("p (h t) -> p h t", t=TL)
    SC = 128.0 ** -0.5
    okeys = []

    def gen_indexer(j):
        Kc = 1024 * (j + 1)
        iqt = iqtb[j % 2]
        S.dma(iqt[:], iq3[:, :, j * 128:(j + 1) * 128], w=[("iqt", j % 2)])
        for k5 in range(Kc // 512):
            sc = score[:, k5 * 512:(k5 + 1) * 512]
            for h in range(16):
                pb = P.ps[h % 2]
                S.pe(lambda e, h=h, pb=pb, k5=k5: e.matmul(pb[:], lhsT=iqt[:, h, :], rhs=ikT[:, k5 * 512:(k5 + 1) * 512],
                                                         start=True, stop=True), r=[("iqt", j % 2), "ikT"], w=[("ps", h % 2)])
                r_ = rl[h % 2]
                S.act(lambda e, pb=pb, r_=r_: e.activation(out=r_[:], in_=pb[:], func=AF.Relu),
                      r=[("ps", h % 2)], w=[("rl", h % 2)])
                if h == 0:
                    S.dve(lambda e, r_=r_, sc=sc: e.tensor_scalar(sc, r_[:], iwsb[:, j, 0:1], None, ALU.mult),
                          r=[("rl", h % 2), "iwsb"], w=["score"])
                else:
                    S.dve(lambda e, r_=r_, sc=sc, h=h: e.scalar_tensor_tensor(
                        out=sc, in0=r_[:], scalar=iwsb[:, j, h:h + 1], in1=sc, op0=ALU.mult, op1=ALU.add),
                        r=[("rl", h % 2), "iwsb", "score"], w=["score"])
                yield

    def post_indexer(j):
        Kc = 1024 * (j + 1)
        S.dve(lambda e: e.tensor_reduce(out=sm["M"][:], in_=score[:, 0:Kc], axis=AX.X, op=ALU.max), r=["score"], w=["M"])
        S.dve(lambda e: e.tensor_reduce(out=sm["cnt"][:], in_=score[:, 0:Kc], axis=AX.X, op=ALU.min), r=["score"], w=["cnt"])
        S.dve(lambda e: e.tensor_tensor(out=sm["dl"][:], in0=sm["M"][:], in1=sm["cnt"][:], op=ALU.subtract),
              r=["M", "cnt"], w=["dl"])
        win = score[:, Kc - 1024:Kc]
        S.dve(lambda e: e.tensor_tensor(out=win, in0=win, in1=cm[:], op=ALU.mult), r=["score", "cm"], w=["score"])
        S.dve(lambda e: e.tensor_tensor(out=win, in0=win, in1=pen[:], op=ALU.add), r=["score", "pen"], w=["score"])
        S.dve(lambda e: e.scalar_tensor_tensor(out=sm["lo"][:], in0=sm["dl"][:], scalar=-0.001, in1=sm["cnt"][:],
                                               op0=ALU.mult, op1=ALU.add), r=["dl", "cnt"], w=["lo"])
        S.dve(lambda e: e.tensor_scalar(sm["lo"][:], sm["lo"][:], -1e-6, None, ALU.add), r=["lo"], w=["lo"])
        S.dve(lambda e: e.tensor_scalar(sm["hi"][:], sm["dl"][:], 1.002, 2e-6, ALU.mult, ALU.add), r=["dl"], w=["hi"])
        for it in range(NBIS):
            S.dve(lambda e: e.tensor_scalar(sm["hi"][:], sm["hi"][:], 0.5, None, ALU.mult), r=["hi"], w=["hi"])
            S.dve(lambda e: e.tensor_tensor(out=sm["mid"][:], in0=sm["lo"][:], in1=sm["hi"][:], op=ALU.add),
                  r=["lo", "hi"], w=["mid"])
            S.dve(lambda e: e.tensor_scalar(junk[:, 0:Kc], score[:, 0:Kc], sm["mid"][:], 0.0, ALU.is_ge, ALU.add,
                                            accum_out=sm["cnt"][:]), r=["score", "mid"], w=["junk", "cnt"])
            S.dve(lambda e: e.tensor_scalar(sm["pred"][:], sm["cnt"][:], float(TOPK), None, ALU.is_ge),
                  r=["cnt"], w=["pred"])
            S.dve(lambda e: e.scalar_tensor_tensor(out=sm["lo"][:], in0=sm["hi"][:], scalar=sm["pred"][:], in1=sm["lo"][:],
                                                   op0=ALU.mult, op1=ALU.add), r=["hi", "pred", "lo"], w=["lo"])
        for k5 in range(Kc // 512):
            mk = mkb[k5 % 2]
            S.dve(lambda e, mk=mk, k5=k5: e.tensor_scalar(mk[:], score[:, k5 * 512:(k5 + 1) * 512], sm["lo"][:], None,
                                                          ALU.is_ge), r=["score", "lo"], w=[("mkb", k5 % 2)])
            pbb = P.psb[k5 % 2]
            for i in range(4):
                S.pe(lambda e, mk=mk, i=i, pbb=pbb: e.transpose(out=pbb[:, i * 128:(i + 1) * 128],
                                                                in_=mk[:, i * 128:(i + 1) * 128], identity=c["ident"][:]),
                     r=[("mkb", k5 % 2), "ident"], w=[("psb", k5 % 2)])
            S.act(lambda e, k5=k5, pbb=pbb: e.copy(out=maskT[:, k5 * 4:(k5 + 1) * 4, :],
                                                   in_=pbb[:, 0:512].rearrange("p (a b) -> p a b", b=128)),
                  r=[("psb", k5 % 2)], w=["maskT"])

    def gen_attention(j):
        Kc = 1024 * (j + 1)
        n1 = Kc // 128
        for g in range(4):
            gi = j * 4 + g
            qg = qt[gi % 2]
            kT = kTb[gi % 2]
            vg = vgb[gi % 2]
            kk, vk = ("kTg", gi % 2), ("vg", gi % 2)
            S.dma(qg[:], q3[:, 4 * g:4 * g + 4, j * 128:(j + 1) * 128], w=[("qt", gi % 2)])
            S.dma(kT[:, 0:Kc], d["kTa"][:, g * T:g * T + Kc], w=[kk])
            for k0 in range(0, n1, 16):
                S.dma(vg[:, k0:k0 + 16, :],
                      d["va"][k0 * 128:(k0 + 16) * 128, g * 128:(g + 1) * 128].rearrange("(k p) e -> p k e", p=128),
                      w=[vk])
            qg2 = qg[:].rearrange("p a b -> p (a b)")

            def st(kc, kT=kT, kk=kk, qg2=qg2, gi=gi):
                pb = P.ps[2 + kc % 2]
                S.pe(lambda e, pb=pb: e.matmul(pb[:], lhsT=kT[:, kc * 128:(kc + 1) * 128], rhs=qg2, start=True, stop=True),
                     r=[kk, ("qt", gi % 2)], w=[("ps", 2 + kc % 2)])
            st(0)
            for kc in range(n1):
                if kc + 1 < n1:
                    st(kc + 1)
                pb = P.ps[2 + kc % 2]
                pt = ptb[kc % 3]
                S.act(lambda e, pb=pb, pt=pt: e.activation(out=pt[:], in_=pb[:], func=AF.Exp, scale=SC),
                      r=[("ps", 2 + kc % 2)], w=[("ptb", kc % 3)])
                pt3 = pt[:].rearrange("p (a b) -> p a b", b=128)
                mb = maskT[:, kc:kc + 1, :].to_broadcast([128, 4, 128])
                S.pool(lambda e, pt3=pt3, mb=mb: e.tensor_tensor(out=pt3, in0=pt3, in1=mb, op=ALU.mult),
                       r=[("ptb", kc % 3), "maskT"], w=[("ptb", kc % 3)])
                S.pe(lambda e, kc=kc, pt=pt, vg=vg: e.matmul(P.ps[4][:], lhsT=vg[:, kc, :], rhs=pt[:],
                                                             start=(kc == 0), stop=(kc == n1 - 1)),
                     r=[vk, ("ptb", kc % 3)], w=[("ps", 4)])
                S.pe(lambda e, kc=kc, pt=pt: e.matmul(P.ps[5][:], lhsT=c["ones"][:], rhs=pt[:],
                                                      start=(kc == 0), stop=(kc == n1 - 1)),
                     r=["ones", ("ptb", kc % 3)], w=[("ps", 5)])
                yield
            S.dve(lambda e: e.reciprocal(out=rden[:], in_=P.ps[5][:]), r=[("ps", 5)], w=["rden"])
            o = ot[g % 2]
            S.dve(lambda e, o=o: e.tensor_tensor(out=o[:], in0=P.ps[4][:], in1=rden[:], op=ALU.mult),
                  r=[("ps", 4), "rden"], w=[("ot", g % 2)])
            S.dma(o3[:, 4 * g:4 * g + 4, j * 128:(j + 1) * 128], o[:].rearrange("p (a b) -> p a b", b=128),
                  r=[("ot", g % 2)], w=[("oT_s", j, g)])
            okeys.append(("oT_s", j, g))

    for _ in gen_indexer(0):
        pass
    post_indexer(0)
    for j in range(NT):
        ga = gen_attention(j)
        gx = gen_indexer(j + 1) if j + 1 < NT else iter(())
        da = dx = False
        while not (da and dx):
            if not da:
                try:
                    next(ga)
                except StopIteration:
                    da = True
            if not dx:
                try:
                    next(gx)
                except StopIteration:
                    dx = True
        if j + 1 < NT:
            post_indexer(j + 1)
    return okeys


def build_A2(dbg=False):
    P = Prog()
    S = P.S
    x_in = P.inp("x", [TL, D])
    d = {"qT": P.inp("qT", [128, 16 * TL], BF16), "iqT": P.inp("iqT", [128, 16 * TL], BF16),
         "iw": P.inp("iw", [128, NT * 16]), "kTa": P.inp("kTa", [128, 4 * T], BF16), "va": P.inp("va", [T, 512], BF16),
         "ikTa": P.inp("ikTa", [128, T], BF16),
         "cm": P.inp("cm", [128, 1024]), "pen": P.inp("pen", [128, 1024])}
    w_out = P.inp("w_out", [D, D])
    g2 = P.inp("g_bc", [128, D])
    w_up = P.inp("w_up", [D, DFF])
    w_down = P.inp("w_down", [DFF, D])
    y = P.out("y", [TL, D])
    oT_s = P.out("oT_dbg", [128, 16 * TL]) if dbg else P.scratch("oT_s", [128, 16 * TL])
    with ExitStack() as es:
        c = load_consts(P, es)
        with ExitStack() as es1:
            emit_attention(P, c, es1, d, oT_s)
            S.barrier()
        x_res = P.sb(es, "x_res", [128, NT, D], F32)
        load_x(P, x_res, x_in)
        with ExitStack() as es2:
            oT = P.sb(es2, "oT", [128, KC, TL], BF16)
            S.dma(oT[:], oT_s.rearrange("p (h t) -> p h t", t=TL), w=["oT"], q="pool")
            ws = WStream(P, es2, "wout", [128, 4096], nbuf=3)
            emit_linear_tm_res(P, ws, oT, "oT", KC, w_out, x_res)
            S.barrier()
        emit_mlp(P, c, es, x_res, g2, w_up, w_down)
        keys = store_x(P, y, x_res)
        nc = P.close(keys)
    return nc


def a2_inputs(xs, a1, d, j, li):
    kT = np.stack([a1[c]["kT"].reshape(128, 4, NT, 128) for c in range(NCORES)], 3)
    kTa = np.ascontiguousarray(kT.reshape(128, 4 * T))
    ik = np.stack([a1[c]["ikT"].reshape(128, NT, 128) for c in range(NCORES)], 2)
    ikTa = np.ascontiguousarray(ik.reshape(128, T))
    va = unshard_tokens([a1[c]["v"] for c in range(NCORES)])
    common = {"kTa": kTa, "va": va, "ikTa": ikTa, "w_out": np.ascontiguousarray(d["attn_w_out"][j]),
              "g_bc": bc128(d["mlp_norm"][li]), "w_up": np.ascontiguousarray(d["mlp_w_up"][li]),
              "w_down": np.ascontiguousarray(d["mlp_w_down"][li]), "c_ident": IDENT}
    ins = []
    f = np.arange(1024).reshape(1, 1024)
    p = np.arange(128).reshape(128, 1)
    for c in range(NCORES):
        cm = (f <= 128 * c + p)
        ins.append(dict(common, x=xs[c], qT=a1[c]["qT"], iqT=a1[c]["iqT"], iw=a1[c]["iw"],
                        cm=np.where(cm, np.float32(1.0), np.float32(0.0)).astype(np.float32),
                        pen=np.where(cm, np.float32(0.0), np.float32(-BIG)).astype(np.float32)))
    return ins


POOL_W = (2, 4, 8, 16)


def emit_pool(P, c, es_outer, x_res, hT9, d):
    S = P.S
    with ExitStack() as es:
        hx = P.sb(es, "hx", [128, 8, 144], F32)
        sA = P.sb(es, "sA", [128, 8, 144], F32)
        sB = P.sb(es, "sB", [128, 8, 144], F32)
        yT = P.sb(es, "yTg", [128, 4, TL], BF16)
        rd = P.sb(es, "rd", [128, TL], F32)
        wp = P.sb(es, "wp", [128, 4, 4, 512], BF16)
        bbc = P.sb(es, "bbc", [128, D], F32)
        sbc = P.sb(es, "sbc", [128, D], F32)
        tt = [P.sb(es, "ptt", [128, 512], F32) for _ in range(2)]
        for g in range(4):
            S.dma(wp[:, g], d["pool_w"][g * 512:(g + 1) * 512, :].rearrange("(k p) n -> p k n", p=128), w=["wp"], q="pool")
        S.dma(bbc[:], d["pool_b"], w=["bbc"])
        S.dma(sbc[:], d["pool_s"], w=["sbc"])
        for g in range(4):
            w = POOL_W[g]
            S.dma(rd[:], d["mind"][:, g * TL:(g + 1) * TL], w=["rd"])
            S.dve(lambda e: e.reciprocal(out=rd[:], in_=rd[:]), r=["rd"], w=["rd"])
            rd3 = rd[:].rearrange("p (a b) -> p a b", b=128)
            for ci in range(4):
                ch = 4 * g + ci
                S.act(lambda e, ch=ch: e.copy(out=hx[:, :, 16:144], in_=hT9[:, ch, 0:TL].rearrange("p (a b) -> p a b", b=128)),
                      r=["hT"], w=["hx"])
                S.act(lambda e, ch=ch: e.copy(out=hx[:, :, 0:16], in_=hT9[:, ch, TL:TL + 128].rearrange("p (a b) -> p a b", b=16)),
                      r=["hT"], w=["hx"])
                cur, ck = hx, "hx"
                nxt = [(sA, "sA"), (sB, "sB")]
                step = 1
                k = 0
                while step < w:
                    o, ok = nxt[k % 2]
                    S.dve(lambda e, o=o, cur=cur, step=step: e.tensor_tensor(
                        out=o[:, :, step:144], in0=cur[:, :, step:144], in1=cur[:, :, 0:144 - step], op=ALU.add),
                        r=[ck], w=[ok])
                    cur, ck = o, ok
                    step *= 2
                    k += 1
                S.dve(lambda e, cur=cur, rd3=rd3: e.tensor_tensor(out=cur[:, :, 16:144], in0=cur[:, :, 16:144], in1=rd3,
                                                                 op=ALU.mult), r=[ck, "rd"], w=[ck])
                S.dve(lambda e, cur=cur, ci=ci: e.tensor_tensor(
                    out=yT[:, ci, :].rearrange("p (a b) -> p a b", b=128), in0=cur[:, :, 16:144], in1=hx[:, :, 16:144],
                    op=ALU.subtract), r=[ck, "hx"], w=["yTg"])
            for j in range(NT):
                pb = P.ps[j % 4]
                for kc in range(4):
                    S.pe(lambda e, kc=kc, j=j, pb=pb, g=g: e.matmul(pb[:], lhsT=yT[:, kc, j * 128:(j + 1) * 128],
                                                                   rhs=wp[:, g, kc, :], start=(kc == 0), stop=(kc == 3)),
                         r=["yTg", "wp"], w=[("ps", j % 4)])
                t_ = tt[j % 2]
                gs = slice(g * 512, (g + 1) * 512)
                S.dve(lambda e, t_=t_, pb=pb, gs=gs: e.tensor_tensor(out=t_[:], in0=pb[:], in1=bbc[:, gs], op=ALU.add),
                      r=[("ps", j % 4), "bbc"], w=[("ptt", j % 2)])
                S.pool(lambda e, t_=t_, gs=gs: e.tensor_tensor(out=t_[:], in0=t_[:], in1=sbc[:, gs], op=ALU.mult),
                       r=[("ptt", j % 2), "sbc"], w=[("ptt", j % 2)])
                xs_ = x_res[:, j, gs]
                S.dve(lambda e, t_=t_, xs_=xs_: e.tensor_tensor(out=xs_, in0=xs_, in1=t_[:], op=ALU.add),
                      r=[("ptt", j % 2), ("x", j)], w=[("x", j)])
        S.barrier()


def build_PA1():
    P = Prog()
    S = P.S
    x9 = P.inp("x9", [9 * 128, D])
    gp = P.inp("gp_bc", [128, D])
    dp = {"pool_w": P.inp("pool_w", [D, 512]), "pool_b": P.inp("pool_b", [128, D]), "pool_s": P.inp("pool_s", [128, D]),
          "mind": P.inp("mind", [128, 4 * TL])}
    g2 = P.inp("g_bc", [128, D])
    w_up = P.inp("w_up", [D, DFF])
    w_down = P.inp("w_down", [DFF, D])
    ga = P.inp("ga_bc", [128, D])
    w_in = P.inp("w_in", [D, A_IN])
    d_in, d_out = attn_a1_io(P)
    y = P.out("y", [TL, D])
    with ExitStack() as es:
        c = load_consts(P, es)
        x_res = P.sb(es, "x_res", [128, NT, D], F32)
        load_x(P, x_res, x9)
        with ExitStack() as es1:
            gbc = P.sb(es1, "gbc", [128, D], F32)
            S.dma(gbc[:], gp, w=["gbc"])
            wk = norm_work(P, es1)
            hT9 = P.sb(es1, "hT9", [128, KC, 9 * 128], BF16)
            xt = P.sb(es1, "xt", [128, D], F32)
            S.dma(xt[:], x9[TL:TL + 128, :], w=["xt"])
            for t in range(NT):
                emit_norm_T(P, c, x_res[:, t, :], ("x", t), gbc[:], hT9, "hT", t * 128, wk)
            emit_norm_T(P, c, xt[:], "xt", gbc[:], hT9, "hT", TL, wk)
            emit_pool(P, c, es1, x_res, hT9, dp)
        emit_mlp(P, c, es, x_res, g2, w_up, w_down)
        keys = store_x(P, y, x_res)
        with ExitStack() as es2:
            gbc = P.sb(es2, "gbc", [128, D], F32)
            S.dma(gbc[:], ga, w=["gbc"])
            wk = norm_work(P, es2)
            hT = P.sb(es2, "hT", [128, KC, TL], BF16)
            for t in range(NT):
                emit_norm_T(P, c, x_res[:, t, :], ("x", t), gbc[:], hT, "hT", t * 128, wk)
            keys += emit_attn_inproj(P, c, es2, hT, w_in, d_in, d_out)
            S.barrier()
        nc = P.close(keys)
    return nc


def pa1_inputs(xg, pos, d, li, ja):
    xs = shard_tokens(xg)
    a1 = a1_inputs(xs, pos, d, ja, with_x=False)
    common = {"gp_bc": bc128(d["pool_norm"][0]), "pool_w": np.ascontiguousarray(d["pool_w"][0].reshape(D, 512)),
              "pool_b": bc128(d["pool_b"][0].reshape(-1)), "pool_s": bc128(d["pool_scale"][0]),
              "g_bc": bc128(d["mlp_norm"][li]), "w_up": np.ascontiguousarray(d["mlp_w_up"][li]),
              "w_down": np.ascontiguousarray(d["mlp_w_down"][li]), "ga_bc": bc128(d["attn_norm"][ja])}
    ins = []
    for c in range(NCORES):
        idx = (np.arange(NT).reshape(NT, 1) * 8 + c) * 128 + np.arange(128).reshape(1, 128)
        mind = np.stack([np.minimum(idx + 1, w) for w in POOL_W], 0).reshape(1, 4 * TL).astype(np.float32)
        m = dict(common, **a1[c])
        m["x9"] = np.concatenate([xs[c], halo_tile(xg, c)], 0)
        m["mind"] = np.ascontiguousarray(np.broadcast_to(mind, (128, 4 * TL)))
        ins.append(m)
    return ins


_CACHE = {}


def _prog(name, fn):
    if name not in _CACHE:
        _CACHE[name] = fn()
    return _CACHE[name]


def _run(nc, ins):
    return run_bass_kernel_spmd(nc, ins, core_ids=list(range(NCORES))).results


def kernel(**inp):
    d = {k: np.asarray(v) for k, v in inp.items()}
    xg = np.ascontiguousarray(d["x"].reshape(T, D).astype(np.float32, copy=False))
    pos = d["positions"].reshape(T)
    xs = shard_tokens(xg)
    a1 = _run(_prog("A1", build_A1), a1_inputs(xs, pos, d, 0))
    r = _run(_prog("A2", build_A2), a2_inputs(xs, a1, d, 0, 0))
    xg = unshard_tokens([r[c]["y"] for c in range(NCORES)])
    r1 = _run(_prog("R1", build_R1), r1_inputs(xg, d, 0))
    r = _run(_prog("R2", build_R2), r2_inputs(xg, r1, d, 0, 1))
    xg = unshard_tokens([r[c]["y"] for c in range(NCORES)])
    r = _run(_prog("PA1", build_PA1), pa1_inputs(xg, pos, d, 2, 1))
    xg = unshard_tokens([r[c]["y"] for c in range(NCORES)])
    xs = shard_tokens(xg)
    r = _run(_prog("A2", build_A2), a2_inputs(xs, r, d, 1, 3))
    out = unshard_tokens([r[c]["y"] for c in range(NCORES)])
    return out.reshape(1, T, D).astype(np.float32, copy=False)
```

```python
import numpy as np
from contextlib import ExitStack
import concourse.bass as bass
import concourse.mybir as mybir
from concourse.bass_utils import run_bass_kernel_spmd

F32 = mybir.dt.float32
BF16 = mybir.dt.bfloat16
I32 = mybir.dt.int32
ALU = mybir.AluOpType
AF = mybir.ActivationFunctionType
AX = mybir.AxisListType

NCORES = 8
T = 8192
D = 2048
TL = T // NCORES
NT = TL // 128
KC = D // 128
DFF = 4 * D
EPS = 1e-6
A_IN = 5264
TOPK = 256
NBIS = 21
BIG = 1.0e30


class Sched:
    NDS = 40

    def __init__(self, nc, es):
        self.nc = nc
        self.E = {"pe": nc.tensor, "act": nc.scalar, "dve": nc.vector, "pool": nc.gpsimd, "sp": nc.sync}
        self.csem = {e: es.enter_context(nc.semaphore("c_" + e)) for e in ("pe", "act", "dve", "pool")}
        self.ccnt = {e: 0 for e in self.csem}
        self.dsem = [es.enter_context(nc.semaphore("d%d" % i)) for i in range(self.NDS)]
        self.dcnt = [0] * self.NDS
        self.drr = 0
        self.known = {e: {} for e in self.E}
        self.lastw = {}
        self.readers = {}
        self.nins = 0

    def _wait(self, eng, ev):
        sid, h, val, src, isdma = ev
        if src == "pe" and eng == "pe" and not isdma:
            return
        if self.known[eng].get(sid, 0) >= val:
            return
        self.E[eng].wait_ge(h, val)
        self.known[eng][sid] = val

    def op(self, eng, fn, r=(), w=(), dma=False):
        deps = []
        for k in r:
            if k in self.lastw:
                deps.append(self.lastw[k])
        for k in w:
            if k in self.lastw:
                deps.append(self.lastw[k])
            deps.extend(self.readers.get(k, {}).values())
        for ev in deps:
            self._wait(eng, ev)
        if dma:
            i = self.drr
            self.drr = (self.drr + 1) % self.NDS
            if self.dcnt[i] > 0:
                self._wait(eng, (("d", i), self.dsem[i], self.dcnt[i], eng, True))
        ins = fn(self.E[eng])
        self.nins += 1
        if dma:
            self.dcnt[i] += 16
            ins.then_inc(self.dsem[i], 16)
            ev = (("d", i), self.dsem[i], self.dcnt[i], eng, True)
        else:
            self.ccnt[eng] += 1
            ins.then_inc(self.csem[eng], 1)
            ev = (("c", eng), self.csem[eng], self.ccnt[eng], eng, False)
        for k in w:
            self.lastw[k] = ev
            self.readers[k] = {}
        for k in r:
            self.readers.setdefault(k, {})[ev[0]] = ev
        return ev

    def pe(self, fn, r=(), w=()):
        return self.op("pe", fn, r, w)

    def act(self, fn, r=(), w=()):
        return self.op("act", fn, r, w)

    def dve(self, fn, r=(), w=()):
        return self.op("dve", fn, r, w)

    def pool(self, fn, r=(), w=()):
        return self.op("pool", fn, r, w)

    def dma(self, out, in_, r=(), w=(), q="sp"):
        return self.op(q, lambda e: e.dma_start(out=out, in_=in_), r, w, dma=True)

    def barrier(self):
        evs = []
        for e in self.csem:
            if self.ccnt[e] > 0:
                evs.append((("c", e), self.csem[e], self.ccnt[e], e, False))
        for i in range(self.NDS):
            if self.dcnt[i] > 0:
                evs.append((("d", i), self.dsem[i], self.dcnt[i], "sp", True))
        for eng in self.E:
            for ev in evs:
                if ev[3] == "pe" and eng == "pe" and not ev[4]:
                    continue
                if self.known[eng].get(ev[0], 0) >= ev[2]:
                    continue
                self.E[eng].wait_ge(ev[1], ev[2])
                self.known[eng][ev[0]] = ev[2]
        self.lastw = {}
        self.readers = {}

    def finish(self, keys):
        for k in keys:
            if k in self.lastw:
                self._wait("sp", self.lastw[k])


class Prog:
    def __init__(self):
        self.nc = bass.Bass("TRN2", target_bir_lowering=False)
        self.es = ExitStack()
        self.S = Sched(self.nc, self.es)
        self.ins = {}
        self.outs = {}
        nc = self.nc
        self.ps = [self.es.enter_context(nc.psum_tensor("psf%d" % i, [128, 512], F32)) for i in range(6)]
        self.psb = [self.es.enter_context(nc.psum_tensor("psb%d" % i, [128, 1024], BF16)) for i in range(2)]
        self.uid = 0

    def inp(self, name, shape, dt=F32):
        t = self.nc.dram_tensor(name, list(shape), dt, kind="ExternalInput").ap()
        self.ins[name] = t
        return t

    def out(self, name, shape, dt=F32):
        t = self.nc.dram_tensor(name, list(shape), dt, kind="ExternalOutput").ap()
        self.outs[name] = t
        return t

    def scratch(self, name, shape, dt=F32):
        return self.nc.dram_tensor(name, list(shape), dt).ap()

    def sb(self, es, name, shape, dt=F32):
        self.uid += 1
        return es.enter_context(self.nc.sbuf_tensor("%s_%d" % (name, self.uid), list(shape), dt))

    def close(self, out_keys):
        self.S.finish(out_keys)
        self.es.close()
        return self.nc


def load_consts(P, es):
    S = P.S
    c = {}
    ident_d = P.inp("c_ident", [128, 128])
    c["ident"] = P.sb(es, "ident", [128, 128], BF16)
    c["ones"] = P.sb(es, "ones", [128, 128], BF16)
    S.dma(c["ident"][:], ident_d, w=["ident"], q="pool")
    S.pool(lambda e: e.memset(c["ones"][:], 1.0), w=["ones"])
    return c


def emit_norm_T(P, c, xsrc, xkey, g_bc, hT, hkey, col0, work):
    S = P.S
    junk, ss, rs, hbf = work["junk"], work["ss"], work["rs"], work["hbf"]
    S.dve(lambda e: e.scalar_tensor_tensor(out=junk[:], in0=xsrc, scalar=1.0, in1=xsrc,
                                           op0=ALU.mult, op1=ALU.mult, accum_out=ss[:]),
          r=[xkey], w=["n_junk", "n_ss"])
    S.act(lambda e: e.activation(out=rs[:], in_=ss[:], func=AF.Sqrt, bias=work["eps"][:], scale=1.0 / D),
          r=["n_ss", "n_eps"], w=["n_rs"])
    S.dve(lambda e: e.reciprocal(out=rs[:], in_=rs[:]), r=["n_rs"], w=["n_rs"])
    S.dve(lambda e: e.scalar_tensor_tensor(out=hbf[:], in0=xsrc, scalar=rs[:], in1=g_bc,
                                           op0=ALU.mult, op1=ALU.mult),
          r=[xkey, "n_rs", "gbc"], w=["n_hbf"])
    for q in range(KC // 8):
        pb = P.psb[q % 2]
        for i in range(8):
            kc = q * 8 + i
            S.pe(lambda e, kc=kc, i=i: e.transpose(out=pb[:, i * 128:(i + 1) * 128],
                                                   in_=hbf[:, kc * 128:(kc + 1) * 128],
                                                   identity=c["ident"][:]),
                 r=["n_hbf", "ident"], w=[("psb", q % 2)])
        dst = hT[:, q * 8:(q + 1) * 8, col0:col0 + 128]
        src = pb[:].rearrange("p (a b) -> p a b", b=128)
        if q % 2 == 0:
            S.act(lambda e, dst=dst, src=src: e.copy(out=dst, in_=src), r=[("psb", q % 2)], w=[hkey])
        else:
            S.dve(lambda e, dst=dst, src=src: e.tensor_copy(out=dst, in_=src), r=[("psb", q % 2)], w=[hkey])


def norm_work(P, es):
    w = {
        "junk": P.sb(es, "n_junk", [128, D], BF16),
        "ss": P.sb(es, "n_ss", [128, 1], F32),
        "rs": P.sb(es, "n_rs", [128, 1], F32),
        "hbf": P.sb(es, "n_hbf", [128, D], BF16),
        "eps": P.sb(es, "n_eps", [128, 1], F32),
    }
    P.S.pool(lambda e: e.memset(w["eps"][:], EPS), w=["n_eps"])
    return w


class WStream:
    def __init__(self, P, es, name, shape, nbuf=4):
        self.P = P
        self.name = name
        self.bufs = [P.sb(es, name, shape, BF16) for _ in range(nbuf)]
        self.nbuf = nbuf
        self.pieces = []
        self.issued = 0
        self.used = 0

    def plan(self, pieces):
        self.pieces = list(pieces)
        self.issued = 0
        self.used = 0

    def _issue(self):
        i = self.issued
        b = i % self.nbuf
        dst, src = self.pieces[i](self.bufs[b])
        self.P.S.dma(dst, src, w=[(self.name, b)], q="pool")
        self.issued += 1

    def next(self):
        while self.issued < len(self.pieces) and self.issued < self.used + self.nbuf - 1:
            self._issue()
        if self.issued <= self.used:
            self._issue()
        b = self.used % self.nbuf
        self.used += 1
        return self.bufs[b], (self.name, b)


def emit_mlp(P, c, es_outer, x_res, g_bc_dram, w_up, w_down):
    S = P.S
    with ExitStack() as es:
        gbc = P.sb(es, "gbc", [128, D], F32)
        S.dma(gbc[:], g_bc_dram, w=["gbc"])
        wk = norm_work(P, es)
        hT = P.sb(es, "hT", [128, KC, 512], BF16)
        actT = P.sb(es, "actT", [128, DFF // 128, 512], BF16)
        rl = [P.sb(es, "rl", [128, 512], F32) for _ in range(2)]
        ws = WStream(P, es, "wmlp", [128, 4096], nbuf=4)
        for half in range(2):
            for t in range(4):
                j = half * 4 + t
                emit_norm_T(P, c, x_res[:, j, :], ("x", j), gbc[:], hT, "hT", t * 128, wk)
            npc = DFF // 256

            def up_piece(i):
                def f(buf):
                    dst = buf[:].rearrange("p (k n) -> p k n", n=256)
                    src = w_up[:, i * 256:(i + 1) * 256].rearrange("(k p) n -> p k n", p=128)
                    return dst, src
                return f
            ws.plan([up_piece(i) for i in range(npc)])
            for i in range(npc):
                buf, bkey = ws.next()
                wv = buf[:].rearrange("p (k n) -> p k n", n=256)
                for s in range(2):
                    oc = i * 2 + s
                    pb = P.ps[oc % 4]
                    for kc in range(KC):
                        S.pe(lambda e, kc=kc, s=s, pb=pb, wv=wv: e.matmul(
                            pb[:], lhsT=wv[:, kc, s * 128:(s + 1) * 128], rhs=hT[:, kc, :],
                            start=(kc == 0), stop=(kc == KC - 1)),
                            r=[bkey, "hT"], w=[("ps", oc % 4)])
                    r_ = rl[oc % 2]
                    S.act(lambda e, pb=pb, r_=r_: e.activation(out=r_[:], in_=pb[:], func=AF.Relu),
                          r=[("ps", oc % 4)], w=[("rl", oc % 2)])
                    S.pool(lambda e, oc=oc, r_=r_: e.tensor_tensor(out=actT[:, oc, :], in0=r_[:], in1=r_[:],
                                                                  op=ALU.mult),
                           r=[("rl", oc % 2)], w=["actT"])
            nkp = (DFF // 128) // 8

            def dn_piece(cc, kp):
                def f(buf):
                    dst = buf[:].rearrange("p (k n) -> p k n", n=512)
                    src = w_down[kp * 1024:(kp + 1) * 1024, cc * 512:(cc + 1) * 512].rearrange(
                        "(k p) n -> p k n", p=128)
                    return dst, src
                return f
            ws.plan([dn_piece(cc, kp) for cc in range(4) for kp in range(nkp)])
            for cc in range(4):
                for kp in range(nkp):
                    buf, bkey = ws.next()
                    wv = buf[:].rearrange("p (k n) -> p k n", n=512)
                    for t in range(4):
                        for k in range(8):
                            kc = kp * 8 + k
                            S.pe(lambda e, t=t, k=k, kc=kc, wv=wv: e.matmul(
                                P.ps[t][:], lhsT=actT[:, kc, t * 128:(t + 1) * 128], rhs=wv[:, k, :],
                                start=(kc == 0), stop=(kc == DFF // 128 - 1)),
                                r=[bkey, "actT"], w=[("ps", t)])
                for t in range(4):
                    j = half * 4 + t
                    xs = x_res[:, j, cc * 512:(cc + 1) * 512]
                    S.dve(lambda e, xs=xs, t=t: e.tensor_tensor(out=xs, in0=xs, in1=P.ps[t][:], op=ALU.add),
                          r=[("ps", t), ("x", j)], w=[("x", j)])
        S.barrier()


def build_mlp_only():
    P = Prog()
    S = P.S
    x_in = P.inp("x", [TL, D])
    g = P.inp("g_bc", [128, D])
    w_up = P.inp("w_up", [D, DFF])
    w_down = P.inp("w_down", [DFF, D])
    y = P.out("y", [TL, D])
    with ExitStack() as es:
        c = load_consts(P, es)
        x_res = P.sb(es, "x_res", [128, NT, D], F32)
        for j in range(NT):
            S.dma(x_res[:, j, :], x_in[j * 128:(j + 1) * 128, :], w=[("x", j)])
        emit_mlp(P, c, es, x_res, g, w_up, w_down)
        for j in range(NT):
            S.dma(y[j * 128:(j + 1) * 128, :], x_res[:, j, :], r=[("x", j)], w=[("y", j)])
        nc = P.close([("y", j) for j in range(NT)])
    return nc


def shard_tokens(a):
    b = a.reshape((NT, NCORES, 128) + a.shape[1:])
    return [np.ascontiguousarray(b[:, c].reshape((TL,) + a.shape[1:])) for c in range(NCORES)]


def unshard_tokens(parts):
    a = np.stack([p.reshape((NT, 128) + p.shape[1:]) for p in parts], axis=1)
    return np.ascontiguousarray(a.reshape((T,) + parts[0].shape[1:]))


def bc128(v):
    return np.ascontiguousarray(np.broadcast_to(np.asarray(v, np.float32).reshape(1, -1), (128, v.size)))


IDENT = np.eye(128, dtype=np.float32)


def emit_linear_tm_res(P, ws, AT, atkey, nkc, w_dram, x_res):
    S = P.S
    nkp = nkc // 8
    for half in range(2):
        def piece(cc, kp):
            def f(buf):
                dst = buf[:].rearrange("p (k n) -> p k n", n=512)
                src = w_dram[kp * 1024:(kp + 1) * 1024, cc * 512:(cc + 1) * 512].rearrange(
                    "(k p) n -> p k n", p=128)
                return dst, src
            return f
        ws.plan([piece(cc, kp) for cc in range(4) for kp in range(nkp)])
        for cc in range(4):
            for kp in range(nkp):
                buf, bkey = ws.next()
                wv = buf[:].rearrange("p (k n) -> p k n", n=512)
                for t in range(4):
                    j = half * 4 + t
                    for k in range(8):
                        kc = kp * 8 + k
                        S.pe(lambda e, t=t, j=j, k=k, kc=kc, wv=wv: e.matmul(
                            P.ps[t][:], lhsT=AT[:, kc, j * 128:(j + 1) * 128], rhs=wv[:, k, :],
                            start=(kc == 0), stop=(kc == nkc - 1)),
                            r=[bkey, atkey], w=[("ps", t)])
            for t in range(4):
                j = half * 4 + t
                xs = x_res[:, j, cc * 512:(cc + 1) * 512]
                S.dve(lambda e, xs=xs, t=t: e.tensor_tensor(out=xs, in0=xs, in1=P.ps[t][:], op=ALU.add),
                      r=[("ps", t), ("x", j)], w=[("x", j)])


def load_x(P, x_res, x_in):
    for j in range(NT):
        P.S.dma(x_res[:, j, :], x_in[j * 128:(j + 1) * 128, :], w=[("x", j)])


def store_x(P, y, x_res):
    for j in range(NT):
        P.S.dma(y[j * 128:(j + 1) * 128, :], x_res[:, j, :], r=[("x", j)], w=[("y", j)])
    return [("y", j) for j in range(NT)]


GELU_C = 0.044715
GELU_S = 1.5957691216057308


def build_R1(stage=99):
    P = Prog()
    S = P.S
    x9 = P.inp("x9", [9 * 128, D])
    g = P.inp("g_bc", [128, D])
    w_in = P.inp("w_in", [D, 2 * D])
    cw_d = P.inp("cw", [128, KC * 4])
    vec_d = P.inp("vecs", [128, KC * 4])
    wa_d = P.inp("wa", [8 * 256, 256])
    wx_d = P.inp("wx", [8 * 256, 256])
    gel_o = P.out("gel", [128, KC * TL])
    hl_o = P.out("hloc", [128, KC * TL])
    pp_o = P.out("pp", [128, KC * TL])
    ab_o = P.out("ab", [128, KC * 16])
    with ExitStack() as es:
        c = load_consts(P, es)
        gbc = P.sb(es, "gbc", [128, D], F32)
        S.dma(gbc[:], g, w=["gbc"])
        wk = norm_work(P, es)
        hT = P.sb(es, "hT9", [128, KC, 9 * 128], BF16)
        xt = [P.sb(es, "xt", [128, D], F32) for _ in range(2)]
        for t in range(9):
            S.dma(xt[t % 2][:], x9[t * 128:(t + 1) * 128, :], w=[("xt", t % 2)])
            emit_norm_T(P, c, xt[t % 2][:], ("xt", t % 2), gbc[:], hT, "hT", t * 128, wk)
        cw = P.sb(es, "cw", [128, KC, 4], F32)
        vec = P.sb(es, "vec", [128, KC, 4], F32)
        S.dma(cw[:], cw_d.rearrange("p (k n) -> p k n", n=4), w=["cw"])
        S.dma(vec[:], vec_d.rearrange("p (k n) -> p k n", n=4), w=["vec"])
        wa = P.sb(es, "wa", [128, 8, 2, 256], BF16)
        wx = P.sb(es, "wx", [128, 8, 2, 256], BF16)
        for n in range(8):
            S.dma(wa[:, n], wa_d[n * 256:(n + 1) * 256, :].rearrange("(k p) c -> p k c", p=128), w=["wa"], q="pool")
            S.dma(wx[:, n], wx_d[n * 256:(n + 1) * 256, :].rearrange("(k p) c -> p k c", p=128), w=["wx"], q="pool")
        one = P.sb(es, "one", [128, 1], F32)
        S.pool(lambda e: e.memset(one[:], 1.0), w=["one"])
        cl = P.sb(es, "cl", [128, KC], F32)
        S.act(lambda e: e.activation(out=cl[:], in_=vec[:, :, 3], func=AF.Exp, scale=-1.0), r=["vec"], w=["cl"])
        S.act(lambda e: e.activation(out=cl[:], in_=cl[:], func=AF.Ln, bias=one[:], scale=1.0),
              r=["cl", "one"], w=["cl"])
        S.dve(lambda e: e.tensor_scalar(cl[:], cl[:], -8.0, None, ALU.mult), r=["cl"], w=["cl"])
        if stage == 1:
            return P.close([])
        ws = WStream(P, es, "wrin", [128, 4096], nbuf=3)
        f4 = lambda name: P.sb(es, name, [128, TL], F32)
        gelb = [f4("gelb"), f4("gelb")]
        hlb = [f4("hlb"), f4("hlb")]
        ppb = [f4("ppb"), f4("ppb")]
        rr, ii, aa, a2, bb, ap_, dd = f4("rr"), f4("ii"), f4("aa"), f4("a2"), f4("bb"), f4("ap"), f4("dd")
        t1 = [P.sb(es, "t1", [128, 512], F32) for _ in range(2)]
        xrx = P.sb(es, "xrx", [128, 2, 8, 131], F32)
        xc = P.sb(es, "xc", [128, 2, TL], F32)
        xcb = P.sb(es, "xcb", [128, 2, TL], BF16)
        ab = P.sb(es, "ab", [128, KC, 2, 8], F32)
        S.pool(lambda e: e.memset(dd[:], 0.0), w=["dd"])

        def piece(col0):
            def f(buf):
                dst = buf[:].rearrange("p (k n) -> p k n", n=256)
                src = w_in[:, col0:col0 + 256].rearrange("(k p) n -> p k n", p=128)
                return dst, src
            return f
        pcs = []
        for n in range(8):
            pcs.append(piece(n * 256))
            pcs.append(piece(D + n * 256))
        ws.plan(pcs)
        for n in range(8):
            buf, bkey = ws.next()
            wv = buf[:].rearrange("p (k n) -> p k n", n=256)
            for s in range(2):
                ch = 2 * n + s
                gb = gelb[ch % 2]
                for tc in range(2):
                    pb = P.ps[tc]
                    for kc in range(KC):
                        S.pe(lambda e, kc=kc, s=s, tc=tc, pb=pb, wv=wv: e.matmul(
                            pb[:], lhsT=wv[:, kc, s * 128:(s + 1) * 128], rhs=hT[:, kc, tc * 512:(tc + 1) * 512],
                            start=(kc == 0), stop=(kc == KC - 1)), r=[bkey, "hT"], w=[("ps", tc)])
                    tt = t1[tc]
                    S.act(lambda e, tt=tt, pb=pb: e.activation(out=tt[:], in_=pb[:], func=AF.Square),
                          r=[("ps", tc)], w=[("t1", tc)])
                    S.dve(lambda e, tt=tt: e.tensor_scalar(tt[:], tt[:], GELU_C, 1.0, ALU.mult, ALU.add),
                          r=[("t1", tc)], w=[("t1", tc)])
                    S.dve(lambda e, tt=tt, pb=pb: e.tensor_tensor(out=tt[:], in0=tt[:], in1=pb[:], op=ALU.mult),
                          r=[("t1", tc), ("ps", tc)], w=[("t1", tc)])
                    S.act(lambda e, tt=tt: e.activation(out=tt[:], in_=tt[:], func=AF.Sigmoid, scale=GELU_S),
                          r=[("t1", tc)], w=[("t1", tc)])
                    gs = gb[:, tc * 512:(tc + 1) * 512]
                    S.dve(lambda e, tt=tt, pb=pb, gs=gs: e.tensor_tensor(out=gs, in0=tt[:], in1=pb[:], op=ALU.mult),
                          r=[("t1", tc), ("ps", tc)], w=[("gelb", ch % 2)])
                S.dma(gel_o[:, ch * TL:(ch + 1) * TL], gb[:], r=[("gelb", ch % 2)], w=[("gel_o", ch)])
            if stage == 2:
                return P.close([("gel_o", 0), ("gel_o", 1)])
            buf, bkey = ws.next()
            wv = buf[:].rearrange("p (k n) -> p k n", n=256)
            for s in range(2):
                ch = 2 * n + s
                for tc in range(3):
                    pb = P.ps[2 + tc]
                    rhs_of = (lambda kc, tc=tc: hT[:, kc, tc * 512:(tc + 1) * 512]) if tc < 2 else \
                        (lambda kc: hT[:, kc, 1024:1152])
                    po = pb[:] if tc < 2 else pb[:, 0:128]
                    for kc in range(KC):
                        S.pe(lambda e, kc=kc, s=s, po=po, wv=wv, rhs_of=rhs_of: e.matmul(
                            po, lhsT=wv[:, kc, s * 128:(s + 1) * 128], rhs=rhs_of(kc),
                            start=(kc == 0), stop=(kc == KC - 1)), r=[bkey, "hT"], w=[("ps", 2 + tc)])
                    if tc < 2:
                        S.act(lambda e, s=s, tc=tc, pb=pb: e.copy(
                            out=xrx[:, s, tc * 4:(tc + 1) * 4, 3:131],
                            in_=pb[:].rearrange("p (a b) -> p a b", b=128)),
                            r=[("ps", 2 + tc)], w=["xrx"])
                    else:
                        S.act(lambda e, s=s, pb=pb: e.copy(
                            out=xrx[:, s, :, 0:3],
                            in_=pb[:, 0:128].rearrange("p (a b) -> p a b", b=16)[:, :, 13:16]),
                            r=[("ps", 2 + tc)], w=["xrx"])
                xcv = xc[:, s, :].rearrange("p (a b) -> p a b", b=128)
                S.dve(lambda e, s=s, ch=ch, xcv=xcv: e.tensor_scalar(
                    xcv, xrx[:, s, :, 0:128], cw[:, ch, 0:1], vec[:, ch, 0:1], ALU.mult, ALU.add),
                    r=["xrx", "cw", "vec"], w=["xc"])
                for i in range(1, 4):
                    S.dve(lambda e, s=s, ch=ch, i=i, xcv=xcv: e.scalar_tensor_tensor(
                        out=xcv, in0=xrx[:, s, :, i:i + 128], scalar=cw[:, ch, i:i + 1], in1=xcv,
                        op0=ALU.mult, op1=ALU.add), r=["xrx", "cw", "xc"], w=["xc"])
                S.pool(lambda e, s=s: e.tensor_copy(out=xcb[:, s, :], in_=xc[:, s, :]), r=["xc"], w=["xcb"])
            if stage == 3:
                return P.close([("gel_o", 0), ("gel_o", 1)])
            for s in range(2):
                ch = 2 * n + s
                for (wg, dst, bcol, pbase, nm) in ((wa, rr, 1, 0, "rr"), (wx, ii, 2, 2, "ii")):
                    for tc in range(2):
                        pb = P.ps[pbase + tc]
                        for k in range(2):
                            S.pe(lambda e, k=k, s=s, tc=tc, pb=pb, wg=wg: e.matmul(
                                pb[:], lhsT=wg[:, n, k, s * 128:(s + 1) * 128], rhs=xcb[:, k, tc * 512:(tc + 1) * 512],
                                start=(k == 0), stop=(k == 1)), r=["wa", "wx", "xcb"], w=[("ps", pbase + tc)])
                        S.act(lambda e, tc=tc, pb=pb, dst=dst, bcol=bcol, ch=ch: e.activation(
                            out=dst[:, tc * 512:(tc + 1) * 512], in_=pb[:], func=AF.Sigmoid,
                            bias=vec[:, ch, bcol:bcol + 1], scale=1.0), r=[("ps", pbase + tc), "vec"], w=[nm])
                S.act(lambda e, ch=ch: e.activation(out=aa[:], in_=rr[:], func=AF.Exp, scale=cl[:, ch:ch + 1]),
                      r=["rr", "cl"], w=["aa"])
                S.pool(lambda e: e.tensor_tensor(out=a2[:], in0=aa[:], in1=aa[:], op=ALU.mult), r=["aa"], w=["a2"])
                S.act(lambda e: e.activation(out=a2[:], in_=a2[:], func=AF.Sqrt, bias=one[:], scale=-1.0),
                      r=["a2", "one"], w=["a2"])
                S.dve(lambda e, s=s: e.tensor_tensor(out=bb[:], in0=xc[:, s, :], in1=ii[:], op=ALU.mult),
                      r=["xc", "ii"], w=["bb"])
                S.dve(lambda e: e.tensor_tensor(out=bb[:], in0=bb[:], in1=a2[:], op=ALU.mult),
                      r=["bb", "a2"], w=["bb"])
                a3 = aa[:].rearrange("p (a b) -> p a b", b=128)
                ap3 = ap_[:].rearrange("p (a b) -> p a b", b=128)
                d3 = dd[:].rearrange("p (a b) -> p a b", b=128)
                S.pool(lambda e: e.tensor_copy(out=ap_[:], in_=aa[:]), r=["aa"], w=["ap"])
                S.pool(lambda e, ap3=ap3: e.memset(ap3[:, :, 0:1], 0.0), w=["ap"])
                S.pool(lambda e, d3=d3, a3=a3: e.tensor_copy(out=d3[:, :, 0:1], in_=a3[:, :, 0:1]),
                       r=["aa"], w=["dd"])
                hb, pb_ = hlb[ch % 2], ppb[ch % 2]
                S.dve(lambda e, hb=hb: e.tensor_tensor_scan(out=hb[:], data0=ap_[:], data1=bb[:], initial=0.0,
                                                            op0=ALU.mult, op1=ALU.add),
                      r=["ap", "bb"], w=[("hlb", ch % 2)])
                S.dve(lambda e, pb_=pb_: e.tensor_tensor_scan(out=pb_[:], data0=ap_[:], data1=dd[:], initial=0.0,
                                                              op0=ALU.mult, op1=ALU.add),
                      r=["ap", "dd"], w=[("ppb", ch % 2)])
                h3 = hb[:].rearrange("p (a b) -> p a b", b=128)
                p3 = pb_[:].rearrange("p (a b) -> p a b", b=128)
                S.pool(lambda e, ch=ch, p3=p3: e.tensor_copy(out=ab[:, ch, 0, :], in_=p3[:, :, 127]),
                       r=[("ppb", ch % 2)], w=["ab"])
                S.pool(lambda e, ch=ch, h3=h3: e.tensor_copy(out=ab[:, ch, 1, :], in_=h3[:, :, 127]),
                       r=[("hlb", ch % 2)], w=["ab"])
                S.dma(hl_o[:, ch * TL:(ch + 1) * TL], hb[:], r=[("hlb", ch % 2)], w=[("hl_o", ch)])
                S.dma(pp_o[:, ch * TL:(ch + 1) * TL], pb_[:], r=[("ppb", ch % 2)], w=[("pp_o", ch)])
        S.dma(ab_o, ab[:].rearrange("p a b c -> p (a b c)"), r=["ab"], w=["ab_o"])
        keys = ["ab_o"] + [(k, ch) for k in ("gel_o", "hl_o", "pp_o") for ch in range(KC)]
        nc = P.close(keys)
    return nc


def build_R2(final=False):
    P = Prog()
    S = P.S
    x_in = P.inp("x", [TL, D])
    gel_d = P.inp("gel", [128, KC * TL])
    hl_d = P.inp("hloc", [128, KC * TL])
    pp_d = P.inp("pp", [128, KC * TL])
    abg_d = P.inp("abg", [128, KC * 2 * 64])
    sel_d = P.inp("selb", [128, 8 * 64])
    w_out = P.inp("w_out", [D, D])
    g2 = P.inp("g_bc", [128, D])
    w_up = P.inp("w_up", [D, DFF])
    w_down = P.inp("w_down", [DFF, D])
    y = P.out("y", [TL, D])
    with ExitStack() as es:
        c = load_consts(P, es)
        x_res = P.sb(es, "x_res", [128, NT, D], F32)
        load_x(P, x_res, x_in)
        with ExitStack() as es2:
            yT = P.sb(es2, "yT", [128, KC, TL], BF16)
            abg = P.sb(es2, "abg", [128, KC, 2, 64], F32)
            sel = P.sb(es2, "sel", [128, 8, 64], F32)
            hs = P.sb(es2, "hs", [128, KC, 64], F32)
            carry = P.sb(es2, "carry", [128, KC, 8], F32)
            junk = P.sb(es2, "junk", [128, 64], F32)
            S.dma(abg[:], abg_d.rearrange("p (a b c) -> p a b c", b=2, c=64), w=["abg"])
            S.dma(sel[:], sel_d.rearrange("p (a b) -> p a b", b=64), w=["sel"])
            for ch in range(KC):
                S.dve(lambda e, ch=ch: e.tensor_tensor_scan(out=hs[:, ch, :], data0=abg[:, ch, 0, :],
                                                            data1=abg[:, ch, 1, :], initial=0.0,
                                                            op0=ALU.mult, op1=ALU.add),
                      r=["abg"], w=["hs"])
            for ch in range(KC):
                for j in range(NT):
                    S.dve(lambda e, ch=ch, j=j: e.scalar_tensor_tensor(
                        out=junk[:], in0=hs[:, ch, :], scalar=1.0, in1=sel[:, j, :], op0=ALU.mult, op1=ALU.mult,
                        accum_out=carry[:, ch, j:j + 1]), r=["hs", "sel"], w=["junk", "carry"])
            f4 = lambda name: P.sb(es2, name, [128, TL], F32)
            gb = [f4("gb"), f4("gb")]
            hb = [f4("hb"), f4("hb")]
            pb = [f4("pb"), f4("pb")]
            for ch in range(KC):
                q = ch % 2
                S.dma(gb[q][:], gel_d[:, ch * TL:(ch + 1) * TL], w=[("gb", q)])
                S.dma(hb[q][:], hl_d[:, ch * TL:(ch + 1) * TL], w=[("hb", q)])
                S.dma(pb[q][:], pp_d[:, ch * TL:(ch + 1) * TL], w=[("pb", q)])
                for j in range(NT):
                    sl = slice(j * 128, (j + 1) * 128)
                    S.dve(lambda e, q=q, ch=ch, j=j, sl=sl: e.scalar_tensor_tensor(
                        out=hb[q][:, sl], in0=pb[q][:, sl], scalar=carry[:, ch, j:j + 1], in1=hb[q][:, sl],
                        op0=ALU.mult, op1=ALU.add), r=[("pb", q), ("hb", q), "carry"], w=[("hb", q)])
                S.pool(lambda e, q=q, ch=ch: e.tensor_tensor(out=yT[:, ch, :], in0=hb[q][:], in1=gb[q][:],
                                                             op=ALU.mult),
                       r=[("hb", q), ("gb", q)], w=["yT"])
            ws = WStream(P, es2, "wout", [128, 4096], nbuf=3)
            emit_linear_tm_res(P, ws, yT, "yT", KC, w_out, x_res)
            S.barrier()
        emit_mlp(P, c, es, x_res, g2, w_up, w_down)
        keys = store_x(P, y, x_res)
        nc = P.close(keys)
    return nc


def halo_tile(xg, c):
    out = np.zeros((128, xg.shape[1]), np.float32)
    for j in range(NT):
        t0 = (8 * j + c) * 128 - 16
        if t0 >= 0:
            out[j * 16:(j + 1) * 16] = xg[t0:t0 + 16]
    return out


def pk(v):
    return np.ascontiguousarray(np.asarray(v, np.float32).reshape(KC, 128).T)


def r1_inputs(xg, d, j):
    xs = shard_tokens(xg)
    cw = np.stack([pk(d["rnn_conv_w"][j][i]) for i in range(4)], -1).reshape(128, KC * 4)
    vecs = np.stack([pk(d["rnn_conv_b"][j]), pk(d["rnn_gate_a_b"][j]), pk(d["rnn_gate_x_b"][j]),
                     pk(d["rnn_lambda"][j])], -1).reshape(128, KC * 4)
    common = {
        "g_bc": bc128(d["rnn_norm"][j]), "w_in": np.ascontiguousarray(d["rnn_w_in"][j]),
        "cw": np.ascontiguousarray(cw), "vecs": np.ascontiguousarray(vecs),
        "wa": np.ascontiguousarray(d["rnn_gate_a_w"][j].reshape(8 * 256, 256)),
        "wx": np.ascontiguousarray(d["rnn_gate_x_w"][j].reshape(8 * 256, 256)),
        "c_ident": IDENT,
    }
    return [dict(common, x9=np.concatenate([xs[c], halo_tile(xg, c)], 0)) for c in range(NCORES)]


def r2_inputs(xg, r1, d, j, li):
    xs = shard_tokens(xg)
    ab = np.stack([r1[c]["ab"].reshape(128, KC, 2, NT) for c in range(NCORES)], -1)
    abg = np.ascontiguousarray(ab.reshape(128, KC * 2 * 64))
    common = {
        "abg": abg, "w_out": np.ascontiguousarray(d["rnn_w_out"][j]), "g_bc": bc128(d["mlp_norm"][li]),
        "w_up": np.ascontiguousarray(d["mlp_w_up"][li]), "w_down": np.ascontiguousarray(d["mlp_w_down"][li]),
        "c_ident": IDENT,
    }
    ins = []
    for c in range(NCORES):
        sel = np.zeros((NT, 64), np.float32)
        for jj in range(NT):
            b = 8 * jj + c - 1
            if b >= 0:
                sel[jj, b] = 1.0
        selb = np.ascontiguousarray(np.broadcast_to(sel.reshape(1, NT * 64), (128, NT * 64)))
        ins.append(dict(common, x=xs[c], gel=r1[c]["gel"], hloc=r1[c]["hloc"], pp=r1[c]["pp"], selb=selb))
    return ins


TWO_PI = 6.283185307179586
CW1 = 6.28125
CW2 = TWO_PI - CW1
PI = 3.141592653589793


def emit_rope_tables(P, es, posb_d, invf_d):
    S = P.S
    posi = P.sb(es, "posi", [128, TL], I32)
    posf = P.sb(es, "posf", [128, TL], F32)
    invf = P.sb(es, "invf", [128, 2], F32)
    ang = P.sb(es, "ang", [128, TL], F32)
    kf = P.sb(es, "kf", [128, TL], F32)
    ki = P.sb(es, "ki", [128, TL], I32)
    mm = P.sb(es, "mm", [128, TL], F32)
    cosT = P.sb(es, "cosT", [128, 2, TL], F32)
    sinT = P.sb(es, "sinT", [128, 2, TL], F32)
    S.dma(posi[:], posb_d, w=["posi"])
    S.dma(invf[:], invf_d, w=["invf"])
    S.dve(lambda e: e.tensor_copy(out=posf[:], in_=posi[:]), r=["posi"], w=["posf"])

    def wrap(buf, key):
        S.dve(lambda e: e.tensor_single_scalar(out=mm[:], in_=buf, scalar=PI, op=ALU.is_gt), r=[key], w=["mm"])
        S.dve(lambda e: e.scalar_tensor_tensor(out=buf, in0=mm[:], scalar=-TWO_PI, in1=buf, op0=ALU.mult, op1=ALU.add),
              r=["mm", key], w=[key])
        S.dve(lambda e: e.tensor_single_scalar(out=mm[:], in_=buf, scalar=-PI, op=ALU.is_lt), r=[key], w=["mm"])
        S.dve(lambda e: e.scalar_tensor_tensor(out=buf, in0=mm[:], scalar=TWO_PI, in1=buf, op0=ALU.mult, op1=ALU.add),
              r=["mm", key], w=[key])

    for t in range(2):
        S.dve(lambda e, t=t: e.tensor_scalar(ang[:], posf[:], invf[:, t:t + 1], None, ALU.mult),
              r=["posf", "invf"], w=["ang"])
        S.dve(lambda e: e.tensor_scalar(kf[:], ang[:], 1.0 / TWO_PI, None, ALU.mult), r=["ang"], w=["kf"])
        S.dve(lambda e: e.tensor_copy(out=ki[:], in_=kf[:]), r=["kf"], w=["ki"])
        S.dve(lambda e: e.tensor_copy(out=kf[:], in_=ki[:]), r=["ki"], w=["kf"])
        S.dve(lambda e: e.scalar_tensor_tensor(out=ang[:], in0=kf[:], scalar=-CW1, in1=ang[:], op0=ALU.mult, op1=ALU.add),
              r=["kf", "ang"], w=["ang"])
        S.dve(lambda e: e.scalar_tensor_tensor(out=ang[:], in0=kf[:], scalar=-CW2, in1=ang[:], op0=ALU.mult, op1=ALU.add),
              r=["kf", "ang"], w=["ang"])
        wrap(ang[:], "ang")
        S.act(lambda e, t=t: e.activation(out=sinT[:, t, :], in_=ang[:], func=AF.Sin), r=["ang"], w=["sinT"])
        S.dve(lambda e: e.tensor_scalar(ang[:], ang[:], PI / 2, None, ALU.add), r=["ang"], w=["ang"])
        wrap(ang[:], "ang")
        S.act(lambda e, t=t: e.activation(out=cosT[:, t, :], in_=ang[:], func=AF.Sin), r=["ang"], w=["cosT"])
    return cosT, sinT


def emit_attn_inproj(P, c, es, hT, w_in, d_in, d_out):
    S = P.S
    cosT, sinT = emit_rope_tables(P, es, d_in["posb"], d_in["invf"])
    rt = P.sb(es, "rt", [128, 2, 128], BF16)
    S.dma(rt[:, 0, :], d_in["rt"][0:128, :], w=["rt"], q="pool")
    S.dma(rt[:, 1, :], d_in["rt"][128:256, :], w=["rt"], q="pool")
    qkg = P.sb(es, "qkg", [128, 2], F32)
    S.dma(qkg[:], d_in["qkg"], w=["qkg"])
    epsh = P.sb(es, "epsh", [128, 1], F32)
    S.pool(lambda e: e.memset(epsh[:], EPS), w=["epsh"])
    sqb = P.sb(es, "sqb", [128, 512], BF16)
    rsb = P.sb(es, "rsb", [128, 512], F32)
    xnb = P.sb(es, "xnb", [128, 512], BF16)
    t1 = P.sb(es, "t1", [128, 512], F32)
    t2 = P.sb(es, "t2", [128, 512], F32)
    ob = [P.sb(es, "ob", [128, 512], BF16) for _ in range(2)]
    ws = WStream(P, es, "wain", [128, 4096], nbuf=3)

    def head_chunk(wv, bkey, s, kind, dst_of_tc):
        tab = 0 if kind in ("q", "k") else 1
        for tc in range(2):
            pb = P.ps[tc]
            for kc in range(KC):
                S.pe(lambda e, kc=kc, pb=pb: e.matmul(
                    pb[:], lhsT=wv[:, kc, s * 128:(s + 1) * 128], rhs=hT[:, kc, tc * 512:(tc + 1) * 512],
                    start=(kc == 0), stop=(kc == KC - 1)), r=[bkey, "hT"], w=[("ps", tc)])
            tsl = slice(tc * 512, (tc + 1) * 512)
            if kind in ("q", "k"):
                gcol = 0 if kind == "q" else 1
                S.act(lambda e, pb=pb: e.activation(out=sqb[:], in_=pb[:], func=AF.Square), r=[("ps", tc)], w=["sqb"])
                S.pe(lambda e: e.matmul(P.ps[2][:], lhsT=c["ones"][:], rhs=sqb[:], start=True, stop=True),
                     r=["ones", "sqb"], w=[("ps", 2)])
                S.act(lambda e: e.activation(out=rsb[:], in_=P.ps[2][:], func=AF.Sqrt, bias=epsh[:], scale=1.0 / 128),
                      r=[("ps", 2), "epsh"], w=["rsb"])
                S.dve(lambda e: e.reciprocal(out=rsb[:], in_=rsb[:]), r=["rsb"], w=["rsb"])
                S.dve(lambda e, pb=pb, gcol=gcol: e.scalar_tensor_tensor(
                    out=xnb[:], in0=pb[:], scalar=qkg[:, gcol:gcol + 1], in1=rsb[:], op0=ALU.mult, op1=ALU.mult),
                    r=[("ps", tc), "qkg", "rsb"], w=["xnb"])
            else:
                S.act(lambda e, pb=pb: e.copy(out=xnb[:], in_=pb[:]), r=[("ps", tc)], w=["xnb"])
            S.pe(lambda e, tab=tab: e.matmul(P.ps[3][:], lhsT=rt[:, tab, :], rhs=xnb[:], start=True, stop=True),
                 r=["rt", "xnb"], w=[("ps", 3)])
            S.dve(lambda e, tab=tab, tsl=tsl: e.tensor_tensor(out=t1[:], in0=xnb[:], in1=cosT[:, tab, tsl], op=ALU.mult),
                  r=["xnb", "cosT"], w=["t1"])
            S.dve(lambda e, tab=tab, tsl=tsl: e.tensor_tensor(out=t2[:], in0=P.ps[3][:], in1=sinT[:, tab, tsl], op=ALU.mult),
                  r=[("ps", 3), "sinT"], w=["t2"])
            o = ob[tc]
            S.pool(lambda e, o=o: e.tensor_tensor(out=o[:], in0=t1[:], in1=t2[:], op=ALU.add),
                   r=["t1", "t2"], w=[("ob", tc)])
            S.dma(dst_of_tc(tc), o[:], r=[("ob", tc)], w=[("hp_out", P.uid)])
            P.uid += 1

    def piece(col0, ncols):
        def f(buf):
            dst = buf[:, 0:KC * ncols].rearrange("p (k n) -> p k n", n=ncols)
            src = w_in[:, col0:col0 + ncols].rearrange("(k p) n -> p k n", p=128)
            return dst, src
        return f

    groups = [("q", 0, 16, d_out["qT"]), ("k", 2048, 4, d_out["kT"]), ("iq", 3072, 16, d_out["iqT"])]
    pcs = []
    for kind, col0, nch, _ in groups:
        for i in range(nch // 2):
            pcs.append(piece(col0 + i * 256, 256))
    pcs.append(piece(5120, 144))
    ws.plan(pcs)
    for kind, col0, nch, dst in groups:
        for i in range(nch // 2):
            buf, bkey = ws.next()
            wv = buf[:].rearrange("p (k n) -> p k n", n=256)
            for s in range(2):
                hh = 2 * i + s
                head_chunk(wv, bkey, s, kind,
                           lambda tc, hh=hh, dst=dst: dst[:, hh * TL + tc * 512: hh * TL + (tc + 1) * 512])
    buf, bkey = ws.next()
    wv = buf[:, 0:KC * 144].rearrange("p (k n) -> p k n", n=144)
    head_chunk(wv, bkey, 0, "ik", lambda tc: d_out["ikT"][:, tc * 512:(tc + 1) * 512])
    iwsb = P.sb(es, "iwsb", [128, NT, 16], F32)
    for t in range(NT):
        for kc in range(KC):
            S.pe(lambda e, kc=kc, t=t: e.matmul(P.ps[4][:, t * 16:(t + 1) * 16], lhsT=hT[:, kc, t * 128:(t + 1) * 128],
                                                rhs=wv[:, kc, 128:144], start=(kc == 0), stop=(kc == KC - 1)),
                 r=[bkey, "hT"], w=[("ps", 4)])
    S.act(lambda e: e.copy(out=iwsb[:], in_=P.ps[4][:, 0:NT * 16].rearrange("p (a b) -> p a b", b=16)),
          r=[("ps", 4)], w=["iwsb"])
    S.dma(d_out["iw"], iwsb[:].rearrange("p a b -> p (a b)"), r=["iwsb"], w=["iw_out"])
    vsb = [P.sb(es, "vsb", [128, 512], BF16) for _ in range(2)]

    def vpiece(kp):
        def f(buf):
            dst = buf[:].rearrange("p (k n) -> p k n", n=512)
            src = w_in[kp * 1024:(kp + 1) * 1024, 2560:3072].rearrange("(k p) n -> p k n", p=128)
            return dst, src
        return f
    for half in range(2):
        ws.plan([vpiece(0), vpiece(1)])
        for kp in range(2):
            buf, bkey = ws.next()
            wv = buf[:].rearrange("p (k n) -> p k n", n=512)
            for t in range(4):
                j = half * 4 + t
                for k in range(8):
                    kc = kp * 8 + k
                    S.pe(lambda e, t=t, j=j, k=k, kc=kc, wv=wv: e.matmul(
                        P.ps[t][:], lhsT=hT[:, kc, j * 128:(j + 1) * 128], rhs=wv[:, k, :],
                        start=(kc == 0), stop=(kc == KC - 1)), r=[bkey, "hT"], w=[("ps", t)])
        for t in range(4):
            j = half * 4 + t
            S.act(lambda e, t=t: e.copy(out=vsb[t % 2][:], in_=P.ps[t][:]), r=[("ps", t)], w=[("vsb", t % 2)])
            S.dma(d_out["v"][j * 128:(j + 1) * 128, :], vsb[t % 2][:], r=[("vsb", t % 2)], w=[("v_out", j)])
    keys = ["iw_out"] + [("v_out", j) for j in range(NT)]
    return keys


def attn_a1_io(P):
    d_in = {"posb": P.inp("posb", [128, TL], I32), "invf": P.inp("invf", [128, 2]),
            "rt": P.inp("rt", [256, 128]), "qkg": P.inp("qkg", [128, 2])}
    d_out = {"qT": P.out("qT", [128, 16 * TL], BF16), "kT": P.out("kT", [128, 4 * TL], BF16),
             "iqT": P.out("iqT", [128, 16 * TL], BF16), "ikT": P.out("ikT", [128, TL], BF16),
             "iw": P.out("iw", [128, NT * 16]), "v": P.out("v", [TL, 512], BF16)}
    return d_in, d_out


def build_A1():
    P = Prog()
    S = P.S
    x_in = P.inp("x", [TL, D])
    g = P.inp("g_bc", [128, D])
    w_in = P.inp("w_in", [D, A_IN])
    d_in, d_out = attn_a1_io(P)
    with ExitStack() as es:
        c = load_consts(P, es)
        gbc = P.sb(es, "gbc", [128, D], F32)
        S.dma(gbc[:], g, w=["gbc"])
        wk = norm_work(P, es)
        hT = P.sb(es, "hT", [128, KC, TL], BF16)
        xt = [P.sb(es, "xt", [128, D], F32) for _ in range(2)]
        for t in range(NT):
            S.dma(xt[t % 2][:], x_in[t * 128:(t + 1) * 128, :], w=[("xt", t % 2)])
            emit_norm_T(P, c, xt[t % 2][:], ("xt", t % 2), gbc[:], hT, "hT", t * 128, wk)
        keys = emit_attn_inproj(P, c, es, hT, w_in, d_in, d_out)
        S.barrier()
        nc = P.close(keys)
    return nc


def rope_consts():
    inv_h = (1.0 / (10000.0 ** (np.arange(0, 128, 2, dtype=np.float32) / np.float32(128)))).astype(np.float32)
    inv_i = (1.0 / (10000.0 ** (np.arange(0, 64, 2, dtype=np.float32) / np.float32(64)))).astype(np.float32)
    invf = np.zeros((128, 2), np.float32)
    invf[:, 0] = np.concatenate([inv_h, inv_h])
    invf[:64, 1] = np.concatenate([inv_i, inv_i])
    R = np.zeros((128, 128), np.float32)
    for i in range(64):
        R[i, i + 64] = -1.0
        R[i + 64, i] = 1.0
    R2 = np.zeros((128, 128), np.float32)
    for i in range(32):
        R2[i, i + 32] = -1.0
        R2[i + 32, i] = 1.0
    rt = np.concatenate([R.T, R2.T], 0)
    return invf, np.ascontiguousarray(rt)


def a1_inputs(xs, pos, d, j, with_x=True):
    invf, rt = rope_consts()
    ps = shard_tokens(np.asarray(pos).reshape(T))
    qkg = np.ascontiguousarray(np.stack([d["attn_q_norm"][j], d["attn_k_norm"][j]], -1).astype(np.float32))
    ins = []
    for c in range(NCORES):
        m = {"posb": np.ascontiguousarray(np.broadcast_to(ps[c].astype(np.int32).reshape(1, TL), (128, TL))),
             "invf": invf, "rt": rt, "qkg": qkg, "w_in": np.ascontiguousarray(d["attn_w_in"][j]), "c_ident": IDENT}
        if with_x:
            m["x"] = xs[c]
            m["g_bc"] = bc128(d["attn_norm"][j])
        ins.append(m)
    return ins


def emit_attention(P, c, es, d, oT_s):
    S = P.S
    ikT = P.sb(es, "ikT", [128, T], BF16)
    for q in range(4):
        S.dma(ikT[:, q * 2048:(q + 1) * 2048], d["ikTa"][:, q * 2048:(q + 1) * 2048], w=["ikT"])
    iwsb = P.sb(es, "iwsb", [128, NT, 16], F32)
    S.dma(iwsb[:], d["iw"].rearrange("p (a b) -> p a b", b=16), w=["iwsb"])
    cm = P.sb(es, "cm", [128, 1024], F32)
    pen = P.sb(es, "pen", [128, 1024], F32)
    S.dma(cm[:], d["cm"], w=["cm"])
    S.dma(pen[:], d["pen"], w=["pen"])
    score = P.sb(es, "score", [128, T], F32)
    junk = P.sb(es, "junkb", [128, T], BF16)
    maskT = P.sb(es, "maskT", [128, T // 128, 128], BF16)
    mkb = [P.sb(es, "mkb", [128, 512], BF16) for _ in range(2)]
    rl = [P.sb(es, "rl", [128, 512], F32) for _ in range(2)]
    iqtb = [P.sb(es, "iqt", [128, 16, 128], BF16) for _ in range(2)]
    qt = [P.sb(es, "qt", [128, 4, 128], BF16) for _ in range(2)]
    kTb = [P.sb(es, "kTg", [128, T], BF16) for _ in range(2)]
    vgb = [P.sb(es, "vg", [128, T // 128, 128], BF16) for _ in range(2)]
    ptb = [P.sb(es, "ptb", [128, 512], BF16) for _ in range(3)]
    rden = P.sb(es, "rden", [128, 512], F32)
    ot = [P.sb(es, "ot", [128, 512], F32) for _ in range(2)]
    sm = {n: P.sb(es, n, [128, 1], F32) for n in ("M", "lo", "hi", "mid", "cnt", "pred", "dl")}
    iq3 = d["iqT"].rearrange("p (h t) -> p h t", t=TL)
    q3 = d["qT"].rearrange("p (h t) -> p h t", t=TL)
    o3 = oT_s.rearrange("p (h t) -> p h t", t=TL)
    SC = 128.0 ** -0.5
    okeys = []

    def gen_indexer(j):
        Kc = 1024 * (j + 1)
        iqt = iqtb[j % 2]
        S.dma(iqt[:], iq3[:, :, j * 128:(j + 1) * 128], w=[("iqt", j % 2)])
        for k5 in range(Kc // 512):
            sc = score[:, k5 * 512:(k5 + 1) * 512]
            for h in range(16):
                pb = P.ps[h % 2]
                S.pe(lambda e, h=h, pb=pb, k5=k5: e.matmul(pb[:], lhsT=iqt[:, h, :], rhs=ikT[:, k5 * 512:(k5 + 1) * 512],
                                                         start=True, stop=True), r=[("iqt", j % 2), "ikT"], w=[("ps", h % 2)])
                r_ = rl[h % 2]
                S.act(lambda e, pb=pb, r_=r_: e.activation(out=r_[:], in_=pb[:], func=AF.Relu),
                      r=[("ps", h % 2)], w=[("rl", h % 2)])
                if h == 0:
                    S.dve(lambda e, r_=r_, sc=sc: e.tensor_scalar(sc, r_[:], iwsb[:, j, 0:1], None, ALU.mult),
                          r=[("rl", h % 2), "iwsb"], w=["score"])
                else:
                    S.dve(lambda e, r_=r_, sc=sc, h=h: e.scalar_tensor_tensor(
                        out=sc, in0=r_[:], scalar=iwsb[:, j, h:h + 1], in1=sc, op0=ALU.mult, op1=ALU.add),
                        r=[("rl", h % 2), "iwsb", "score"], w=["score"])
                yield

    def post_indexer(j):
        Kc = 1024 * (j + 1)
        S.dve(lambda e: e.tensor_reduce(out=sm["M"][:], in_=score[:, 0:Kc], axis=AX.X, op=ALU.max), r=["score"], w=["M"])
        S.dve(lambda e: e.tensor_reduce(out=sm["cnt"][:], in_=score[:, 0:Kc], axis=AX.X, op=ALU.min), r=["score"], w=["cnt"])
        S.dve(lambda e: e.tensor_tensor(out=sm["dl"][:], in0=sm["M"][:], in1=sm["cnt"][:], op=ALU.subtract),
              r=["M", "cnt"], w=["dl"])
        win = score[:, Kc - 1024:Kc]
        S.dve(lambda e: e.tensor_tensor(out=win, in0=win, in1=cm[:], op=ALU.mult), r=["score", "cm"], w=["score"])
        S.dve(lambda e: e.tensor_tensor(out=win, in0=win, in1=pen[:], op=ALU.add), r=["score", "pen"], w=["score"])
        S.dve(lambda e: e.scalar_tensor_tensor(out=sm["lo"][:], in0=sm["dl"][:], scalar=-0.001, in1=sm["cnt"][:],
                                               op0=ALU.mult, op1=ALU.add), r=["dl", "cnt"], w=["lo"])
        S.dve(lambda e: e.tensor_scalar(sm["lo"][:], sm["lo"][:], -1e-6, None, ALU.add), r=["lo"], w=["lo"])
        S.dve(lambda e: e.tensor_scalar(sm["hi"][:], sm["dl"][:], 1.002, 2e-6, ALU.mult, ALU.add), r=["dl"], w=["hi"])
        for it in range(NBIS):
            S.dve(lambda e: e.tensor_scalar(sm["hi"][:], sm["hi"][:], 0.5, None, ALU.mult), r=["hi"], w=["hi"])
            S.dve(lambda e: e.tensor_tensor(out=sm["mid"][:], in0=sm["lo"][:], in1=sm["hi"][:], op=ALU.add),
                  r=["lo", "hi"], w=["mid"])
            S.dve(lambda e: e.tensor_scalar(junk[:, 0:Kc], score[:, 0:Kc], sm["mid"][:], 0.0, ALU.is_ge, ALU.add,
                                            accum_out=sm["cnt"][:]), r=["score", "mid"], w=["junk", "cnt"])
            S.dve(lambda e: e.tensor_scalar(sm["pred"][:], sm["cnt"][:], float(TOPK), None, ALU.is_ge),
                  r=["cnt"], w=["pred"])
            S.dve(lambda e: e.scalar_tensor_tensor(out=sm["lo"][:], in0=sm["hi"][:], scalar=sm["pred"][:], in1=sm["lo"][:],
                                                   op0=ALU.mult, op1=ALU.add), r=["hi", "pred", "lo"], w=["lo"])
        for k5 in range(Kc // 512):
            mk = mkb[k5 % 2]
            S.dve(lambda e, mk=mk, k5=k5: e.tensor_scalar(mk[:], score[:, k5 * 512:(k5 + 1) * 512], sm["lo"][:], -30000.0,
                                                          ALU.is_lt, ALU.mult), r=["score", "lo"], w=[("mkb", k5 % 2)])
            pbb = P.psb[k5 % 2]
            for i in range(4):
                S.pe(lambda e, mk=mk, i=i, pbb=pbb: e.transpose(out=pbb[:, i * 128:(i + 1) * 128],
                                                                in_=mk[:, i * 128:(i + 1) * 128], identity=c["ident"][:]),
                     r=[("mkb", k5 % 2), "ident"], w=[("psb", k5 % 2)])
            S.act(lambda e, k5=k5, pbb=pbb: e.copy(out=maskT[:, k5 * 4:(k5 + 1) * 4, :],
                                                   in_=pbb[:, 0:512].rearrange("p (a b) -> p a b", b=128)),
                  r=[("psb", k5 % 2)], w=["maskT"])

    def gen_attention(j):
        Kc = 1024 * (j + 1)
        n1 = Kc // 128
        for g in range(4):
            gi = j * 4 + g
            qg = qt[gi % 2]
            kT = kTb[gi % 2]
            vg = vgb[gi % 2]
            kk, vk = ("kTg", gi % 2), ("vg", gi % 2)
            S.dma(qg[:], q3[:, 4 * g:4 * g + 4, j * 128:(j + 1) * 128], w=[("qt", gi % 2)])
            S.dma(kT[:, 0:Kc], d["kTa"][:, g * T:g * T + Kc], w=[kk])
            for k0 in range(0, n1, 16):
                S.dma(vg[:, k0:k0 + 16, :],
                      d["va"][k0 * 128:(k0 + 16) * 128, g * 128:(g + 1) * 128].rearrange("(k p) e -> p k e", p=128),
                      w=[vk])
            qg2 = qg[:].rearrange("p a b -> p (a b)")

            def st(kc, kT=kT, kk=kk, qg2=qg2, gi=gi):
                pb = P.ps[2 + kc % 2]
                S.pe(lambda e, pb=pb: e.matmul(pb[:], lhsT=kT[:, kc * 128:(kc + 1) * 128], rhs=qg2, start=True, stop=False),
                     r=[kk, ("qt", gi % 2)], w=[("ps", 2 + kc % 2)])
                for hh in range(4):
                    S.pe(lambda e, pb=pb, hh=hh: e.matmul(pb[:, hh * 128:(hh + 1) * 128], lhsT=c["ident"][:], rhs=maskT[:, kc, :],
                                                          start=False, stop=(hh == 3)),
                         r=["ident", "maskT"], w=[("ps", 2 + kc % 2)])
            st(0)
            for kc in range(n1):
                if kc + 1 < n1:
                    st(kc + 1)
                pb = P.ps[2 + kc % 2]
                pt = ptb[kc % 3]
                S.act(lambda e, pb=pb, pt=pt: e.activation(out=pt[:], in_=pb[:], func=AF.Exp, scale=SC),
                      r=[("ps", 2 + kc % 2)], w=[("ptb", kc % 3)])
                S.pe(lambda e, kc=kc, pt=pt, vg=vg: e.matmul(P.ps[4][:], lhsT=vg[:, kc, :], rhs=pt[:],
                                                             start=(kc == 0), stop=(kc == n1 - 1)),
                     r=[vk, ("ptb", kc % 3)], w=[("ps", 4)])
                S.pe(lambda e, kc=kc, pt=pt: e.matmul(P.ps[5][:], lhsT=c["ones"][:], rhs=pt[:],
                                                      start=(kc == 0), stop=(kc == n1 - 1)),
                     r=["ones", ("ptb", kc % 3)], w=[("ps", 5)])
                yield
            S.dve(lambda e: e.reciprocal(out=rden[:], in_=P.ps[5][:]), r=[("ps", 5)], w=["rden"])
            o = ot[g % 2]
            S.dve(lambda e, o=o: e.tensor_tensor(out=o[:], in0=P.ps[4][:], in1=rden[:], op=ALU.mult),
                  r=[("ps", 4), "rden"], w=[("ot", g % 2)])
            S.dma(o3[:, 4 * g:4 * g + 4, j * 128:(j + 1) * 128], o[:].rearrange("p (a b) -> p a b", b=128),
                  r=[("ot", g % 2)], w=[("oT_s", j, g)])
            okeys.append(("oT_s", j, g))

    for _ in gen_indexer(0):
        pass
    post_indexer(0)
    for j in range(NT):
        ga = gen_attention(j)
        gx = gen_indexer(j + 1) if j + 1 < NT else iter(())
        da = dx = False
        while not (da and dx):
            if not da:
                try:
                    next(ga)
                except StopIteration:
                    da = True
            if not dx:
                try:
                    next(gx)
                except StopIteration:
                    dx = True
        if j + 1 < NT:
            post_indexer(j + 1)
    return okeys


def build_A2(dbg=False):
    P = Prog()
    S = P.S
    x_in = P.inp("x", [TL, D])
    d = {"qT": P.inp("qT", [128, 16 * TL], BF16), "iqT": P.inp("iqT", [128, 16 * TL], BF16),
         "iw": P.inp("iw", [128, NT * 16]), "kTa": P.inp("kTa", [128, 4 * T], BF16), "va": P.inp("va", [T, 512], BF16),
         "ikTa": P.inp("ikTa", [128, T], BF16),
         "cm": P.inp("cm", [128, 1024]), "pen": P.inp("pen", [128, 1024])}
    w_out = P.inp("w_out", [D, D])
    g2 = P.inp("g_bc", [128, D])
    w_up = P.inp("w_up", [D, DFF])
    w_down = P.inp("w_down", [DFF, D])
    y = P.out("y", [TL, D])
    oT_s = P.out("oT_dbg", [128, 16 * TL]) if dbg else P.scratch("oT_s", [128, 16 * TL])
    with ExitStack() as es:
        c = load_consts(P, es)
        with ExitStack() as es1:
            emit_attention(P, c, es1, d, oT_s)
            S.barrier()
        x_res = P.sb(es, "x_res", [128, NT, D], F32)
        load_x(P, x_res, x_in)
        with ExitStack() as es2:
            oT = P.sb(es2, "oT", [128, KC, TL], BF16)
            S.dma(oT[:], oT_s.rearrange("p (h t) -> p h t", t=TL), w=["oT"], q="pool")
            ws = WStream(P, es2, "wout", [128, 4096], nbuf=3)
            emit_linear_tm_res(P, ws, oT, "oT", KC, w_out, x_res)
            S.barrier()
        emit_mlp(P, c, es, x_res, g2, w_up, w_down)
        keys = store_x(P, y, x_res)
        nc = P.close(keys)
    return nc


def a2_inputs(xs, a1, d, j, li):
    kT = np.stack([a1[c]["kT"].reshape(128, 4, NT, 128) for c in range(NCORES)], 3)
    kTa = np.ascontiguousarray(kT.reshape(128, 4 * T))
    ik = np.stack([a1[c]["ikT"].reshape(128, NT, 128) for c in range(NCORES)], 2)
    ikTa = np.ascontiguousarray(ik.reshape(128, T))
    va = unshard_tokens([a1[c]["v"] for c in range(NCORES)])
    common = {"kTa": kTa, "va": va, "ikTa": ikTa, "w_out": np.ascontiguousarray(d["attn_w_out"][j]),
              "g_bc": bc128(d["mlp_norm"][li]), "w_up": np.ascontiguousarray(d["mlp_w_up"][li]),
              "w_down": np.ascontiguousarray(d["mlp_w_down"][li]), "c_ident": IDENT}
    ins = []
    f = np.arange(1024).reshape(1, 1024)
    p = np.arange(128).reshape(128, 1)
    for c in range(NCORES):
        cm = (f <= 128 * c + p)
        ins.append(dict(common, x=xs[c], qT=a1[c]["qT"], iqT=a1[c]["iqT"], iw=a1[c]["iw"],
                        cm=np.where(cm, np.float32(1.0), np.float32(0.0)).astype(np.float32),
                        pen=np.where(cm, np.float32(0.0), np.float32(-BIG)).astype(np.float32)))
    return ins


POOL_W = (2, 4, 8, 16)


def emit_pool(P, c, es_outer, x_res, hT9, d):
    S = P.S
    with ExitStack() as es:
        hx = P.sb(es, "hx", [128, 8, 144], F32)
        sA = P.sb(es, "sA", [128, 8, 144], F32)
        sB = P.sb(es, "sB", [128, 8, 144], F32)
        yT = P.sb(es, "yTg", [128, 4, TL], BF16)
        rd = P.sb(es, "rd", [128, TL], F32)
        wp = P.sb(es, "wp", [128, 4, 4, 512], BF16)
        bbc = P.sb(es, "bbc", [128, D], F32)
        sbc = P.sb(es, "sbc", [128, D], F32)
        tt = [P.sb(es, "ptt", [128, 512], F32) for _ in range(2)]
        for g in range(4):
            S.dma(wp[:, g], d["pool_w"][g * 512:(g + 1) * 512, :].rearrange("(k p) n -> p k n", p=128), w=["wp"], q="pool")
        S.dma(bbc[:], d["pool_b"], w=["bbc"])
        S.dma(sbc[:], d["pool_s"], w=["sbc"])
        for g in range(4):
            w = POOL_W[g]
            S.dma(rd[:], d["mind"][:, g * TL:(g + 1) * TL], w=["rd"])
            S.dve(lambda e: e.reciprocal(out=rd[:], in_=rd[:]), r=["rd"], w=["rd"])
            rd3 = rd[:].rearrange("p (a b) -> p a b", b=128)
            for ci in range(4):
                ch = 4 * g + ci
                S.act(lambda e, ch=ch: e.copy(out=hx[:, :, 16:144], in_=hT9[:, ch, 0:TL].rearrange("p (a b) -> p a b", b=128)),
                      r=["hT"], w=["hx"])
                S.act(lambda e, ch=ch: e.copy(out=hx[:, :, 0:16], in_=hT9[:, ch, TL:TL + 128].rearrange("p (a b) -> p a b", b=16)),
                      r=["hT"], w=["hx"])
                cur, ck = hx, "hx"
                nxt = [(sA, "sA"), (sB, "sB")]
                step = 1
                k = 0
                while step < w:
                    o, ok = nxt[k % 2]
                    S.dve(lambda e, o=o, cur=cur, step=step: e.tensor_tensor(
                        out=o[:, :, step:144], in0=cur[:, :, step:144], in1=cur[:, :, 0:144 - step], op=ALU.add),
                        r=[ck], w=[ok])
                    cur, ck = o, ok
                    step *= 2
                    k += 1
                S.dve(lambda e, cur=cur, rd3=rd3: e.tensor_tensor(out=cur[:, :, 16:144], in0=cur[:, :, 16:144], in1=rd3,
                                                                 op=ALU.mult), r=[ck, "rd"], w=[ck])
                S.dve(lambda e, cur=cur, ci=ci: e.tensor_tensor(
                    out=yT[:, ci, :].rearrange("p (a b) -> p a b", b=128), in0=cur[:, :, 16:144], in1=hx[:, :, 16:144],
                    op=ALU.subtract), r=[ck, "hx"], w=["yTg"])
            for j in range(NT):
                pb = P.ps[j % 4]
                for kc in range(4):
                    S.pe(lambda e, kc=kc, j=j, pb=pb, g=g: e.matmul(pb[:], lhsT=yT[:, kc, j * 128:(j + 1) * 128],
                                                                   rhs=wp[:, g, kc, :], start=(kc == 0), stop=(kc == 3)),
                         r=["yTg", "wp"], w=[("ps", j % 4)])
                t_ = tt[j % 2]
                gs = slice(g * 512, (g + 1) * 512)
                S.dve(lambda e, t_=t_, pb=pb, gs=gs: e.tensor_tensor(out=t_[:], in0=pb[:], in1=bbc[:, gs], op=ALU.add),
                      r=[("ps", j % 4), "bbc"], w=[("ptt", j % 2)])
                S.pool(lambda e, t_=t_, gs=gs: e.tensor_tensor(out=t_[:], in0=t_[:], in1=sbc[:, gs], op=ALU.mult),
                       r=[("ptt", j % 2), "sbc"], w=[("ptt", j % 2)])
                xs_ = x_res[:, j, gs]
                S.dve(lambda e, t_=t_, xs_=xs_: e.tensor_tensor(out=xs_, in0=xs_, in1=t_[:], op=ALU.add),
                      r=[("ptt", j % 2), ("x", j)], w=[("x", j)])
        S.barrier()


def build_PA1():
    P = Prog()
    S = P.S
    x9 = P.inp("x9", [9 * 128, D])
    gp = P.inp("gp_bc", [128, D])
    dp = {"pool_w": P.inp("pool_w", [D, 512]), "pool_b": P.inp("pool_b", [128, D]), "pool_s": P.inp("pool_s", [128, D]),
          "mind": P.inp("mind", [128, 4 * TL])}
    g2 = P.inp("g_bc", [128, D])
    w_up = P.inp("w_up", [D, DFF])
    w_down = P.inp("w_down", [DFF, D])
    ga = P.inp("ga_bc", [128, D])
    w_in = P.inp("w_in", [D, A_IN])
    d_in, d_out = attn_a1_io(P)
    y = P.out("y", [TL, D])
    with ExitStack() as es:
        c = load_consts(P, es)
        x_res = P.sb(es, "x_res", [128, NT, D], F32)
        load_x(P, x_res, x9)
        with ExitStack() as es1:
            gbc = P.sb(es1, "gbc", [128, D], F32)
            S.dma(gbc[:], gp, w=["gbc"])
            wk = norm_work(P, es1)
            hT9 = P.sb(es1, "hT9", [128, KC, 9 * 128], BF16)
            xt = P.sb(es1, "xt", [128, D], F32)
            S.dma(xt[:], x9[TL:TL + 128, :], w=["xt"])
            for t in range(NT):
                emit_norm_T(P, c, x_res[:, t, :], ("x", t), gbc[:], hT9, "hT", t * 128, wk)
            emit_norm_T(P, c, xt[:], "xt", gbc[:], hT9, "hT", TL, wk)
            emit_pool(P, c, es1, x_res, hT9, dp)
        emit_mlp(P, c, es, x_res, g2, w_up, w_down)
        keys = store_x(P, y, x_res)
        with ExitStack() as es2:
            gbc = P.sb(es2, "gbc", [128, D], F32)
            S.dma(gbc[:], ga, w=["gbc"])
            wk = norm_work(P, es2)
            hT = P.sb(es2, "hT", [128, KC, TL], BF16)
            for t in range(NT):
                emit_norm_T(P, c, x_res[:, t, :], ("x", t), gbc[:], hT, "hT", t * 128, wk)
            keys += emit_attn_inproj(P, c, es2, hT, w_in, d_in, d_out)
            S.barrier()
        nc = P.close(keys)
    return nc


def pa1_inputs(xg, pos, d, li, ja):
    xs = shard_tokens(xg)
    a1 = a1_inputs(xs, pos, d, ja, with_x=False)
    common = {"gp_bc": bc128(d["pool_norm"][0]), "pool_w": np.ascontiguousarray(d["pool_w"][0].reshape(D, 512)),
              "pool_b": bc128(d["pool_b"][0].reshape(-1)), "pool_s": bc128(d["pool_scale"][0]),
              "g_bc": bc128(d["mlp_norm"][li]), "w_up": np.ascontiguousarray(d["mlp_w_up"][li]),
              "w_down": np.ascontiguousarray(d["mlp_w_down"][li]), "ga_bc": bc128(d["attn_norm"][ja])}
    ins = []
    for c in range(NCORES):
        idx = (np.arange(NT).reshape(NT, 1) * 8 + c) * 128 + np.arange(128).reshape(1, 128)
        mind = np.stack([np.minimum(idx + 1, w) for w in POOL_W], 0).reshape(1, 4 * TL).astype(np.float32)
        m = dict(common, **a1[c])
        m["x9"] = np.concatenate([xs[c], halo_tile(xg, c)], 0)
        m["mind"] = np.ascontiguousarray(np.broadcast_to(mind, (128, 4 * TL)))
        ins.append(m)
    return ins


_CACHE = {}


def _prog(name, fn):
    if name not in _CACHE:
        _CACHE[name] = fn()
    return _CACHE[name]


def _run(nc, ins):
    return run_bass_kernel_spmd(nc, ins, core_ids=list(range(NCORES))).results


def kernel(**inp):
    d = {k: np.asarray(v) for k, v in inp.items()}
    xg = np.ascontiguousarray(d["x"].reshape(T, D).astype(np.float32, copy=False))
    pos = d["positions"].reshape(T)
    xs = shard_tokens(xg)
    a1 = _run(_prog("A1", build_A1), a1_inputs(xs, pos, d, 0))
    r = _run(_prog("A2", build_A2), a2_inputs(xs, a1, d, 0, 0))
    xg = unshard_tokens([r[c]["y"] for c in range(NCORES)])
    r1 = _run(_prog("R1", build_R1), r1_inputs(xg, d, 0))
    r = _run(_prog("R2", build_R2), r2_inputs(xg, r1, d, 0, 1))
    xg = unshard_tokens([r[c]["y"] for c in range(NCORES)])
    r = _run(_prog("PA1", build_PA1), pa1_inputs(xg, pos, d, 2, 1))
    xg = unshard_tokens([r[c]["y"] for c in range(NCORES)])
    xs = shard_tokens(xg)
    r = _run(_prog("A2", build_A2), a2_inputs(xs, r, d, 1, 3))
    out = unshard_tokens([r[c]["y"] for c in range(NCORES)])
    return out.reshape(1, T, D).astype(np.float32, copy=False)
```

```python
import numpy as np
from contextlib import ExitStack
import concourse.bass as bass
import concourse.mybir as mybir
from concourse.bass_utils import run_bass_kernel_spmd

F32 = mybir.dt.float32
BF16 = mybir.dt.bfloat16
I32 = mybir.dt.int32
ALU = mybir.AluOpType
AF = mybir.ActivationFunctionType
AX = mybir.AxisListType

NCORES = 8
T = 8192
D = 2048
TL = T // NCORES
NT = TL // 128
KC = D // 128
DFF = 4 * D
EPS = 1e-6
A_IN = 5264
TOPK = 256
NBIS = 21
BIG = 1.0e30


class Sched:
    NDS = 40

    def __init__(self, nc, es):
        self.nc = nc
        self.E = {"pe": nc.tensor, "act": nc.scalar, "dve": nc.vector, "pool": nc.gpsimd, "sp": nc.sync}
        self.csem = {e: es.enter_context(nc.semaphore("c_" + e)) for e in ("pe", "act", "dve", "pool")}
        self.ccnt = {e: 0 for e in self.csem}
        self.dsem = [es.enter_context(nc.semaphore("d%d" % i)) for i in range(self.NDS)]
        self.dcnt = [0] * self.NDS
        self.drr = 0
        self.known = {e: {} for e in self.E}
        self.lastw = {}
        self.readers = {}
        self.nins = 0

    def _wait(self, eng, ev):
        sid, h, val, src, isdma = ev
        if src == "pe" and eng == "pe" and not isdma:
            return
        if self.known[eng].get(sid, 0) >= val:
            return
        self.E[eng].wait_ge(h, val)
        self.known[eng][sid] = val

    def op(self, eng, fn, r=(), w=(), dma=False):
        deps = []
        for k in r:
            if k in self.lastw:
                deps.append(self.lastw[k])
        for k in w:
            if k in self.lastw:
                deps.append(self.lastw[k])
            deps.extend(self.readers.get(k, {}).values())
        for ev in deps:
            self._wait(eng, ev)
        if dma:
            i = self.drr
            self.drr = (self.drr + 1) % self.NDS
            if self.dcnt[i] > 0:
                self._wait(eng, (("d", i), self.dsem[i], self.dcnt[i], eng, True))
        ins = fn(self.E[eng])
        self.nins += 1
        if dma:
            self.dcnt[i] += 16
            ins.then_inc(self.dsem[i], 16)
            ev = (("d", i), self.dsem[i], self.dcnt[i], eng, True)
        else:
            self.ccnt[eng] += 1
            ins.then_inc(self.csem[eng], 1)
            ev = (("c", eng), self.csem[eng], self.ccnt[eng], eng, False)
        for k in w:
            self.lastw[k] = ev
            self.readers[k] = {}
        for k in r:
            self.readers.setdefault(k, {})[ev[0]] = ev
        return ev

    def pe(self, fn, r=(), w=()):
        return self.op("pe", fn, r, w)

    def act(self, fn, r=(), w=()):
        return self.op("act", fn, r, w)

    def dve(self, fn, r=(), w=()):
        return self.op("dve", fn, r, w)

    def pool(self, fn, r=(), w=()):
        return self.op("pool", fn, r, w)

    def dma(self, out, in_, r=(), w=(), q="sp"):
        return self.op(q, lambda e: e.dma_start(out=out, in_=in_), r, w, dma=True)

    def barrier(self):
        evs = []
        for e in self.csem:
            if self.ccnt[e] > 0:
                evs.append((("c", e), self.csem[e], self.ccnt[e], e, False))
        for i in range(self.NDS):
            if self.dcnt[i] > 0:
                evs.append((("d", i), self.dsem[i], self.dcnt[i], "sp", True))
        for eng in self.E:
            for ev in evs:
                if ev[3] == "pe" and eng == "pe" and not ev[4]:
                    continue
                if self.known[eng].get(ev[0], 0) >= ev[2]:
                    continue
                self.E[eng].wait_ge(ev[1], ev[2])
                self.known[eng][ev[0]] = ev[2]
        self.lastw = {}
        self.readers = {}

    def finish(self, keys):
        for k in keys:
            if k in self.lastw:
                self._wait("sp", self.lastw[k])


class Prog:
    def __init__(self):
        self.nc = bass.Bass("TRN2", target_bir_lowering=False)
        self.es = ExitStack()
        self.S = Sched(self.nc, self.es)
        self.ins = {}
        self.outs = {}
        nc = self.nc
        self.ps = [self.es.enter_context(nc.psum_tensor("psf%d" % i, [128, 512], F32)) for i in range(6)]
        self.psb = [self.es.enter_context(nc.psum_tensor("psb%d" % i, [128, 1024], BF16)) for i in range(2)]
        self.uid = 0

    def inp(self, name, shape, dt=F32):
        t = self.nc.dram_tensor(name, list(shape), dt, kind="ExternalInput").ap()
        self.ins[name] = t
        return t

    def out(self, name, shape, dt=F32):
        t = self.nc.dram_tensor(name, list(shape), dt, kind="ExternalOutput").ap()
        self.outs[name] = t
        return t

    def scratch(self, name, shape, dt=F32):
        return self.nc.dram_tensor(name, list(shape), dt).ap()

    def sb(self, es, name, shape, dt=F32):
        self.uid += 1
        return es.enter_context(self.nc.sbuf_tensor("%s_%d" % (name, self.uid), list(shape), dt))

    def close(self, out_keys):
        self.S.finish(out_keys)
        self.es.close()
        return self.nc


def load_consts(P, es):
    S = P.S
    c = {}
    ident_d = P.inp("c_ident", [128, 128])
    c["ident"] = P.sb(es, "ident", [128, 128], BF16)
    c["ones"] = P.sb(es, "ones", [128, 128], BF16)
    S.dma(c["ident"][:], ident_d, w=["ident"], q="pool")
    S.pool(lambda e: e.memset(c["ones"][:], 1.0), w=["ones"])
    return c


def emit_norm_T(P, c, xsrc, xkey, g_bc, hT, hkey, col0, work):
    S = P.S
    junk, ss, rs, hbf = work["junk"], work["ss"], work["rs"], work["hbf"]
    S.dve(lambda e: e.scalar_tensor_tensor(out=junk[:], in0=xsrc, scalar=1.0, in1=xsrc,
                                           op0=ALU.mult, op1=ALU.mult, accum_out=ss[:]),
          r=[xkey], w=["n_junk", "n_ss"])
    S.act(lambda e: e.activation(out=rs[:], in_=ss[:], func=AF.Sqrt, bias=work["eps"][:], scale=1.0 / D),
          r=["n_ss", "n_eps"], w=["n_rs"])
    S.dve(lambda e: e.reciprocal(out=rs[:], in_=rs[:]), r=["n_rs"], w=["n_rs"])
    S.dve(lambda e: e.scalar_tensor_tensor(out=hbf[:], in0=xsrc, scalar=rs[:], in1=g_bc,
                                           op0=ALU.mult, op1=ALU.mult),
          r=[xkey, "n_rs", "gbc"], w=["n_hbf"])
    for q in range(KC // 8):
        pb = P.psb[q % 2]
        for i in range(8):
            kc = q * 8 + i
            S.pe(lambda e, kc=kc, i=i: e.transpose(out=pb[:, i * 128:(i + 1) * 128],
                                                   in_=hbf[:, kc * 128:(kc + 1) * 128],
                                                   identity=c["ident"][:]),
                 r=["n_hbf", "ident"], w=[("psb", q % 2)])
        dst = hT[:, q * 8:(q + 1) * 8, col0:col0 + 128]
        src = pb[:].rearrange("p (a b) -> p a b", b=128)
        if q % 2 == 0:
            S.act(lambda e, dst=dst, src=src: e.copy(out=dst, in_=src), r=[("psb", q % 2)], w=[hkey])
        else:
            S.dve(lambda e, dst=dst, src=src: e.tensor_copy(out=dst, in_=src), r=[("psb", q % 2)], w=[hkey])


def norm_work(P, es):
    w = {
        "junk": P.sb(es, "n_junk", [128, D], BF16),
        "ss": P.sb(es, "n_ss", [128, 1], F32),
        "rs": P.sb(es, "n_rs", [128, 1], F32),
        "hbf": P.sb(es, "n_hbf", [128, D], BF16),
        "eps": P.sb(es, "n_eps", [128, 1], F32),
    }
    P.S.pool(lambda e: e.memset(w["eps"][:], EPS), w=["n_eps"])
    return w


class WStream:
    def __init__(self, P, es, name, shape, nbuf=4):
        self.P = P
        self.name = name
        self.bufs = [P.sb(es, name, shape, BF16) for _ in range(nbuf)]
        self.nbuf = nbuf
        self.pieces = []
        self.issued = 0
        self.used = 0

    def plan(self, pieces):
        self.pieces = list(pieces)
        self.issued = 0
        self.used = 0

    def _issue(self):
        i = self.issued
        b = i % self.nbuf
        dst, src = self.pieces[i](self.bufs[b])
        self.P.S.dma(dst, src, w=[(self.name, b)], q="pool")
        self.issued += 1

    def next(self):
        while self.issued < len(self.pieces) and self.issued < self.used + self.nbuf - 1:
            self._issue()
        if self.issued <= self.used:
            self._issue()
        b = self.used % self.nbuf
        self.used += 1
        return self.bufs[b], (self.name, b)


def emit_mlp(P, c, es_outer, x_res, g_bc_dram, w_up, w_down):
    S = P.S
    HF = DFF // 2
    NOC = HF // 128
    acc = [P.ps[t][:] for t in range(6)] + [P.psb[t][:].bitcast(F32) for t in range(2)]
    akey = [("ps", t) for t in range(6)] + [("psb", t) for t in range(2)]
    with ExitStack() as es:
        gbc = P.sb(es, "gbc", [128, D], F32)
        S.dma(gbc[:], g_bc_dram, w=["gbc"])
        wk = norm_work(P, es)
        hT = P.sb(es, "hT", [128, KC, TL], BF16)
        actT = P.sb(es, "actT", [128, NOC, TL], BF16)
        rl = [P.sb(es, "rl", [128, 512], F32) for _ in range(2)]
        ws = WStream(P, es, "wmlp", [128, 4096], nbuf=3)
        for j in range(NT):
            emit_norm_T(P, c, x_res[:, j, :], ("x", j), gbc[:], hT, "hT", j * 128, wk)
        for fh in range(2):
            def up_piece(i):
                def f(buf):
                    dst = buf[:].rearrange("p (k n) -> p k n", n=256)
                    c0 = fh * HF + i * 256
                    src = w_up[:, c0:c0 + 256].rearrange("(k p) n -> p k n", p=128)
                    return dst, src
                return f
            ws.plan([up_piece(i) for i in range(NOC // 2)])
            cnt = 0
            for i in range(NOC // 2):
                buf, bkey = ws.next()
                wv = buf[:].rearrange("p (k n) -> p k n", n=256)
                for s in range(2):
                    oc = i * 2 + s
                    for tc in range(2):
                        bi = cnt % 4
                        pb = P.ps[bi]
                        for kc in range(KC):
                            S.pe(lambda e, kc=kc, s=s, pb=pb, wv=wv, tc=tc: e.matmul(
                                pb[:], lhsT=wv[:, kc, s * 128:(s + 1) * 128], rhs=hT[:, kc, tc * 512:(tc + 1) * 512],
                                start=(kc == 0), stop=(kc == KC - 1)),
                                r=[bkey, "hT"], w=[("ps", bi)])
                        r_ = rl[cnt % 2]
                        S.act(lambda e, pb=pb, r_=r_: e.activation(out=r_[:], in_=pb[:], func=AF.Relu),
                              r=[("ps", bi)], w=[("rl", cnt % 2)])
                        S.pool(lambda e, oc=oc, r_=r_, tc=tc: e.tensor_tensor(
                            out=actT[:, oc, tc * 512:(tc + 1) * 512], in0=r_[:], in1=r_[:], op=ALU.mult),
                            r=[("rl", cnt % 2)], w=["actT"])
                        cnt += 1
            nkp = NOC // 8

            def dn_piece(cc, kp):
                def f(buf):
                    dst = buf[:].rearrange("p (k n) -> p k n", n=512)
                    r0 = fh * HF + kp * 1024
                    src = w_down[r0:r0 + 1024, cc * 512:(cc + 1) * 512].rearrange("(k p) n -> p k n", p=128)
                    return dst, src
                return f
            ws.plan([dn_piece(cc, kp) for cc in range(4) for kp in range(nkp)])
            for cc in range(4):
                for kp in range(nkp):
                    buf, bkey = ws.next()
                    wv = buf[:].rearrange("p (k n) -> p k n", n=512)
                    for t in range(NT):
                        for k in range(8):
                            kc = kp * 8 + k
                            S.pe(lambda e, t=t, k=k, kc=kc, wv=wv: e.matmul(
                                acc[t], lhsT=actT[:, kc, t * 128:(t + 1) * 128], rhs=wv[:, k, :],
                                start=(kc == 0), stop=(kc == NOC - 1)),
                                r=[bkey, "actT"], w=[akey[t]])
                for t in range(NT):
                    xs = x_res[:, t, cc * 512:(cc + 1) * 512]
                    S.dve(lambda e, xs=xs, t=t: e.tensor_tensor(out=xs, in0=xs, in1=acc[t], op=ALU.add),
                          r=[akey[t], ("x", t)], w=[("x", t)])
        S.barrier()


def build_mlp_only():
    P = Prog()
    S = P.S
    x_in = P.inp("x", [TL, D])
    g = P.inp("g_bc", [128, D])
    w_up = P.inp("w_up", [D, DFF])
    w_down = P.inp("w_down", [DFF, D])
    y = P.out("y", [TL, D])
    with ExitStack() as es:
        c = load_consts(P, es)
        x_res = P.sb(es, "x_res", [128, NT, D], F32)
        for j in range(NT):
            S.dma(x_res[:, j, :], x_in[j * 128:(j + 1) * 128, :], w=[("x", j)])
        emit_mlp(P, c, es, x_res, g, w_up, w_down)
        for j in range(NT):
            S.dma(y[j * 128:(j + 1) * 128, :], x_res[:, j, :], r=[("x", j)], w=[("y", j)])
        nc = P.close([("y", j) for j in range(NT)])
    return nc


def shard_tokens(a):
    b = a.reshape((NT, NCORES, 128) + a.shape[1:])
    return [np.ascontiguousarray(b[:, c].reshape((TL,) + a.shape[1:])) for c in range(NCORES)]


def unshard_tokens(parts):
    a = np.stack([p.reshape((NT, 128) + p.shape[1:]) for p in parts], axis=1)
    return np.ascontiguousarray(a.reshape((T,) + parts[0].shape[1:]))


def bc128(v):
    return np.ascontiguousarray(np.broadcast_to(np.asarray(v, np.float32).reshape(1, -1), (128, v.size)))


IDENT = np.eye(128, dtype=np.float32)


def emit_linear_tm_res(P, ws, AT, atkey, nkc, w_dram, x_res):
    S = P.S
    nkp = nkc // 8
    for half in range(2):
        def piece(cc, kp):
            def f(buf):
                dst = buf[:].rearrange("p (k n) -> p k n", n=512)
                src = w_dram[kp * 1024:(kp + 1) * 1024, cc * 512:(cc + 1) * 512].rearrange(
                    "(k p) n -> p k n", p=128)
                return dst, src
            return f
        ws.plan([piece(cc, kp) for cc in range(4) for kp in range(nkp)])
        for cc in range(4):
            for kp in range(nkp):
                buf, bkey = ws.next()
                wv = buf[:].rearrange("p (k n) -> p k n", n=512)
                for t in range(4):
                    j = half * 4 + t
                    for k in range(8):
                        kc = kp * 8 + k
                        S.pe(lambda e, t=t, j=j, k=k, kc=kc, wv=wv: e.matmul(
                            P.ps[t][:], lhsT=AT[:, kc, j * 128:(j + 1) * 128], rhs=wv[:, k, :],
                            start=(kc == 0), stop=(kc == nkc - 1)),
                            r=[bkey, atkey], w=[("ps", t)])
            for t in range(4):
                j = half * 4 + t
                xs = x_res[:, j, cc * 512:(cc + 1) * 512]
                S.dve(lambda e, xs=xs, t=t: e.tensor_tensor(out=xs, in0=xs, in1=P.ps[t][:], op=ALU.add),
                      r=[("ps", t), ("x", j)], w=[("x", j)])


def load_x(P, x_res, x_in):
    for j in range(NT):
        P.S.dma(x_res[:, j, :], x_in[j * 128:(j + 1) * 128, :], w=[("x", j)])


def store_x(P, y, x_res):
    for j in range(NT):
        P.S.dma(y[j * 128:(j + 1) * 128, :], x_res[:, j, :], r=[("x", j)], w=[("y", j)])
    return [("y", j) for j in range(NT)]


GELU_C = 0.044715
GELU_S = 1.5957691216057308


def build_R1(stage=99):
    P = Prog()
    S = P.S
    x9 = P.inp("x9", [9 * 128, D])
    g = P.inp("g_bc", [128, D])
    w_in = P.inp("w_in", [D, 2 * D])
    cw_d = P.inp("cw", [128, KC * 4])
    vec_d = P.inp("vecs", [128, KC * 4])
    wa_d = P.inp("wa", [8 * 256, 256])
    wx_d = P.inp("wx", [8 * 256, 256])
    gel_o = P.out("gel", [128, KC * TL])
    hl_o = P.out("hloc", [128, KC * TL])
    pp_o = P.out("pp", [128, KC * TL])
    ab_o = P.out("ab", [128, KC * 16])
    with ExitStack() as es:
        c = load_consts(P, es)
        gbc = P.sb(es, "gbc", [128, D], F32)
        S.dma(gbc[:], g, w=["gbc"])
        wk = norm_work(P, es)
        hT = P.sb(es, "hT9", [128, KC, 9 * 128], BF16)
        xt = [P.sb(es, "xt", [128, D], F32) for _ in range(2)]
        for t in range(9):
            S.dma(xt[t % 2][:], x9[t * 128:(t + 1) * 128, :], w=[("xt", t % 2)])
            emit_norm_T(P, c, xt[t % 2][:], ("xt", t % 2), gbc[:], hT, "hT", t * 128, wk)
        cw = P.sb(es, "cw", [128, KC, 4], F32)
        vec = P.sb(es, "vec", [128, KC, 4], F32)
        S.dma(cw[:], cw_d.rearrange("p (k n) -> p k n", n=4), w=["cw"])
        S.dma(vec[:], vec_d.rearrange("p (k n) -> p k n", n=4), w=["vec"])
        wa = P.sb(es, "wa", [128, 8, 2, 256], BF16)
        wx = P.sb(es, "wx", [128, 8, 2, 256], BF16)
        for n in range(8):
            S.dma(wa[:, n], wa_d[n * 256:(n + 1) * 256, :].rearrange("(k p) c -> p k c", p=128), w=["wa"], q="pool")
            S.dma(wx[:, n], wx_d[n * 256:(n + 1) * 256, :].rearrange("(k p) c -> p k c", p=128), w=["wx"], q="pool")
        one = P.sb(es, "one", [128, 1], F32)
        S.pool(lambda e: e.memset(one[:], 1.0), w=["one"])
        cl = P.sb(es, "cl", [128, KC], F32)
        S.act(lambda e: e.activation(out=cl[:], in_=vec[:, :, 3], func=AF.Exp, scale=-1.0), r=["vec"], w=["cl"])
        S.act(lambda e: e.activation(out=cl[:], in_=cl[:], func=AF.Ln, bias=one[:], scale=1.0),
              r=["cl", "one"], w=["cl"])
        S.dve(lambda e: e.tensor_scalar(cl[:], cl[:], -8.0, None, ALU.mult), r=["cl"], w=["cl"])
        if stage == 1:
            return P.close([])
        ws = WStream(P, es, "wrin", [128, 4096], nbuf=3)
        f4 = lambda name: P.sb(es, name, [128, TL], F32)
        gelb = [f4("gelb"), f4("gelb")]
        hlb = [f4("hlb"), f4("hlb")]
        ppb = [f4("ppb"), f4("ppb")]
        rr, ii, aa, a2, bb, ap_, dd = f4("rr"), f4("ii"), f4("aa"), f4("a2"), f4("bb"), f4("ap"), f4("dd")
        t1 = [P.sb(es, "t1", [128, 512], F32) for _ in range(2)]
        xrx = P.sb(es, "xrx", [128, 2, 8, 131], F32)
        xc = P.sb(es, "xc", [128, 2, TL], F32)
        xcb = P.sb(es, "xcb", [128, 2, TL], BF16)
        ab = P.sb(es, "ab", [128, KC, 2, 8], F32)
        S.pool(lambda e: e.memset(dd[:], 0.0), w=["dd"])

        def piece(col0):
            def f(buf):
                dst = buf[:].rearrange("p (k n) -> p k n", n=256)
                src = w_in[:, col0:col0 + 256].rearrange("(k p) n -> p k n", p=128)
                return dst, src
            return f
        pcs = []
        for n in range(8):
            pcs.append(piece(n * 256))
            pcs.append(piece(D + n * 256))
        ws.plan(pcs)
        for n in range(8):
            buf, bkey = ws.next()
            wv = buf[:].rearrange("p (k n) -> p k n", n=256)
            for s in range(2):
                ch = 2 * n + s
                gb = gelb[ch % 2]
                for tc in range(2):
                    pb = P.ps[tc]
                    for kc in range(KC):
                        S.pe(lambda e, kc=kc, s=s, tc=tc, pb=pb, wv=wv: e.matmul(
                            pb[:], lhsT=wv[:, kc, s * 128:(s + 1) * 128], rhs=hT[:, kc, tc * 512:(tc + 1) * 512],
                            start=(kc == 0), stop=(kc == KC - 1)), r=[bkey, "hT"], w=[("ps", tc)])
                    tt = t1[tc]
                    S.act(lambda e, tt=tt, pb=pb: e.activation(out=tt[:], in_=pb[:], func=AF.Square),
                          r=[("ps", tc)], w=[("t1", tc)])
                    S.dve(lambda e, tt=tt: e.tensor_scalar(tt[:], tt[:], GELU_C, 1.0, ALU.mult, ALU.add),
                          r=[("t1", tc)], w=[("t1", tc)])
                    S.dve(lambda e, tt=tt, pb=pb: e.tensor_tensor(out=tt[:], in0=tt[:], in1=pb[:], op=ALU.mult),
                          r=[("t1", tc), ("ps", tc)], w=[("t1", tc)])
                    S.act(lambda e, tt=tt: e.activation(out=tt[:], in_=tt[:], func=AF.Sigmoid, scale=GELU_S),
                          r=[("t1", tc)], w=[("t1", tc)])
                    gs = gb[:, tc * 512:(tc + 1) * 512]
                    S.dve(lambda e, tt=tt, pb=pb, gs=gs: e.tensor_tensor(out=gs, in0=tt[:], in1=pb[:], op=ALU.mult),
                          r=[("t1", tc), ("ps", tc)], w=[("gelb", ch % 2)])
                S.dma(gel_o[:, ch * TL:(ch + 1) * TL], gb[:], r=[("gelb", ch % 2)], w=[("gel_o", ch)])
            if stage == 2:
                return P.close([("gel_o", 0), ("gel_o", 1)])
            buf, bkey = ws.next()
            wv = buf[:].rearrange("p (k n) -> p k n", n=256)
            for s in range(2):
                ch = 2 * n + s
                for tc in range(3):
                    pb = P.ps[2 + tc]
                    rhs_of = (lambda kc, tc=tc: hT[:, kc, tc * 512:(tc + 1) * 512]) if tc < 2 else \
                        (lambda kc: hT[:, kc, 1024:1152])
                    po = pb[:] if tc < 2 else pb[:, 0:128]
                    for kc in range(KC):
                        S.pe(lambda e, kc=kc, s=s, po=po, wv=wv, rhs_of=rhs_of: e.matmul(
                            po, lhsT=wv[:, kc, s * 128:(s + 1) * 128], rhs=rhs_of(kc),
                            start=(kc == 0), stop=(kc == KC - 1)), r=[bkey, "hT"], w=[("ps", 2 + tc)])
                    if tc < 2:
                        S.act(lambda e, s=s, tc=tc, pb=pb: e.copy(
                            out=xrx[:, s, tc * 4:(tc + 1) * 4, 3:131],
                            in_=pb[:].rearrange("p (a b) -> p a b", b=128)),
                            r=[("ps", 2 + tc)], w=["xrx"])
                    else:
                        S.act(lambda e, s=s, pb=pb: e.copy(
                            out=xrx[:, s, :, 0:3],
                            in_=pb[:, 0:128].rearrange("p (a b) -> p a b", b=16)[:, :, 13:16]),
                            r=[("ps", 2 + tc)], w=["xrx"])
                xcv = xc[:, s, :].rearrange("p (a b) -> p a b", b=128)
                S.dve(lambda e, s=s, ch=ch, xcv=xcv: e.tensor_scalar(
                    xcv, xrx[:, s, :, 0:128], cw[:, ch, 0:1], vec[:, ch, 0:1], ALU.mult, ALU.add),
                    r=["xrx", "cw", "vec"], w=["xc"])
                for i in range(1, 4):
                    S.dve(lambda e, s=s, ch=ch, i=i, xcv=xcv: e.scalar_tensor_tensor(
                        out=xcv, in0=xrx[:, s, :, i:i + 128], scalar=cw[:, ch, i:i + 1], in1=xcv,
                        op0=ALU.mult, op1=ALU.add), r=["xrx", "cw", "xc"], w=["xc"])
                S.pool(lambda e, s=s: e.tensor_copy(out=xcb[:, s, :], in_=xc[:, s, :]), r=["xc"], w=["xcb"])
            if stage == 3:
                return P.close([("gel_o", 0), ("gel_o", 1)])
            for s in range(2):
                ch = 2 * n + s
                for (wg, dst, bcol, pbase, nm) in ((wa, rr, 1, 0, "rr"), (wx, ii, 2, 2, "ii")):
                    for tc in range(2):
                        pb = P.ps[pbase + tc]
                        for k in range(2):
                            S.pe(lambda e, k=k, s=s, tc=tc, pb=pb, wg=wg: e.matmul(
                                pb[:], lhsT=wg[:, n, k, s * 128:(s + 1) * 128], rhs=xcb[:, k, tc * 512:(tc + 1) * 512],
                                start=(k == 0), stop=(k == 1)), r=["wa", "wx", "xcb"], w=[("ps", pbase + tc)])
                        S.act(lambda e, tc=tc, pb=pb, dst=dst, bcol=bcol, ch=ch: e.activation(
                            out=dst[:, tc * 512:(tc + 1) * 512], in_=pb[:], func=AF.Sigmoid,
                            bias=vec[:, ch, bcol:bcol + 1], scale=1.0), r=[("ps", pbase + tc), "vec"], w=[nm])
                S.act(lambda e, ch=ch: e.activation(out=aa[:], in_=rr[:], func=AF.Exp, scale=cl[:, ch:ch + 1]),
                      r=["rr", "cl"], w=["aa"])
                S.pool(lambda e: e.tensor_tensor(out=a2[:], in0=aa[:], in1=aa[:], op=ALU.mult), r=["aa"], w=["a2"])
                S.act(lambda e: e.activation(out=a2[:], in_=a2[:], func=AF.Sqrt, bias=one[:], scale=-1.0),
                      r=["a2", "one"], w=["a2"])
                S.dve(lambda e, s=s: e.tensor_tensor(out=bb[:], in0=xc[:, s, :], in1=ii[:], op=ALU.mult),
                      r=["xc", "ii"], w=["bb"])
                S.dve(lambda e: e.tensor_tensor(out=bb[:], in0=bb[:], in1=a2[:], op=ALU.mult),
                      r=["bb", "a2"], w=["bb"])
                a3 = aa[:].rearrange("p (a b) -> p a b", b=128)
                ap3 = ap_[:].rearrange("p (a b) -> p a b", b=128)
                d3 = dd[:].rearrange("p (a b) -> p a b", b=128)
                S.pool(lambda e: e.tensor_copy(out=ap_[:], in_=aa[:]), r=["aa"], w=["ap"])
                S.pool(lambda e, ap3=ap3: e.memset(ap3[:, :, 0:1], 0.0), w=["ap"])
                S.pool(lambda e, d3=d3, a3=a3: e.tensor_copy(out=d3[:, :, 0:1], in_=a3[:, :, 0:1]),
                       r=["aa"], w=["dd"])
                hb, pb_ = hlb[ch % 2], ppb[ch % 2]
                S.dve(lambda e, hb=hb: e.tensor_tensor_scan(out=hb[:], data0=ap_[:], data1=bb[:], initial=0.0,
                                                            op0=ALU.mult, op1=ALU.add),
                      r=["ap", "bb"], w=[("hlb", ch % 2)])
                S.dve(lambda e, pb_=pb_: e.tensor_tensor_scan(out=pb_[:], data0=ap_[:], data1=dd[:], initial=0.0,
                                                              op0=ALU.mult, op1=ALU.add),
                      r=["ap", "dd"], w=[("ppb", ch % 2)])
                h3 = hb[:].rearrange("p (a b) -> p a b", b=128)
                p3 = pb_[:].rearrange("p (a b) -> p a b", b=128)
                S.pool(lambda e, ch=ch, p3=p3: e.tensor_copy(out=ab[:, ch, 0, :], in_=p3[:, :, 127]),
                       r=[("ppb", ch % 2)], w=["ab"])
                S.pool(lambda e, ch=ch, h3=h3: e.tensor_copy(out=ab[:, ch, 1, :], in_=h3[:, :, 127]),
                       r=[("hlb", ch % 2)], w=["ab"])
                S.dma(hl_o[:, ch * TL:(ch + 1) * TL], hb[:], r=[("hlb", ch % 2)], w=[("hl_o", ch)])
                S.dma(pp_o[:, ch * TL:(ch + 1) * TL], pb_[:], r=[("ppb", ch % 2)], w=[("pp_o", ch)])
        S.dma(ab_o, ab[:].rearrange("p a b c -> p (a b c)"), r=["ab"], w=["ab_o"])
        keys = ["ab_o"] + [(k, ch) for k in ("gel_o", "hl_o", "pp_o") for ch in range(KC)]
        nc = P.close(keys)
    return nc


def build_R2(final=False):
    P = Prog()
    S = P.S
    x_in = P.inp("x", [TL, D])
    gel_d = P.inp("gel", [128, KC * TL])
    hl_d = P.inp("hloc", [128, KC * TL])
    pp_d = P.inp("pp", [128, KC * TL])
    abg_d = P.inp("abg", [128, KC * 2 * 64])
    sel_d = P.inp("selb", [128, 8 * 64])
    w_out = P.inp("w_out", [D, D])
    g2 = P.inp("g_bc", [128, D])
    w_up = P.inp("w_up", [D, DFF])
    w_down = P.inp("w_down", [DFF, D])
    y = P.out("y", [TL, D])
    with ExitStack() as es:
        c = load_consts(P, es)
        x_res = P.sb(es, "x_res", [128, NT, D], F32)
        load_x(P, x_res, x_in)
        with ExitStack() as es2:
            yT = P.sb(es2, "yT", [128, KC, TL], BF16)
            abg = P.sb(es2, "abg", [128, KC, 2, 64], F32)
            sel = P.sb(es2, "sel", [128, 8, 64], F32)
            hs = P.sb(es2, "hs", [128, KC, 64], F32)
            carry = P.sb(es2, "carry", [128, KC, 8], F32)
            junk = P.sb(es2, "junk", [128, 64], F32)
            S.dma(abg[:], abg_d.rearrange("p (a b c) -> p a b c", b=2, c=64), w=["abg"])
            S.dma(sel[:], sel_d.rearrange("p (a b) -> p a b", b=64), w=["sel"])
            for ch in range(KC):
                S.dve(lambda e, ch=ch: e.tensor_tensor_scan(out=hs[:, ch, :], data0=abg[:, ch, 0, :],
                                                            data1=abg[:, ch, 1, :], initial=0.0,
                                                            op0=ALU.mult, op1=ALU.add),
                      r=["abg"], w=["hs"])
            for ch in range(KC):
                for j in range(NT):
                    S.dve(lambda e, ch=ch, j=j: e.scalar_tensor_tensor(
                        out=junk[:], in0=hs[:, ch, :], scalar=1.0, in1=sel[:, j, :], op0=ALU.mult, op1=ALU.mult,
                        accum_out=carry[:, ch, j:j + 1]), r=["hs", "sel"], w=["junk", "carry"])
            f4 = lambda name: P.sb(es2, name, [128, TL], F32)
            gb = [f4("gb"), f4("gb")]
            hb = [f4("hb"), f4("hb")]
            pb = [f4("pb"), f4("pb")]
            for ch in range(KC):
                q = ch % 2
                S.dma(gb[q][:], gel_d[:, ch * TL:(ch + 1) * TL], w=[("gb", q)])
                S.dma(hb[q][:], hl_d[:, ch * TL:(ch + 1) * TL], w=[("hb", q)])
                S.dma(pb[q][:], pp_d[:, ch * TL:(ch + 1) * TL], w=[("pb", q)])
                for j in range(NT):
                    sl = slice(j * 128, (j + 1) * 128)
                    S.dve(lambda e, q=q, ch=ch, j=j, sl=sl: e.scalar_tensor_tensor(
                        out=hb[q][:, sl], in0=pb[q][:, sl], scalar=carry[:, ch, j:j + 1], in1=hb[q][:, sl],
                        op0=ALU.mult, op1=ALU.add), r=[("pb", q), ("hb", q), "carry"], w=[("hb", q)])
                S.pool(lambda e, q=q, ch=ch: e.tensor_tensor(out=yT[:, ch, :], in0=hb[q][:], in1=gb[q][:],
                                                             op=ALU.mult),
                       r=[("hb", q), ("gb", q)], w=["yT"])
            ws = WStream(P, es2, "wout", [128, 4096], nbuf=3)
            emit_linear_tm_res(P, ws, yT, "yT", KC, w_out, x_res)
            S.barrier()
        emit_mlp(P, c, es, x_res, g2, w_up, w_down)
        keys = store_x(P, y, x_res)
        nc = P.close(keys)
    return nc


def halo_tile(xg, c):
    out = np.zeros((128, xg.shape[1]), np.float32)
    for j in range(NT):
        t0 = (8 * j + c) * 128 - 16
        if t0 >= 0:
            out[j * 16:(j + 1) * 16] = xg[t0:t0 + 16]
    return out


def pk(v):
    return np.ascontiguousarray(np.asarray(v, np.float32).reshape(KC, 128).T)


def r1_inputs(xg, d, j):
    xs = shard_tokens(xg)
    cw = np.stack([pk(d["rnn_conv_w"][j][i]) for i in range(4)], -1).reshape(128, KC * 4)
    vecs = np.stack([pk(d["rnn_conv_b"][j]), pk(d["rnn_gate_a_b"][j]), pk(d["rnn_gate_x_b"][j]),
                     pk(d["rnn_lambda"][j])], -1).reshape(128, KC * 4)
    common = {
        "g_bc": bc128(d["rnn_norm"][j]), "w_in": np.ascontiguousarray(d["rnn_w_in"][j]),
        "cw": np.ascontiguousarray(cw), "vecs": np.ascontiguousarray(vecs),
        "wa": np.ascontiguousarray(d["rnn_gate_a_w"][j].reshape(8 * 256, 256)),
        "wx": np.ascontiguousarray(d["rnn_gate_x_w"][j].reshape(8 * 256, 256)),
        "c_ident": IDENT,
    }
    return [dict(common, x9=np.concatenate([xs[c], halo_tile(xg, c)], 0)) for c in range(NCORES)]


def r2_inputs(xg, r1, d, j, li):
    xs = shard_tokens(xg)
    ab = np.stack([r1[c]["ab"].reshape(128, KC, 2, NT) for c in range(NCORES)], -1)
    abg = np.ascontiguousarray(ab.reshape(128, KC * 2 * 64))
    common = {
        "abg": abg, "w_out": np.ascontiguousarray(d["rnn_w_out"][j]), "g_bc": bc128(d["mlp_norm"][li]),
        "w_up": np.ascontiguousarray(d["mlp_w_up"][li]), "w_down": np.ascontiguousarray(d["mlp_w_down"][li]),
        "c_ident": IDENT,
    }
    ins = []
    for c in range(NCORES):
        sel = np.zeros((NT, 64), np.float32)
        for jj in range(NT):
            b = 8 * jj + c - 1
            if b >= 0:
                sel[jj, b] = 1.0
        selb = np.ascontiguousarray(np.broadcast_to(sel.reshape(1, NT * 64), (128, NT * 64)))
        ins.append(dict(common, x=xs[c], gel=r1[c]["gel"], hloc=r1[c]["hloc"], pp=r1[c]["pp"], selb=selb))
    return ins


TWO_PI = 6.283185307179586
CW1 = 6.28125
CW2 = TWO_PI - CW1
PI = 3.141592653589793


def emit_rope_tables(P, es, posb_d, invf_d):
    S = P.S
    posi = P.sb(es, "posi", [128, TL], I32)
    posf = P.sb(es, "posf", [128, TL], F32)
    invf = P.sb(es, "invf", [128, 2], F32)
    ang = P.sb(es, "ang", [128, TL], F32)
    kf = P.sb(es, "kf", [128, TL], F32)
    ki = P.sb(es, "ki", [128, TL], I32)
    mm = P.sb(es, "mm", [128, TL], F32)
    cosT = P.sb(es, "cosT", [128, 2, TL], F32)
    sinT = P.sb(es, "sinT", [128, 2, TL], F32)
    S.dma(posi[:], posb_d, w=["posi"])
    S.dma(invf[:], invf_d, w=["invf"])
    S.dve(lambda e: e.tensor_copy(out=posf[:], in_=posi[:]), r=["posi"], w=["posf"])

    def wrap(buf, key):
        S.dve(lambda e: e.tensor_single_scalar(out=mm[:], in_=buf, scalar=PI, op=ALU.is_gt), r=[key], w=["mm"])
        S.dve(lambda e: e.scalar_tensor_tensor(out=buf, in0=mm[:], scalar=-TWO_PI, in1=buf, op0=ALU.mult, op1=ALU.add),
              r=["mm", key], w=[key])
        S.dve(lambda e: e.tensor_single_scalar(out=mm[:], in_=buf, scalar=-PI, op=ALU.is_lt), r=[key], w=["mm"])
        S.dve(lambda e: e.scalar_tensor_tensor(out=buf, in0=mm[:], scalar=TWO_PI, in1=buf, op0=ALU.mult, op1=ALU.add),
              r=["mm", key], w=[key])

    for t in range(2):
        S.dve(lambda e, t=t: e.tensor_scalar(ang[:], posf[:], invf[:, t:t + 1], None, ALU.mult),
              r=["posf", "invf"], w=["ang"])
        S.dve(lambda e: e.tensor_scalar(kf[:], ang[:], 1.0 / TWO_PI, None, ALU.mult), r=["ang"], w=["kf"])
        S.dve(lambda e: e.tensor_copy(out=ki[:], in_=kf[:]), r=["kf"], w=["ki"])
        S.dve(lambda e: e.tensor_copy(out=kf[:], in_=ki[:]), r=["ki"], w=["kf"])
        S.dve(lambda e: e.scalar_tensor_tensor(out=ang[:], in0=kf[:], scalar=-CW1, in1=ang[:], op0=ALU.mult, op1=ALU.add),
              r=["kf", "ang"], w=["ang"])
        S.dve(lambda e: e.scalar_tensor_tensor(out=ang[:], in0=kf[:], scalar=-CW2, in1=ang[:], op0=ALU.mult, op1=ALU.add),
              r=["kf", "ang"], w=["ang"])
        wrap(ang[:], "ang")
        S.act(lambda e, t=t: e.activation(out=sinT[:, t, :], in_=ang[:], func=AF.Sin), r=["ang"], w=["sinT"])
        S.dve(lambda e: e.tensor_scalar(ang[:], ang[:], PI / 2, None, ALU.add), r=["ang"], w=["ang"])
        wrap(ang[:], "ang")
        S.act(lambda e, t=t: e.activation(out=cosT[:, t, :], in_=ang[:], func=AF.Sin), r=["ang"], w=["cosT"])
    return cosT, sinT


def emit_attn_inproj(P, c, es, hT, w_in, d_in, d_out):
    S = P.S
    cosT, sinT = emit_rope_tables(P, es, d_in["posb"], d_in["invf"])
    rt = P.sb(es, "rt", [128, 2, 128], BF16)
    S.dma(rt[:, 0, :], d_in["rt"][0:128, :], w=["rt"], q="pool")
    S.dma(rt[:, 1, :], d_in["rt"][128:256, :], w=["rt"], q="pool")
    qkg = P.sb(es, "qkg", [128, 2], F32)
    S.dma(qkg[:], d_in["qkg"], w=["qkg"])
    epsh = P.sb(es, "epsh", [128, 1], F32)
    S.pool(lambda e: e.memset(epsh[:], EPS), w=["epsh"])
    sqb = P.sb(es, "sqb", [128, 512], BF16)
    rsb = P.sb(es, "rsb", [128, 512], F32)
    xnb = P.sb(es, "xnb", [128, 512], BF16)
    t1 = P.sb(es, "t1", [128, 512], F32)
    t2 = P.sb(es, "t2", [128, 512], F32)
    ob = [P.sb(es, "ob", [128, 512], BF16) for _ in range(2)]
    ws = WStream(P, es, "wain", [128, 4096], nbuf=3)

    def head_chunk(wv, bkey, s, kind, dst_of_tc):
        tab = 0 if kind in ("q", "k") else 1
        for tc in range(2):
            pb = P.ps[tc]
            for kc in range(KC):
                S.pe(lambda e, kc=kc, pb=pb: e.matmul(
                    pb[:], lhsT=wv[:, kc, s * 128:(s + 1) * 128], rhs=hT[:, kc, tc * 512:(tc + 1) * 512],
                    start=(kc == 0), stop=(kc == KC - 1)), r=[bkey, "hT"], w=[("ps", tc)])
            tsl = slice(tc * 512, (tc + 1) * 512)
            if kind in ("q", "k"):
                gcol = 0 if kind == "q" else 1
                S.act(lambda e, pb=pb: e.activation(out=sqb[:], in_=pb[:], func=AF.Square), r=[("ps", tc)], w=["sqb"])
                S.pe(lambda e: e.matmul(P.ps[2][:], lhsT=c["ones"][:], rhs=sqb[:], start=True, stop=True),
                     r=["ones", "sqb"], w=[("ps", 2)])
                S.act(lambda e: e.activation(out=rsb[:], in_=P.ps[2][:], func=AF.Sqrt, bias=epsh[:], scale=1.0 / 128),
                      r=[("ps", 2), "epsh"], w=["rsb"])
                S.dve(lambda e: e.reciprocal(out=rsb[:], in_=rsb[:]), r=["rsb"], w=["rsb"])
                S.dve(lambda e, pb=pb, gcol=gcol: e.scalar_tensor_tensor(
                    out=xnb[:], in0=pb[:], scalar=qkg[:, gcol:gcol + 1], in1=rsb[:], op0=ALU.mult, op1=ALU.mult),
                    r=[("ps", tc), "qkg", "rsb"], w=["xnb"])
            else:
                S.act(lambda e, pb=pb: e.copy(out=xnb[:], in_=pb[:]), r=[("ps", tc)], w=["xnb"])
            S.pe(lambda e, tab=tab: e.matmul(P.ps[3][:], lhsT=rt[:, tab, :], rhs=xnb[:], start=True, stop=True),
                 r=["rt", "xnb"], w=[("ps", 3)])
            S.dve(lambda e, tab=tab, tsl=tsl: e.tensor_tensor(out=t1[:], in0=xnb[:], in1=cosT[:, tab, tsl], op=ALU.mult),
                  r=["xnb", "cosT"], w=["t1"])
            S.dve(lambda e, tab=tab, tsl=tsl: e.tensor_tensor(out=t2[:], in0=P.ps[3][:], in1=sinT[:, tab, tsl], op=ALU.mult),
                  r=[("ps", 3), "sinT"], w=["t2"])
            o = ob[tc]
            S.pool(lambda e, o=o: e.tensor_tensor(out=o[:], in0=t1[:], in1=t2[:], op=ALU.add),
                   r=["t1", "t2"], w=[("ob", tc)])
            S.dma(dst_of_tc(tc), o[:], r=[("ob", tc)], w=[("hp_out", P.uid)])
            P.uid += 1

    def piece(col0, ncols):
        def f(buf):
            dst = buf[:, 0:KC * ncols].rearrange("p (k n) -> p k n", n=ncols)
            src = w_in[:, col0:col0 + ncols].rearrange("(k p) n -> p k n", p=128)
            return dst, src
        return f

    groups = [("q", 0, 16, d_out["qT"]), ("k", 2048, 4, d_out["kT"]), ("iq", 3072, 16, d_out["iqT"])]
    pcs = []
    for kind, col0, nch, _ in groups:
        for i in range(nch // 2):
            pcs.append(piece(col0 + i * 256, 256))
    pcs.append(piece(5120, 144))
    ws.plan(pcs)
    for kind, col0, nch, dst in groups:
        for i in range(nch // 2):
            buf, bkey = ws.next()
            wv = buf[:].rearrange("p (k n) -> p k n", n=256)
            for s in range(2):
                hh = 2 * i + s
                head_chunk(wv, bkey, s, kind,
                           lambda tc, hh=hh, dst=dst: dst[:, hh * TL + tc * 512: hh * TL + (tc + 1) * 512])
    buf, bkey = ws.next()
    wv = buf[:, 0:KC * 144].rearrange("p (k n) -> p k n", n=144)
    head_chunk(wv, bkey, 0, "ik", lambda tc: d_out["ikT"][:, tc * 512:(tc + 1) * 512])
    iwsb = P.sb(es, "iwsb", [128, NT, 16], F32)
    for t in range(NT):
        for kc in range(KC):
            S.pe(lambda e, kc=kc, t=t: e.matmul(P.ps[4][:, t * 16:(t + 1) * 16], lhsT=hT[:, kc, t * 128:(t + 1) * 128],
                                                rhs=wv[:, kc, 128:144], start=(kc == 0), stop=(kc == KC - 1)),
                 r=[bkey, "hT"], w=[("ps", 4)])
    S.act(lambda e: e.copy(out=iwsb[:], in_=P.ps[4][:, 0:NT * 16].rearrange("p (a b) -> p a b", b=16)),
          r=[("ps", 4)], w=["iwsb"])
    S.dma(d_out["iw"], iwsb[:].rearrange("p a b -> p (a b)"), r=["iwsb"], w=["iw_out"])
    vsb = [P.sb(es, "vsb", [128, 512], BF16) for _ in range(2)]

    def vpiece(kp):
        def f(buf):
            dst = buf[:].rearrange("p (k n) -> p k n", n=512)
            src = w_in[kp * 1024:(kp + 1) * 1024, 2560:3072].rearrange("(k p) n -> p k n", p=128)
            return dst, src
        return f
    for half in range(2):
        ws.plan([vpiece(0), vpiece(1)])
        for kp in range(2):
            buf, bkey = ws.next()
            wv = buf[:].rearrange("p (k n) -> p k n", n=512)
            for t in range(4):
                j = half * 4 + t
                for k in range(8):
                    kc = kp * 8 + k
                    S.pe(lambda e, t=t, j=j, k=k, kc=kc, wv=wv: e.matmul(
                        P.ps[t][:], lhsT=hT[:, kc, j * 128:(j + 1) * 128], rhs=wv[:, k, :],
                        start=(kc == 0), stop=(kc == KC - 1)), r=[bkey, "hT"], w=[("ps", t)])
        for t in range(4):
            j = half * 4 + t
            S.act(lambda e, t=t: e.copy(out=vsb[t % 2][:], in_=P.ps[t][:]), r=[("ps", t)], w=[("vsb", t % 2)])
            S.dma(d_out["v"][j * 128:(j + 1) * 128, :], vsb[t % 2][:], r=[("vsb", t % 2)], w=[("v_out", j)])
    keys = ["iw_out"] + [("v_out", j) for j in range(NT)]
    return keys


def attn_a1_io(P):
    d_in = {"posb": P.inp("posb", [128, TL], I32), "invf": P.inp("invf", [128, 2]),
            "rt": P.inp("rt", [256, 128]), "qkg": P.inp("qkg", [128, 2])}
    d_out = {"qT": P.out("qT", [128, 16 * TL], BF16), "kT": P.out("kT", [128, 4 * TL], BF16),
             "iqT": P.out("iqT", [128, 16 * TL], BF16), "ikT": P.out("ikT", [128, TL], BF16),
             "iw": P.out("iw", [128, NT * 16]), "v": P.out("v", [TL, 512], BF16)}
    return d_in, d_out


def build_A1():
    P = Prog()
    S = P.S
    x_in = P.inp("x", [TL, D])
    g = P.inp("g_bc", [128, D])
    w_in = P.inp("w_in", [D, A_IN])
    d_in, d_out = attn_a1_io(P)
    with ExitStack() as es:
        c = load_consts(P, es)
        gbc = P.sb(es, "gbc", [128, D], F32)
        S.dma(gbc[:], g, w=["gbc"])
        wk = norm_work(P, es)
        hT = P.sb(es, "hT", [128, KC, TL], BF16)
        xt = [P.sb(es, "xt", [128, D], F32) for _ in range(2)]
        for t in range(NT):
            S.dma(xt[t % 2][:], x_in[t * 128:(t + 1) * 128, :], w=[("xt", t % 2)])
            emit_norm_T(P, c, xt[t % 2][:], ("xt", t % 2), gbc[:], hT, "hT", t * 128, wk)
        keys = emit_attn_inproj(P, c, es, hT, w_in, d_in, d_out)
        S.barrier()
        nc = P.close(keys)
    return nc


def rope_consts():
    inv_h = (1.0 / (10000.0 ** (np.arange(0, 128, 2, dtype=np.float32) / np.float32(128)))).astype(np.float32)
    inv_i = (1.0 / (10000.0 ** (np.arange(0, 64, 2, dtype=np.float32) / np.float32(64)))).astype(np.float32)
    invf = np.zeros((128, 2), np.float32)
    invf[:, 0] = np.concatenate([inv_h, inv_h])
    invf[:64, 1] = np.concatenate([inv_i, inv_i])
    R = np.zeros((128, 128), np.float32)
    for i in range(64):
        R[i, i + 64] = -1.0
        R[i + 64, i] = 1.0
    R2 = np.zeros((128, 128), np.float32)
    for i in range(32):
        R2[i, i + 32] = -1.0
        R2[i + 32, i] = 1.0
    rt = np.concatenate([R.T, R2.T], 0)
    return invf, np.ascontiguousarray(rt)


def a1_inputs(xs, pos, d, j, with_x=True):
    invf, rt = rope_consts()
    ps = shard_tokens(np.asarray(pos).reshape(T))
    qkg = np.ascontiguousarray(np.stack([d["attn_q_norm"][j], d["attn_k_norm"][j]], -1).astype(np.float32))
    ins = []
    for c in range(NCORES):
        m = {"posb": np.ascontiguousarray(np.broadcast_to(ps[c].astype(np.int32).reshape(1, TL), (128, TL))),
             "invf": invf, "rt": rt, "qkg": qkg, "w_in": np.ascontiguousarray(d["attn_w_in"][j]), "c_ident": IDENT}
        if with_x:
            m["x"] = xs[c]
            m["g_bc"] = bc128(d["attn_norm"][j])
        ins.append(m)
    return ins


def emit_attention(P, c, es, d, oT_s):
    S = P.S
    ikT = P.sb(es, "ikT", [128, T], BF16)
    for q in range(4):
        S.dma(ikT[:, q * 2048:(q + 1) * 2048], d["ikTa"][:, q * 2048:(q + 1) * 2048], w=["ikT"])
    iwsb = P.sb(es, "iwsb", [128, NT, 16], F32)
    S.dma(iwsb[:], d["iw"].rearrange("p (a b) -> p a b", b=16), w=["iwsb"])
    cm = P.sb(es, "cm", [128, 1024], F32)
    pen = P.sb(es, "pen", [128, 1024], F32)
    S.dma(cm[:], d["cm"], w=["cm"])
    S.dma(pen[:], d["pen"], w=["pen"])
    score = P.sb(es, "score", [128, T], F32)
    junk = P.sb(es, "junkb", [128, T], BF16)
    maskT = P.sb(es, "maskT", [128, T // 128, 128], BF16)
    mkb = [P.sb(es, "mkb", [128, 512], BF16) for _ in range(2)]
    rl = [P.sb(es, "rl", [128, 512], F32) for _ in range(2)]
    iqtb = [P.sb(es, "iqt", [128, 16, 128], BF16) for _ in range(2)]
    qt = [P.sb(es, "qt", [128, 4, 128], BF16) for _ in range(2)]
    kTb = [P.sb(es, "kTg", [128, T], BF16) for _ in range(2)]
    vgb = [P.sb(es, "vg", [128, T // 128, 128], BF16) for _ in range(2)]
    ptb = [P.sb(es, "ptb", [128, 512], BF16) for _ in range(3)]
    rden = P.sb(es, "rden", [128, 512], F32)
    ot = [P.sb(es, "ot", [128, 512], F32) for _ in range(2)]
    sm = {n: P.sb(es, n, [128, 1], F32) for n in ("M", "lo", "hi", "mid", "cnt", "pred", "dl")}
    iq3 = d["iqT"].rearrange("p (h t) -> p h t", t=TL)
    q3 = d["qT"].rearrange("p (h t) -> p h t", t=TL)
    o3 = oT_s.rearrange("p (h t) -> p h t", t=TL)
    SC = 128.0 ** -0.5
    okeys = []

    def gen_indexer(j):
        Kc = 1024 * (j + 1)
        iqt = iqtb[j % 2]
        S.dma(iqt[:], iq3[:, :, j * 128:(j + 1) * 128], w=[("iqt", j % 2)])
        for k5 in range(Kc // 512):
            sc = score[:, k5 * 512:(k5 + 1) * 512]
            for h in range(16):
                pb = P.ps[h % 2]
                S.pe(lambda e, h=h, pb=pb, k5=k5: e.matmul(pb[:], lhsT=iqt[:, h, :], rhs=ikT[:, k5 * 512:(k5 + 1) * 512],
                                                         start=True, stop=True), r=[("iqt", j % 2), "ikT"], w=[("ps", h % 2)])
                r_ = rl[h % 2]
                S.act(lambda e, pb=pb, r_=r_: e.activation(out=r_[:], in_=pb[:], func=AF.Relu),
                      r=[("ps", h % 2)], w=[("rl", h % 2)])
                if h == 0:
                    S.dve(lambda e, r_=r_, sc=sc: e.tensor_scalar(sc, r_[:], iwsb[:, j, 0:1], None, ALU.mult),
                          r=[("rl", h % 2), "iwsb"], w=["score"])
                else:
                    S.dve(lambda e, r_=r_, sc=sc, h=h: e.scalar_tensor_tensor(
                        out=sc, in0=r_[:], scalar=iwsb[:, j, h:h + 1], in1=sc, op0=ALU.mult, op1=ALU.add),
                        r=[("rl", h % 2), "iwsb", "score"], w=["score"])
                yield

    def post_indexer(j):
        Kc = 1024 * (j + 1)
        S.dve(lambda e: e.tensor_reduce(out=sm["M"][:], in_=score[:, 0:Kc], axis=AX.X, op=ALU.max), r=["score"], w=["M"])
        S.dve(lambda e: e.tensor_reduce(out=sm["cnt"][:], in_=score[:, 0:Kc], axis=AX.X, op=ALU.min), r=["score"], w=["cnt"])
        S.dve(lambda e: e.tensor_tensor(out=sm["dl"][:], in0=sm["M"][:], in1=sm["cnt"][:], op=ALU.subtract),
              r=["M", "cnt"], w=["dl"])
        win = score[:, Kc - 1024:Kc]
        S.dve(lambda e: e.tensor_tensor(out=win, in0=win, in1=cm[:], op=ALU.mult), r=["score", "cm"], w=["score"])
        S.dve(lambda e: e.tensor_tensor(out=win, in0=win, in1=pen[:], op=ALU.add), r=["score", "pen"], w=["score"])
        S.dve(lambda e: e.scalar_tensor_tensor(out=sm["lo"][:], in0=sm["dl"][:], scalar=-0.001, in1=sm["cnt"][:],
                                               op0=ALU.mult, op1=ALU.add), r=["dl", "cnt"], w=["lo"])
        S.dve(lambda e: e.tensor_scalar(sm["lo"][:], sm["lo"][:], -1e-6, None, ALU.add), r=["lo"], w=["lo"])
        S.dve(lambda e: e.tensor_scalar(sm["hi"][:], sm["dl"][:], 1.002, 2e-6, ALU.mult, ALU.add), r=["dl"], w=["hi"])
        for it in range(NBIS):
            S.dve(lambda e: e.tensor_scalar(sm["hi"][:], sm["hi"][:], 0.5, None, ALU.mult), r=["hi"], w=["hi"])
            S.dve(lambda e: e.tensor_tensor(out=sm["mid"][:], in0=sm["lo"][:], in1=sm["hi"][:], op=ALU.add),
                  r=["lo", "hi"], w=["mid"])
            S.dve(lambda e: e.tensor_scalar(junk[:, 0:Kc], score[:, 0:Kc], sm["mid"][:], 0.0, ALU.is_ge, ALU.add,
                                            accum_out=sm["cnt"][:]), r=["score", "mid"], w=["junk", "cnt"])
            S.dve(lambda e: e.tensor_scalar(sm["pred"][:], sm["cnt"][:], float(TOPK), None, ALU.is_ge),
                  r=["cnt"], w=["pred"])
            S.dve(lambda e: e.scalar_tensor_tensor(out=sm["lo"][:], in0=sm["hi"][:], scalar=sm["pred"][:], in1=sm["lo"][:],
                                                   op0=ALU.mult, op1=ALU.add), r=["hi", "pred", "lo"], w=["lo"])
        for k5 in range(Kc // 512):
            mk = mkb[k5 % 2]
            S.dve(lambda e, mk=mk, k5=k5: e.tensor_scalar(mk[:], score[:, k5 * 512:(k5 + 1) * 512], sm["lo"][:], -30000.0,
                                                          ALU.is_lt, ALU.mult), r=["score", "lo"], w=[("mkb", k5 % 2)])
            pbb = P.psb[k5 % 2]
            for i in range(4):
                S.pe(lambda e, mk=mk, i=i, pbb=pbb: e.transpose(out=pbb[:, i * 128:(i + 1) * 128],
                                                                in_=mk[:, i * 128:(i + 1) * 128], identity=c["ident"][:]),
                     r=[("mkb", k5 % 2), "ident"], w=[("psb", k5 % 2)])
            S.act(lambda e, k5=k5, pbb=pbb: e.copy(out=maskT[:, k5 * 4:(k5 + 1) * 4, :],
                                                   in_=pbb[:, 0:512].rearrange("p (a b) -> p a b", b=128)),
                  r=[("psb", k5 % 2)], w=["maskT"])

    def gen_attention(j):
        Kc = 1024 * (j + 1)
        n1 = Kc // 128
        for g in range(4):
            gi = j * 4 + g
            qg = qt[gi % 2]
            kT = kTb[gi % 2]
            vg = vgb[gi % 2]
            kk, vk = ("kTg", gi % 2), ("vg", gi % 2)
            S.dma(qg[:], q3[:, 4 * g:4 * g + 4, j * 128:(j + 1) * 128], w=[("qt", gi % 2)])
            S.dma(kT[:, 0:Kc], d["kTa"][:, g * T:g * T + Kc], w=[kk])
            for k0 in range(0, n1, 16):
                S.dma(vg[:, k0:k0 + 16, :],
                      d["va"][k0 * 128:(k0 + 16) * 128, g * 128:(g + 1) * 128].rearrange("(k p) e -> p k e", p=128),
                      w=[vk])
            qg2 = qg[:].rearrange("p a b -> p (a b)")

            def st(kc, kT=kT, kk=kk, qg2=qg2, gi=gi):
                pb = P.ps[2 + kc % 2]
                S.pe(lambda e, pb=pb: e.matmul(pb[:], lhsT=kT[:, kc * 128:(kc + 1) * 128], rhs=qg2, start=True, stop=False),
                     r=[kk, ("qt", gi % 2)], w=[("ps", 2 + kc % 2)])
                for hh in range(4):
                    S.pe(lambda e, pb=pb, hh=hh: e.matmul(pb[:, hh * 128:(hh + 1) * 128], lhsT=c["ident"][:], rhs=maskT[:, kc, :],
                                                          start=False, stop=(hh == 3)),
                         r=["ident", "maskT"], w=[("ps", 2 + kc % 2)])
            st(0)
            for kc in range(n1):
                if kc + 1 < n1:
                    st(kc + 1)
                pb = P.ps[2 + kc % 2]
                pt = ptb[kc % 3]
                S.act(lambda e, pb=pb, pt=pt: e.activation(out=pt[:], in_=pb[:], func=AF.Exp, scale=SC),
                      r=[("ps", 2 + kc % 2)], w=[("ptb", kc % 3)])
                S.pe(lambda e, kc=kc, pt=pt, vg=vg: e.matmul(P.ps[4][:], lhsT=vg[:, kc, :], rhs=pt[:],
                                                             start=(kc == 0), stop=(kc == n1 - 1)),
                     r=[vk, ("ptb", kc % 3)], w=[("ps", 4)])
                S.pe(lambda e, kc=kc, pt=pt: e.matmul(P.ps[5][:], lhsT=c["ones"][:], rhs=pt[:],
                                                      start=(kc == 0), stop=(kc == n1 - 1)),
                     r=["ones", ("ptb", kc % 3)], w=[("ps", 5)])
                yield
            S.dve(lambda e: e.reciprocal(out=rden[:], in_=P.ps[5][:]), r=[("ps", 5)], w=["rden"])
            o = ot[g % 2]
            S.dve(lambda e, o=o: e.tensor_tensor(out=o[:], in0=P.ps[4][:], in1=rden[:], op=ALU.mult),
                  r=[("ps", 4), "rden"], w=[("ot", g % 2)])
            S.dma(o3[:, 4 * g:4 * g + 4, j * 128:(j + 1) * 128], o[:].rearrange("p (a b) -> p a b", b=128),
                  r=[("ot", g % 2)], w=[("oT_s", j, g)])
            okeys.append(("oT_s", j, g))

    for _ in gen_indexer(0):
        pass
    post_indexer(0)
    for j in range(NT):
        ga = gen_attention(j)
        gx = gen_indexer(j + 1) if j + 1 < NT else iter(())
        da = dx = False
        while not (da and dx):
            if not da:
                try:
                    next(ga)
                except StopIteration:
                    da = True
            if not dx:
                try:
                    next(gx)
                except StopIteration:
                    dx = True
        if j + 1 < NT:
            post_indexer(j + 1)
    return okeys


def build_A2(dbg=False):
    P = Prog()
    S = P.S
    x_in = P.inp("x", [TL, D])
    d = {"qT": P.inp("qT", [128, 16 * TL], BF16), "iqT": P.inp("iqT", [128, 16 * TL], BF16),
         "iw": P.inp("iw", [128, NT * 16]), "kTa": P.inp("kTa", [128, 4 * T], BF16), "va": P.inp("va", [T, 512], BF16),
         "ikTa": P.inp("ikTa", [128, T], BF16),
         "cm": P.inp("cm", [128, 1024]), "pen": P.inp("pen", [128, 1024])}
    w_out = P.inp("w_out", [D, D])
    g2 = P.inp("g_bc", [128, D])
    w_up = P.inp("w_up", [D, DFF])
    w_down = P.inp("w_down", [DFF, D])
    y = P.out("y", [TL, D])
    oT_s = P.out("oT_dbg", [128, 16 * TL]) if dbg else P.scratch("oT_s", [128, 16 * TL])
    with ExitStack() as es:
        c = load_consts(P, es)
        with ExitStack() as es1:
            emit_attention(P, c, es1, d, oT_s)
            S.barrier()
        x_res = P.sb(es, "x_res", [128, NT, D], F32)
        load_x(P, x_res, x_in)
        with ExitStack() as es2:
            oT = P.sb(es2, "oT", [128, KC, TL], BF16)
            S.dma(oT[:], oT_s.rearrange("p (h t) -> p h t", t=TL), w=["oT"], q="pool")
            ws = WStream(P, es2, "wout", [128, 4096], nbuf=3)
            emit_linear_tm_res(P, ws, oT, "oT", KC, w_out, x_res)
            S.barrier()
        emit_mlp(P, c, es, x_res, g2, w_up, w_down)
        keys = store_x(P, y, x_res)
        nc = P.close(keys)
    return nc


def a2_inputs(xs, a1, d, j, li):
    kT = np.stack([a1[c]["kT"].reshape(128, 4, NT, 128) for c in range(NCORES)], 3)
    kTa = np.ascontiguousarray(kT.reshape(128, 4 * T))
    ik = np.stack([a1[c]["ikT"].reshape(128, NT, 128) for c in range(NCORES)], 2)
    ikTa = np.ascontiguousarray(ik.reshape(128, T))
    va = unshard_tokens([a1[c]["v"] for c in range(NCORES)])
    common = {"kTa": kTa, "va": va, "ikTa": ikTa, "w_out": np.ascontiguousarray(d["attn_w_out"][j]),
              "g_bc": bc128(d["mlp_norm"][li]), "w_up": np.ascontiguousarray(d["mlp_w_up"][li]),
              "w_down": np.ascontiguousarray(d["mlp_w_down"][li]), "c_ident": IDENT}
    ins = []
    f = np.arange(1024).reshape(1, 1024)
    p = np.arange(128).reshape(128, 1)
    for c in range(NCORES):
        cm = (f <= 128 * c + p)
        ins.append(dict(common, x=xs[c], qT=a1[c]["qT"], iqT=a1[c]["iqT"], iw=a1[c]["iw"],
                        cm=np.where(cm, np.float32(1.0), np.float32(0.0)).astype(np.float32),
                        pen=np.where(cm, np.float32(0.0), np.float32(-BIG)).astype(np.float32)))
    return ins


POOL_W = (2, 4, 8, 16)


def emit_pool(P, c, es_outer, x_res, hT9, d):
    S = P.S
    with ExitStack() as es:
        hx = P.sb(es, "hx", [128, 8, 144], F32)
        sA = P.sb(es, "sA", [128, 8, 144], F32)
        sB = P.sb(es, "sB", [128, 8, 144], F32)
        yT = P.sb(es, "yTg", [128, 4, TL], BF16)
        rd = P.sb(es, "rd", [128, TL], F32)
        wp = P.sb(es, "wp", [128, 4, 4, 512], BF16)
        bbc = P.sb(es, "bbc", [128, D], F32)
        sbc = P.sb(es, "sbc", [128, D], F32)
        tt = [P.sb(es, "ptt", [128, 512], F32) for _ in range(2)]
        for g in range(4):
            S.dma(wp[:, g], d["pool_w"][g * 512:(g + 1) * 512, :].rearrange("(k p) n -> p k n", p=128), w=["wp"], q="pool")
        S.dma(bbc[:], d["pool_b"], w=["bbc"])
        S.dma(sbc[:], d["pool_s"], w=["sbc"])
        for g in range(4):
            w = POOL_W[g]
            S.dma(rd[:], d["mind"][:, g * TL:(g + 1) * TL], w=["rd"])
            S.dve(lambda e: e.reciprocal(out=rd[:], in_=rd[:]), r=["rd"], w=["rd"])
            rd3 = rd[:].rearrange("p (a b) -> p a b", b=128)
            for ci in range(4):
                ch = 4 * g + ci
                S.act(lambda e, ch=ch: e.copy(out=hx[:, :, 16:144], in_=hT9[:, ch, 0:TL].rearrange("p (a b) -> p a b", b=128)),
                      r=["hT"], w=["hx"])
                S.act(lambda e, ch=ch: e.copy(out=hx[:, :, 0:16], in_=hT9[:, ch, TL:TL + 128].rearrange("p (a b) -> p a b", b=16)),
                      r=["hT"], w=["hx"])
                cur, ck = hx, "hx"
                nxt = [(sA, "sA"), (sB, "sB")]
                step = 1
                k = 0
                while step < w:
                    o, ok = nxt[k % 2]
                    S.dve(lambda e, o=o, cur=cur, step=step: e.tensor_tensor(
                        out=o[:, :, step:144], in0=cur[:, :, step:144], in1=cur[:, :, 0:144 - step], op=ALU.add),
                        r=[ck], w=[ok])
                    cur, ck = o, ok
                    step *= 2
                    k += 1
                S.dve(lambda e, cur=cur, rd3=rd3: e.tensor_tensor(out=cur[:, :, 16:144], in0=cur[:, :, 16:144], in1=rd3,
                                                                 op=ALU.mult), r=[ck, "rd"], w=[ck])
                S.dve(lambda e, cur=cur, ci=ci: e.tensor_tensor(
                    out=yT[:, ci, :].rearrange("p (a b) -> p a b", b=128), in0=cur[:, :, 16:144], in1=hx[:, :, 16:144],
                    op=ALU.subtract), r=[ck, "hx"], w=["yTg"])
            for j in range(NT):
                pb = P.ps[j % 4]
                for kc in range(4):
                    S.pe(lambda e, kc=kc, j=j, pb=pb, g=g: e.matmul(pb[:], lhsT=yT[:, kc, j * 128:(j + 1) * 128],
                                                                   rhs=wp[:, g, kc, :], start=(kc == 0), stop=(kc == 3)),
                         r=["yTg", "wp"], w=[("ps", j % 4)])
                t_ = tt[j % 2]
                gs = slice(g * 512, (g + 1) * 512)
                S.dve(lambda e, t_=t_, pb=pb, gs=gs: e.tensor_tensor(out=t_[:], in0=pb[:], in1=bbc[:, gs], op=ALU.add),
                      r=[("ps", j % 4), "bbc"], w=[("ptt", j % 2)])
                S.pool(lambda e, t_=t_, gs=gs: e.tensor_tensor(out=t_[:], in0=t_[:], in1=sbc[:, gs], op=ALU.mult),
                       r=[("ptt", j % 2), "sbc"], w=[("ptt", j % 2)])
                xs_ = x_res[:, j, gs]
                S.dve(lambda e, t_=t_, xs_=xs_: e.tensor_tensor(out=xs_, in0=xs_, in1=t_[:], op=ALU.add),
                      r=[("ptt", j % 2), ("x", j)], w=[("x", j)])
        S.barrier()


def build_PA1():
    P = Prog()
    S = P.S
    x9 = P.inp("x9", [9 * 128, D])
    gp = P.inp("gp_bc", [128, D])
    dp = {"pool_w": P.inp("pool_w", [D, 512]), "pool_b": P.inp("pool_b", [128, D]), "pool_s": P.inp("pool_s", [128, D]),
          "mind": P.inp("mind", [128, 4 * TL])}
    g2 = P.inp("g_bc", [128, D])
    w_up = P.inp("w_up", [D, DFF])
    w_down = P.inp("w_down", [DFF, D])
    ga = P.inp("ga_bc", [128, D])
    w_in = P.inp("w_in", [D, A_IN])
    d_in, d_out = attn_a1_io(P)
    y = P.out("y", [TL, D])
    with ExitStack() as es:
        c = load_consts(P, es)
        x_res = P.sb(es, "x_res", [128, NT, D], F32)
        load_x(P, x_res, x9)
        with ExitStack() as es1:
            gbc = P.sb(es1, "gbc", [128, D], F32)
            S.dma(gbc[:], gp, w=["gbc"])
            wk = norm_work(P, es1)
            hT9 = P.sb(es1, "hT9", [128, KC, 9 * 128], BF16)
            xt = P.sb(es1, "xt", [128, D], F32)
            S.dma(xt[:], x9[TL:TL + 128, :], w=["xt"])
            for t in range(NT):
                emit_norm_T(P, c, x_res[:, t, :], ("x", t), gbc[:], hT9, "hT", t * 128, wk)
            emit_norm_T(P, c, xt[:], "xt", gbc[:], hT9, "hT", TL, wk)
            emit_pool(P, c, es1, x_res, hT9, dp)
        emit_mlp(P, c, es, x_res, g2, w_up, w_down)
        keys = store_x(P, y, x_res)
        with ExitStack() as es2:
            gbc = P.sb(es2, "gbc", [128, D], F32)
            S.dma(gbc[:], ga, w=["gbc"])
            wk = norm_work(P, es2)
            hT = P.sb(es2, "hT", [128, KC, TL], BF16)
            for t in range(NT):
                emit_norm_T(P, c, x_res[:, t, :], ("x", t), gbc[:], hT, "hT", t * 128, wk)
            keys += emit_attn_inproj(P, c, es2, hT, w_in, d_in, d_out)
            S.barrier()
        nc = P.close(keys)
    return nc


def pa1_inputs(xg, pos, d, li, ja):
    xs = shard_tokens(xg)
    a1 = a1_inputs(xs, pos, d, ja, with_x=False)
    common = {"gp_bc": bc128(d["pool_norm"][0]), "pool_w": np.ascontiguousarray(d["pool_w"][0].reshape(D, 512)),
              "pool_b": bc128(d["pool_b"][0].reshape(-1)), "pool_s": bc128(d["pool_scale"][0]),
              "g_bc": bc128(d["mlp_norm"][li]), "w_up": np.ascontiguousarray(d["mlp_w_up"][li]),
              "w_down": np.ascontiguousarray(d["mlp_w_down"][li]), "ga_bc": bc128(d["attn_norm"][ja])}
    ins = []
    for c in range(NCORES):
        idx = (np.arange(NT).reshape(NT, 1) * 8 + c) * 128 + np.arange(128).reshape(1, 128)
        mind = np.stack([np.minimum(idx + 1, w) for w in POOL_W], 0).reshape(1, 4 * TL).astype(np.float32)
        m = dict(common, **a1[c])
        m["x9"] = np.concatenate([xs[c], halo_tile(xg, c)], 0)
        m["mind"] = np.ascontiguousarray(np.broadcast_to(mind, (128, 4 * TL)))
        ins.append(m)
    return ins


_CACHE = {}


def _prog(name, fn):
    if name not in _CACHE:
        _CACHE[name] = fn()
    return _CACHE[name]


def _run(nc, ins):
    return run_bass_kernel_spmd(nc, ins, core_ids=list(range(NCORES))).results


def kernel(**inp):
    d = {k: np.asarray(v) for k, v in inp.items()}
    xg = np.ascontiguousarray(d["x"].reshape(T, D).astype(np.float32, copy=False))
    pos = d["positions"].reshape(T)
    xs = shard_tokens(xg)
    a1 = _run(_prog("A1", build_A1), a1_inputs(xs, pos, d, 0))
    r = _run(_prog("A2", build_A2), a2_inputs(xs, a1, d, 0, 0))
    xg = unshard_tokens([r[c]["y"] for c in range(NCORES)])
    r1 = _run(_prog("R1", build_R1), r1_inputs(xg, d, 0))
    r = _run(_prog("R2", build_R2), r2_inputs(xg, r1, d, 0, 1))
    xg = unshard_tokens([r[c]["y"] for c in range(NCORES)])
    r = _run(_prog("PA1", build_PA1), pa1_inputs(xg, pos, d, 2, 1))
    xg = unshard_tokens([r[c]["y"] for c in range(NCORES)])
    xs = shard_tokens(xg)
    r = _run(_prog("A2", build_A2), a2_inputs(xs, r, d, 1, 3))
    out = unshard_tokens([r[c]["y"] for c in range(NCORES)])
    return out.reshape(1, T, D).astype(np.float32, copy=False)
```

```python
import numpy as np
from contextlib import ExitStack
import concourse.bass as bass
import concourse.mybir as mybir
from concourse.bass_utils import run_bass_kernel_spmd

F32 = mybir.dt.float32
BF16 = mybir.dt.bfloat16
I32 = mybir.dt.int32
ALU = mybir.AluOpType
AF = mybir.ActivationFunctionType
AX = mybir.AxisListType

NCORES = 8
T = 8192
D = 2048
TL = T // NCORES
NT = TL // 128
KC = D // 128
DFF = 4 * D
EPS = 1e-6
A_IN = 5264
TOPK = 256
NBIS = 21
BIG = 1.0e30


class Sched:
    NDS = 40

    def __init__(self, nc, es):
        self.nc = nc
        self.E = {"pe": nc.tensor, "act": nc.scalar, "dve": nc.vector, "pool": nc.gpsimd, "sp": nc.sync}
        self.csem = {e: es.enter_context(nc.semaphore("c_" + e)) for e in ("pe", "act", "dve", "pool")}
        self.ccnt = {e: 0 for e in self.csem}
        self.dsem = [es.enter_context(nc.semaphore("d%d" % i)) for i in range(self.NDS)]
        self.dcnt = [0] * self.NDS
        self.drr = 0
        self.known = {e: {} for e in self.E}
        self.lastw = {}
        self.readers = {}
        self.nins = 0

    def _wait(self, eng, ev):
        sid, h, val, src, isdma = ev
        if src == "pe" and eng == "pe" and not isdma:
            return
        if self.known[eng].get(sid, 0) >= val:
            return
        self.E[eng].wait_ge(h, val)
        self.known[eng][sid] = val

    def op(self, eng, fn, r=(), w=(), dma=False):
        deps = []
        for k in r:
            if k in self.lastw:
                deps.append(self.lastw[k])
        for k in w:
            if k in self.lastw:
                deps.append(self.lastw[k])
            deps.extend(self.readers.get(k, {}).values())
        for ev in deps:
            self._wait(eng, ev)
        if dma:
            i = self.drr
            self.drr = (self.drr + 1) % self.NDS
            if self.dcnt[i] > 0:
                self._wait(eng, (("d", i), self.dsem[i], self.dcnt[i], eng, True))
        ins = fn(self.E[eng])
        self.nins += 1
        if dma:
            self.dcnt[i] += 16
            ins.then_inc(self.dsem[i], 16)
            ev = (("d", i), self.dsem[i], self.dcnt[i], eng, True)
        else:
            self.ccnt[eng] += 1
            ins.then_inc(self.csem[eng], 1)
            ev = (("c", eng), self.csem[eng], self.ccnt[eng], eng, False)
        for k in w:
            self.lastw[k] = ev
            self.readers[k] = {}
        for k in r:
            self.readers.setdefault(k, {})[ev[0]] = ev
        return ev

    def pe(self, fn, r=(), w=()):
        return self.op("pe", fn, r, w)

    def act(self, fn, r=(), w=()):
        return self.op("act", fn, r, w)

    def dve(self, fn, r=(), w=()):
        return self.op("dve", fn, r, w)

    def pool(self, fn, r=(), w=()):
        return self.op("pool", fn, r, w)

    def dma(self, out, in_, r=(), w=(), q="sp"):
        return self.op(q, lambda e: e.dma_start(out=out, in_=in_), r, w, dma=True)

    def barrier(self):
        evs = []
        for e in self.csem:
            if self.ccnt[e] > 0:
                evs.append((("c", e), self.csem[e], self.ccnt[e], e, False))
        for i in range(self.NDS):
            if self.dcnt[i] > 0:
                evs.append((("d", i), self.dsem[i], self.dcnt[i], "sp", True))
        for eng in self.E:
            for ev in evs:
                if ev[3] == "pe" and eng == "pe" and not ev[4]:
                    continue
                if self.known[eng].get(ev[0], 0) >= ev[2]:
                    continue
                self.E[eng].wait_ge(ev[1], ev[2])
                self.known[eng][ev[0]] = ev[2]
        self.lastw = {}
        self.readers = {}

    def finish(self, keys):
        for k in keys:
            if k in self.lastw:
                self._wait("sp", self.lastw[k])


class Prog:
    def __init__(self):
        self.nc = bass.Bass("TRN2", target_bir_lowering=False)
        self.es = ExitStack()
        self.S = Sched(self.nc, self.es)
        self.ins = {}
        self.outs = {}
        nc = self.nc
        self.ps = [self.es.enter_context(nc.psum_tensor("psf%d" % i, [128, 512], F32)) for i in range(6)]
        self.psb = [self.es.enter_context(nc.psum_tensor("psb%d" % i, [128, 1024], BF16)) for i in range(2)]
        self.uid = 0

    def inp(self, name, shape, dt=F32):
        t = self.nc.dram_tensor(name, list(shape), dt, kind="ExternalInput").ap()
        self.ins[name] = t
        return t

    def out(self, name, shape, dt=F32):
        t = self.nc.dram_tensor(name, list(shape), dt, kind="ExternalOutput").ap()
        self.outs[name] = t
        return t

    def scratch(self, name, shape, dt=F32):
        return self.nc.dram_tensor(name, list(shape), dt).ap()

    def sb(self, es, name, shape, dt=F32):
        self.uid += 1
        return es.enter_context(self.nc.sbuf_tensor("%s_%d" % (name, self.uid), list(shape), dt))

    def close(self, out_keys):
        self.S.finish(out_keys)
        self.es.close()
        return self.nc


def load_consts(P, es):
    S = P.S
    c = {}
    ident_d = P.inp("c_ident", [128, 128])
    c["ident"] = P.sb(es, "ident", [128, 128], BF16)
    c["ones"] = P.sb(es, "ones", [128, 128], BF16)
    S.dma(c["ident"][:], ident_d, w=["ident"], q="pool")
    S.pool(lambda e: e.memset(c["ones"][:], 1.0), w=["ones"])
    return c


def emit_norm_T(P, c, xsrc, xkey, g_bc, hT, hkey, col0, work):
    S = P.S
    junk, ss, rs, hbf = work["junk"], work["ss"], work["rs"], work["hbf"]
    S.dve(lambda e: e.scalar_tensor_tensor(out=junk[:], in0=xsrc, scalar=1.0, in1=xsrc,
                                           op0=ALU.mult, op1=ALU.mult, accum_out=ss[:]),
          r=[xkey], w=["n_junk", "n_ss"])
    S.act(lambda e: e.activation(out=rs[:], in_=ss[:], func=AF.Sqrt, bias=work["eps"][:], scale=1.0 / D),
          r=["n_ss", "n_eps"], w=["n_rs"])
    S.dve(lambda e: e.reciprocal(out=rs[:], in_=rs[:]), r=["n_rs"], w=["n_rs"])
    S.dve(lambda e: e.scalar_tensor_tensor(out=hbf[:], in0=xsrc, scalar=rs[:], in1=g_bc,
                                           op0=ALU.mult, op1=ALU.mult),
          r=[xkey, "n_rs", "gbc"], w=["n_hbf"])
    for q in range(KC // 8):
        pb = P.psb[q % 2]
        for i in range(8):
            kc = q * 8 + i
            S.pe(lambda e, kc=kc, i=i: e.transpose(out=pb[:, i * 128:(i + 1) * 128],
                                                   in_=hbf[:, kc * 128:(kc + 1) * 128],
                                                   identity=c["ident"][:]),
                 r=["n_hbf", "ident"], w=[("psb", q % 2)])
        dst = hT[:, q * 8:(q + 1) * 8, col0:col0 + 128]
        src = pb[:].rearrange("p (a b) -> p a b", b=128)
        if q % 2 == 0:
            S.act(lambda e, dst=dst, src=src: e.copy(out=dst, in_=src), r=[("psb", q % 2)], w=[hkey])
        else:
            S.dve(lambda e, dst=dst, src=src: e.tensor_copy(out=dst, in_=src), r=[("psb", q % 2)], w=[hkey])


def norm_work(P, es):
    w = {
        "junk": P.sb(es, "n_junk", [128, D], BF16),
        "ss": P.sb(es, "n_ss", [128, 1], F32),
        "rs": P.sb(es, "n_rs", [128, 1], F32),
        "hbf": P.sb(es, "n_hbf", [128, D], BF16),
        "eps": P.sb(es, "n_eps", [128, 1], F32),
    }
    P.S.pool(lambda e: e.memset(w["eps"][:], EPS), w=["n_eps"])
    return w


class WStream:
    def __init__(self, P, es, name, shape, nbuf=4):
        self.P = P
        self.name = name
        self.bufs = [P.sb(es, name, shape, BF16) for _ in range(nbuf)]
        self.nbuf = nbuf
        self.pieces = []
        self.issued = 0
        self.used = 0

    def plan(self, pieces):
        self.pieces = list(pieces)
        self.issued = 0
        self.used = 0

    def _issue(self):
        i = self.issued
        b = i % self.nbuf
        dst, src = self.pieces[i](self.bufs[b])
        self.P.S.dma(dst, src, w=[(self.name, b)], q="pool")
        self.issued += 1

    def next(self):
        while self.issued < len(self.pieces) and self.issued < self.used + self.nbuf - 1:
            self._issue()
        if self.issued <= self.used:
            self._issue()
        b = self.used % self.nbuf
        self.used += 1
        return self.bufs[b], (self.name, b)


def emit_mlp(P, c, es_outer, x_res, g_bc_dram, w_up, w_down):
    S = P.S
    HF = DFF // 2
    NOC = HF // 128
    acc = [P.ps[t][:] for t in range(6)] + [P.psb[t][:].bitcast(F32) for t in range(2)]
    akey = [("ps", t) for t in range(6)] + [("psb", t) for t in range(2)]
    with ExitStack() as es:
        gbc = P.sb(es, "gbc", [128, D], F32)
        S.dma(gbc[:], g_bc_dram, w=["gbc"])
        wk = norm_work(P, es)
        hT = P.sb(es, "hT", [128, KC, TL], BF16)
        actT = P.sb(es, "actT", [128, NOC, TL], BF16)
        rl = [P.sb(es, "rl", [128, 512], F32) for _ in range(2)]
        ws = WStream(P, es, "wmlp", [128, 4096], nbuf=3)
        for j in range(NT):
            emit_norm_T(P, c, x_res[:, j, :], ("x", j), gbc[:], hT, "hT", j * 128, wk)
        for fh in range(2):
            def up_piece(i):
                def f(buf):
                    dst = buf[:].rearrange("p (k n) -> p k n", n=256)
                    c0 = fh * HF + i * 256
                    src = w_up[:, c0:c0 + 256].rearrange("(k p) n -> p k n", p=128)
                    return dst, src
                return f
            ws.plan([up_piece(i) for i in range(NOC // 2)])
            cnt = 0
            for i in range(NOC // 2):
                buf, bkey = ws.next()
                wv = buf[:].rearrange("p (k n) -> p k n", n=256)
                for s in range(2):
                    oc = i * 2 + s
                    for tc in range(2):
                        bi = cnt % 4
                        pb = P.ps[bi]
                        for kc in range(KC):
                            S.pe(lambda e, kc=kc, s=s, pb=pb, wv=wv, tc=tc: e.matmul(
                                pb[:], lhsT=wv[:, kc, s * 128:(s + 1) * 128], rhs=hT[:, kc, tc * 512:(tc + 1) * 512],
                                start=(kc == 0), stop=(kc == KC - 1)),
                                r=[bkey, "hT"], w=[("ps", bi)])
                        r_ = rl[cnt % 2]
                        S.act(lambda e, pb=pb, r_=r_: e.activation(out=r_[:], in_=pb[:], func=AF.Relu),
                              r=[("ps", bi)], w=[("rl", cnt % 2)])
                        S.pool(lambda e, oc=oc, r_=r_, tc=tc: e.tensor_tensor(
                            out=actT[:, oc, tc * 512:(tc + 1) * 512], in0=r_[:], in1=r_[:], op=ALU.mult),
                            r=[("rl", cnt % 2)], w=["actT"])
                        cnt += 1
            nkp = NOC // 8

            def dn_piece(cc, kp):
                def f(buf):
                    dst = buf[:].rearrange("p (k n) -> p k n", n=512)
                    r0 = fh * HF + kp * 1024
                    src = w_down[r0:r0 + 1024, cc * 512:(cc + 1) * 512].rearrange("(k p) n -> p k n", p=128)
                    return dst, src
                return f
            ws.plan([dn_piece(cc, kp) for cc in range(4) for kp in range(nkp)])
            for cc in range(4):
                for kp in range(nkp):
                    buf, bkey = ws.next()
                    wv = buf[:].rearrange("p (k n) -> p k n", n=512)
                    for t in range(NT):
                        for k in range(8):
                            kc = kp * 8 + k
                            S.pe(lambda e, t=t, k=k, kc=kc, wv=wv: e.matmul(
                                acc[t], lhsT=actT[:, kc, t * 128:(t + 1) * 128], rhs=wv[:, k, :],
                                start=(kc == 0), stop=(kc == NOC - 1)),
                                r=[bkey, "actT"], w=[akey[t]])
                for t in range(NT):
                    xs = x_res[:, t, cc * 512:(cc + 1) * 512]
                    S.dve(lambda e, xs=xs, t=t: e.tensor_tensor(out=xs, in0=xs, in1=acc[t], op=ALU.add),
                          r=[akey[t], ("x", t)], w=[("x", t)])
        S.barrier()


def build_mlp_only():
    P = Prog()
    S = P.S
    x_in = P.inp("x", [TL, D])
    g = P.inp("g_bc", [128, D])
    w_up = P.inp("w_up", [D, DFF])
    w_down = P.inp("w_down", [DFF, D])
    y = P.out("y", [TL, D])
    with ExitStack() as es:
        c = load_consts(P, es)
        x_res = P.sb(es, "x_res", [128, NT, D], F32)
        for j in range(NT):
            S.dma(x_res[:, j, :], x_in[j * 128:(j + 1) * 128, :], w=[("x", j)])
        emit_mlp(P, c, es, x_res, g, w_up, w_down)
        for j in range(NT):
            S.dma(y[j * 128:(j + 1) * 128, :], x_res[:, j, :], r=[("x", j)], w=[("y", j)])
        nc = P.close([("y", j) for j in range(NT)])
    return nc


def shard_tokens(a):
    b = a.reshape((NT, NCORES, 128) + a.shape[1:])
    return [np.ascontiguousarray(b[:, c].reshape((TL,) + a.shape[1:])) for c in range(NCORES)]


def unshard_tokens(parts):
    a = np.stack([p.reshape((NT, 128) + p.shape[1:]) for p in parts], axis=1)
    return np.ascontiguousarray(a.reshape((T,) + parts[0].shape[1:]))


def bc128(v):
    return np.ascontiguousarray(np.broadcast_to(np.asarray(v, np.float32).reshape(1, -1), (128, v.size)))


IDENT = np.eye(128, dtype=np.float32)


def emit_linear_tm_res(P, ws, AT, atkey, nkc, w_dram, x_res):
    S = P.S
    nkp = nkc // 8
    for half in range(2):
        def piece(cc, kp):
            def f(buf):
                dst = buf[:].rearrange("p (k n) -> p k n", n=512)
                src = w_dram[kp * 1024:(kp + 1) * 1024, cc * 512:(cc + 1) * 512].rearrange(
                    "(k p) n -> p k n", p=128)
                return dst, src
            return f
        ws.plan([piece(cc, kp) for cc in range(4) for kp in range(nkp)])
        for cc in range(4):
            for kp in range(nkp):
                buf, bkey = ws.next()
                wv = buf[:].rearrange("p (k n) -> p k n", n=512)
                for t in range(4):
                    j = half * 4 + t
                    for k in range(8):
                        kc = kp * 8 + k
                        S.pe(lambda e, t=t, j=j, k=k, kc=kc, wv=wv: e.matmul(
                            P.ps[t][:], lhsT=AT[:, kc, j * 128:(j + 1) * 128], rhs=wv[:, k, :],
                            start=(kc == 0), stop=(kc == nkc - 1)),
                            r=[bkey, atkey], w=[("ps", t)])
            for t in range(4):
                j = half * 4 + t
                xs = x_res[:, j, cc * 512:(cc + 1) * 512]
                S.dve(lambda e, xs=xs, t=t: e.tensor_tensor(out=xs, in0=xs, in1=P.ps[t][:], op=ALU.add),
                      r=[("ps", t), ("x", j)], w=[("x", j)])


def load_x(P, x_res, x_in):
    for j in range(NT):
        P.S.dma(x_res[:, j, :], x_in[j * 128:(j + 1) * 128, :], w=[("x", j)])


def store_x(P, y, x_res):
    for j in range(NT):
        P.S.dma(y[j * 128:(j + 1) * 128, :], x_res[:, j, :], r=[("x", j)], w=[("y", j)])
    return [("y", j) for j in range(NT)]


GELU_C = 0.044715
GELU_S = 1.5957691216057308


def build_R1(stage=99):
    P = Prog()
    S = P.S
    x9 = P.inp("x9", [9 * 128, D])
    g = P.inp("g_bc", [128, D])
    w_in = P.inp("w_in", [D, 2 * D])
    cw_d = P.inp("cw", [128, KC * 4])
    vec_d = P.inp("vecs", [128, KC * 4])
    wa_d = P.inp("wa", [8 * 256, 256])
    wx_d = P.inp("wx", [8 * 256, 256])
    gel_o = P.out("gel", [128, KC * TL])
    hl_o = P.out("hloc", [128, KC * TL])
    pp_o = P.out("pp", [128, KC * TL])
    ab_o = P.out("ab", [128, KC * 16])
    with ExitStack() as es:
        c = load_consts(P, es)
        gbc = P.sb(es, "gbc", [128, D], F32)
        S.dma(gbc[:], g, w=["gbc"])
        wk = norm_work(P, es)
        hT = P.sb(es, "hT9", [128, KC, 9 * 128], BF16)
        xt = [P.sb(es, "xt", [128, D], F32) for _ in range(2)]
        for t in range(9):
            S.dma(xt[t % 2][:], x9[t * 128:(t + 1) * 128, :], w=[("xt", t % 2)])
            emit_norm_T(P, c, xt[t % 2][:], ("xt", t % 2), gbc[:], hT, "hT", t * 128, wk)
        cw = P.sb(es, "cw", [128, KC, 4], F32)
        vec = P.sb(es, "vec", [128, KC, 4], F32)
        S.dma(cw[:], cw_d.rearrange("p (k n) -> p k n", n=4), w=["cw"])
        S.dma(vec[:], vec_d.rearrange("p (k n) -> p k n", n=4), w=["vec"])
        wa = P.sb(es, "wa", [128, 8, 2, 256], BF16)
        wx = P.sb(es, "wx", [128, 8, 2, 256], BF16)
        for n in range(8):
            S.dma(wa[:, n], wa_d[n * 256:(n + 1) * 256, :].rearrange("(k p) c -> p k c", p=128), w=["wa"], q="pool")
            S.dma(wx[:, n], wx_d[n * 256:(n + 1) * 256, :].rearrange("(k p) c -> p k c", p=128), w=["wx"], q="pool")
        one = P.sb(es, "one", [128, 1], F32)
        S.pool(lambda e: e.memset(one[:], 1.0), w=["one"])
        cl = P.sb(es, "cl", [128, KC], F32)
        S.act(lambda e: e.activation(out=cl[:], in_=vec[:, :, 3], func=AF.Exp, scale=-1.0), r=["vec"], w=["cl"])
        S.act(lambda e: e.activation(out=cl[:], in_=cl[:], func=AF.Ln, bias=one[:], scale=1.0),
              r=["cl", "one"], w=["cl"])
        S.dve(lambda e: e.tensor_scalar(cl[:], cl[:], -8.0, None, ALU.mult), r=["cl"], w=["cl"])
        if stage == 1:
            return P.close([])
        ws = WStream(P, es, "wrin", [128, 4096], nbuf=3)
        f4 = lambda name: P.sb(es, name, [128, TL], F32)
        gelb = [f4("gelb"), f4("gelb")]
        hlb = [f4("hlb"), f4("hlb")]
        ppb = [f4("ppb"), f4("ppb")]
        rr, ii, aa, a2, bb, ap_, dd = f4("rr"), f4("ii"), f4("aa"), f4("a2"), f4("bb"), f4("ap"), f4("dd")
        t1 = [P.sb(es, "t1", [128, 512], F32) for _ in range(2)]
        xrx = P.sb(es, "xrx", [128, 2, 8, 131], F32)
        xc = P.sb(es, "xc", [128, 2, TL], F32)
        xcb = P.sb(es, "xcb", [128, 2, TL], BF16)
        ab = P.sb(es, "ab", [128, KC, 2, 8], F32)
        S.pool(lambda e: e.memset(dd[:], 0.0), w=["dd"])

        def piece(col0):
            def f(buf):
                dst = buf[:].rearrange("p (k n) -> p k n", n=256)
                src = w_in[:, col0:col0 + 256].rearrange("(k p) n -> p k n", p=128)
                return dst, src
            return f
        pcs = []
        for n in range(8):
            pcs.append(piece(n * 256))
            pcs.append(piece(D + n * 256))
        ws.plan(pcs)
        for n in range(8):
            buf, bkey = ws.next()
            wv = buf[:].rearrange("p (k n) -> p k n", n=256)
            for s in range(2):
                ch = 2 * n + s
                gb = gelb[ch % 2]
                for tc in range(2):
                    pb = P.ps[tc]
                    for kc in range(KC):
                        S.pe(lambda e, kc=kc, s=s, tc=tc, pb=pb, wv=wv: e.matmul(
                            pb[:], lhsT=wv[:, kc, s * 128:(s + 1) * 128], rhs=hT[:, kc, tc * 512:(tc + 1) * 512],
                            start=(kc == 0), stop=(kc == KC - 1)), r=[bkey, "hT"], w=[("ps", tc)])
                    tt = t1[tc]
                    S.act(lambda e, tt=tt, pb=pb: e.activation(out=tt[:], in_=pb[:], func=AF.Square),
                          r=[("ps", tc)], w=[("t1", tc)])
                    S.dve(lambda e, tt=tt: e.tensor_scalar(tt[:], tt[:], GELU_C, 1.0, ALU.mult, ALU.add),
                          r=[("t1", tc)], w=[("t1", tc)])
                    S.dve(lambda e, tt=tt, pb=pb: e.tensor_tensor(out=tt[:], in0=tt[:], in1=pb[:], op=ALU.mult),
                          r=[("t1", tc), ("ps", tc)], w=[("t1", tc)])
                    S.act(lambda e, tt=tt: e.activation(out=tt[:], in_=tt[:], func=AF.Sigmoid, scale=GELU_S),
                          r=[("t1", tc)], w=[("t1", tc)])
                    gs = gb[:, tc * 512:(tc + 1) * 512]
                    S.dve(lambda e, tt=tt, pb=pb, gs=gs: e.tensor_tensor(out=gs, in0=tt[:], in1=pb[:], op=ALU.mult),
                          r=[("t1", tc), ("ps", tc)], w=[("gelb", ch % 2)])
                S.dma(gel_o[:, ch * TL:(ch + 1) * TL], gb[:], r=[("gelb", ch % 2)], w=[("gel_o", ch)])
            if stage == 2:
                return P.close([("gel_o", 0), ("gel_o", 1)])
            buf, bkey = ws.next()
            wv = buf[:].rearrange("p (k n) -> p k n", n=256)
            for s in range(2):
                ch = 2 * n + s
                for tc in range(3):
                    pb = P.ps[2 + tc]
                    rhs_of = (lambda kc, tc=tc: hT[:, kc, tc * 512:(tc + 1) * 512]) if tc < 2 else \
                        (lambda kc: hT[:, kc, 1024:1152])
                    po = pb[:] if tc < 2 else pb[:, 0:128]
                    for kc in range(KC):
                        S.pe(lambda e, kc=kc, s=s, po=po, wv=wv, rhs_of=rhs_of: e.matmul(
                            po, lhsT=wv[:, kc, s * 128:(s + 1) * 128], rhs=rhs_of(kc),
                            start=(kc == 0), stop=(kc == KC - 1)), r=[bkey, "hT"], w=[("ps", 2 + tc)])
                    if tc < 2:
                        S.act(lambda e, s=s, tc=tc, pb=pb: e.copy(
                            out=xrx[:, s, tc * 4:(tc + 1) * 4, 3:131],
                            in_=pb[:].rearrange("p (a b) -> p a b", b=128)),
                            r=[("ps", 2 + tc)], w=["xrx"])
                    else:
                        S.act(lambda e, s=s, pb=pb: e.copy(
                            out=xrx[:, s, :, 0:3],
                            in_=pb[:, 0:128].rearrange("p (a b) -> p a b", b=16)[:, :, 13:16]),
                            r=[("ps", 2 + tc)], w=["xrx"])
                xcv = xc[:, s, :].rearrange("p (a b) -> p a b", b=128)
                S.dve(lambda e, s=s, ch=ch, xcv=xcv: e.tensor_scalar(
                    xcv, xrx[:, s, :, 0:128], cw[:, ch, 0:1], vec[:, ch, 0:1], ALU.mult, ALU.add),
                    r=["xrx", "cw", "vec"], w=["xc"])
                for i in range(1, 4):
                    S.dve(lambda e, s=s, ch=ch, i=i, xcv=xcv: e.scalar_tensor_tensor(
                        out=xcv, in0=xrx[:, s, :, i:i + 128], scalar=cw[:, ch, i:i + 1], in1=xcv,
                        op0=ALU.mult, op1=ALU.add), r=["xrx", "cw", "xc"], w=["xc"])
                S.pool(lambda e, s=s: e.tensor_copy(out=xcb[:, s, :], in_=xc[:, s, :]), r=["xc"], w=["xcb"])
            if stage == 3:
                return P.close([("gel_o", 0), ("gel_o", 1)])
            for s in range(2):
                ch = 2 * n + s
                for (wg, dst, bcol, pbase, nm) in ((wa, rr, 1, 0, "rr"), (wx, ii, 2, 2, "ii")):
                    for tc in range(2):
                        pb = P.ps[pbase + tc]
                        for k in range(2):
                            S.pe(lambda e, k=k, s=s, tc=tc, pb=pb, wg=wg: e.matmul(
                                pb[:], lhsT=wg[:, n, k, s * 128:(s + 1) * 128], rhs=xcb[:, k, tc * 512:(tc + 1) * 512],
                                start=(k == 0), stop=(k == 1)), r=["wa", "wx", "xcb"], w=[("ps", pbase + tc)])
                        S.act(lambda e, tc=tc, pb=pb, dst=dst, bcol=bcol, ch=ch: e.activation(
                            out=dst[:, tc * 512:(tc + 1) * 512], in_=pb[:], func=AF.Sigmoid,
                            bias=vec[:, ch, bcol:bcol + 1], scale=1.0), r=[("ps", pbase + tc), "vec"], w=[nm])
                S.act(lambda e, ch=ch: e.activation(out=aa[:], in_=rr[:], func=AF.Exp, scale=cl[:, ch:ch + 1]),
                      r=["rr", "cl"], w=["aa"])
                S.pool(lambda e: e.tensor_tensor(out=a2[:], in0=aa[:], in1=aa[:], op=ALU.mult), r=["aa"], w=["a2"])
                S.act(lambda e: e.activation(out=a2[:], in_=a2[:], func=AF.Sqrt, bias=one[:], scale=-1.0),
                      r=["a2", "one"], w=["a2"])
                S.dve(lambda e, s=s: e.tensor_tensor(out=bb[:], in0=xc[:, s, :], in1=ii[:], op=ALU.mult),
                      r=["xc", "ii"], w=["bb"])
                S.dve(lambda e: e.tensor_tensor(out=bb[:], in0=bb[:], in1=a2[:], op=ALU.mult),
                      r=["bb", "a2"], w=["bb"])
                a3 = aa[:].rearrange("p (a b) -> p a b", b=128)
                ap3 = ap_[:].rearrange("p (a b) -> p a b", b=128)
                d3 = dd[:].rearrange("p (a b) -> p a b", b=128)
                S.pool(lambda e: e.tensor_copy(out=ap_[:], in_=aa[:]), r=["aa"], w=["ap"])
                S.pool(lambda e, ap3=ap3: e.memset(ap3[:, :, 0:1], 0.0), w=["ap"])
                S.pool(lambda e, d3=d3, a3=a3: e.tensor_copy(out=d3[:, :, 0:1], in_=a3[:, :, 0:1]),
                       r=["aa"], w=["dd"])
                hb, pb_ = hlb[ch % 2], ppb[ch % 2]
                S.dve(lambda e, hb=hb: e.tensor_tensor_scan(out=hb[:], data0=ap_[:], data1=bb[:], initial=0.0,
                                                            op0=ALU.mult, op1=ALU.add),
                      r=["ap", "bb"], w=[("hlb", ch % 2)])
                S.dve(lambda e, pb_=pb_: e.tensor_tensor_scan(out=pb_[:], data0=ap_[:], data1=dd[:], initial=0.0,
                                                              op0=ALU.mult, op1=ALU.add),
                      r=["ap", "dd"], w=[("ppb", ch % 2)])
                h3 = hb[:].rearrange("p (a b) -> p a b", b=128)
                p3 = pb_[:].rearrange("p (a b) -> p a b", b=128)
                S.pool(lambda e, ch=ch, p3=p3: e.tensor_copy(out=ab[:, ch, 0, :], in_=p3[:, :, 127]),
                       r=[("ppb", ch % 2)], w=["ab"])
                S.pool(lambda e, ch=ch, h3=h3: e.tensor_copy(out=ab[:, ch, 1, :], in_=h3[:, :, 127]),
                       r=[("hlb", ch % 2)], w=["ab"])
                S.dma(hl_o[:, ch * TL:(ch + 1) * TL], hb[:], r=[("hlb", ch % 2)], w=[("hl_o", ch)])
                S.dma(pp_o[:, ch * TL:(ch + 1) * TL], pb_[:], r=[("ppb", ch % 2)], w=[("pp_o", ch)])
        S.dma(ab_o, ab[:].rearrange("p a b c -> p (a b c)"), r=["ab"], w=["ab_o"])
        keys = ["ab_o"] + [(k, ch) for k in ("gel_o", "hl_o", "pp_o") for ch in range(KC)]
        nc = P.close(keys)
    return nc


def build_R2(final=False):
    P = Prog()
    S = P.S
    x_in = P.inp("x", [TL, D])
    gel_d = P.inp("gel", [128, KC * TL])
    hl_d = P.inp("hloc", [128, KC * TL])
    pp_d = P.inp("pp", [128, KC * TL])
    abg_d = P.inp("abg", [128, KC * 2 * 64])
    sel_d = P.inp("selb", [128, 8 * 64])
    w_out = P.inp("w_out", [D, D])
    g2 = P.inp("g_bc", [128, D])
    w_up = P.inp("w_up", [D, DFF])
    w_down = P.inp("w_down", [DFF, D])
    y = P.out("y", [TL, D])
    with ExitStack() as es:
        c = load_consts(P, es)
        x_res = P.sb(es, "x_res", [128, NT, D], F32)
        load_x(P, x_res, x_in)
        with ExitStack() as es2:
            yT = P.sb(es2, "yT", [128, KC, TL], BF16)
            abg = P.sb(es2, "abg", [128, KC, 2, 64], F32)
            sel = P.sb(es2, "sel", [128, 8, 64], F32)
            hs = P.sb(es2, "hs", [128, KC, 64], F32)
            carry = P.sb(es2, "carry", [128, KC, 8], F32)
            junk = P.sb(es2, "junk", [128, 64], F32)
            S.dma(abg[:], abg_d.rearrange("p (a b c) -> p a b c", b=2, c=64), w=["abg"])
            S.dma(sel[:], sel_d.rearrange("p (a b) -> p a b", b=64), w=["sel"])
            for ch in range(KC):
                S.dve(lambda e, ch=ch: e.tensor_tensor_scan(out=hs[:, ch, :], data0=abg[:, ch, 0, :],
                                                            data1=abg[:, ch, 1, :], initial=0.0,
                                                            op0=ALU.mult, op1=ALU.add),
                      r=["abg"], w=["hs"])
            for ch in range(KC):
                for j in range(NT):
                    S.dve(lambda e, ch=ch, j=j: e.scalar_tensor_tensor(
                        out=junk[:], in0=hs[:, ch, :], scalar=1.0, in1=sel[:, j, :], op0=ALU.mult, op1=ALU.mult,
                        accum_out=carry[:, ch, j:j + 1]), r=["hs", "sel"], w=["junk", "carry"])
            f4 = lambda name: P.sb(es2, name, [128, TL], F32)
            gb = [f4("gb"), f4("gb")]
            hb = [f4("hb"), f4("hb")]
            pb = [f4("pb"), f4("pb")]
            for ch in range(KC):
                q = ch % 2
                S.dma(gb[q][:], gel_d[:, ch * TL:(ch + 1) * TL], w=[("gb", q)])
                S.dma(hb[q][:], hl_d[:, ch * TL:(ch + 1) * TL], w=[("hb", q)])
                S.dma(pb[q][:], pp_d[:, ch * TL:(ch + 1) * TL], w=[("pb", q)])
                for j in range(NT):
                    sl = slice(j * 128, (j + 1) * 128)
                    S.dve(lambda e, q=q, ch=ch, j=j, sl=sl: e.scalar_tensor_tensor(
                        out=hb[q][:, sl], in0=pb[q][:, sl], scalar=carry[:, ch, j:j + 1], in1=hb[q][:, sl],
                        op0=ALU.mult, op1=ALU.add), r=[("pb", q), ("hb", q), "carry"], w=[("hb", q)])
                S.pool(lambda e, q=q, ch=ch: e.tensor_tensor(out=yT[:, ch, :], in0=hb[q][:], in1=gb[q][:],
                                                             op=ALU.mult),
                       r=[("hb", q), ("gb", q)], w=["yT"])
            ws = WStream(P, es2, "wout", [128, 4096], nbuf=3)
            emit_linear_tm_res(P, ws, yT, "yT", KC, w_out, x_res)
            S.barrier()
        emit_mlp(P, c, es, x_res, g2, w_up, w_down)
        keys = store_x(P, y, x_res)
        nc = P.close(keys)
    return nc


def halo_tile(xg, c):
    out = np.zeros((128, xg.shape[1]), np.float32)
    for j in range(NT):
        t0 = (8 * j + c) * 128 - 16
        if t0 >= 0:
            out[j * 16:(j + 1) * 16] = xg[t0:t0 + 16]
    return out


def pk(v):
    return np.ascontiguousarray(np.asarray(v, np.float32).reshape(KC, 128).T)


def r1_inputs(xg, d, j):
    xs = shard_tokens(xg)
    cw = np.stack([pk(d["rnn_conv_w"][j][i]) for i in range(4)], -1).reshape(128, KC * 4)
    vecs = np.stack([pk(d["rnn_conv_b"][j]), pk(d["rnn_gate_a_b"][j]), pk(d["rnn_gate_x_b"][j]),
                     pk(d["rnn_lambda"][j])], -1).reshape(128, KC * 4)
    common = {
        "g_bc": bc128(d["rnn_norm"][j]), "w_in": np.ascontiguousarray(d["rnn_w_in"][j]),
        "cw": np.ascontiguousarray(cw), "vecs": np.ascontiguousarray(vecs),
        "wa": np.ascontiguousarray(d["rnn_gate_a_w"][j].reshape(8 * 256, 256)),
        "wx": np.ascontiguousarray(d["rnn_gate_x_w"][j].reshape(8 * 256, 256)),
        "c_ident": IDENT,
    }
    return [dict(common, x9=np.concatenate([xs[c], halo_tile(xg, c)], 0)) for c in range(NCORES)]


def r2_inputs(xg, r1, d, j, li):
    xs = shard_tokens(xg)
    ab = np.stack([r1[c]["ab"].reshape(128, KC, 2, NT) for c in range(NCORES)], -1)
    abg = np.ascontiguousarray(ab.reshape(128, KC * 2 * 64))
    common = {
        "abg": abg, "w_out": np.ascontiguousarray(d["rnn_w_out"][j]), "g_bc": bc128(d["mlp_norm"][li]),
        "w_up": np.ascontiguousarray(d["mlp_w_up"][li]), "w_down": np.ascontiguousarray(d["mlp_w_down"][li]),
        "c_ident": IDENT,
    }
    ins = []
    for c in range(NCORES):
        sel = np.zeros((NT, 64), np.float32)
        for jj in range(NT):
            b = 8 * jj + c - 1
            if b >= 0:
                sel[jj, b] = 1.0
        selb = np.ascontiguousarray(np.broadcast_to(sel.reshape(1, NT * 64), (128, NT * 64)))
        ins.append(dict(common, x=xs[c], gel=r1[c]["gel"], hloc=r1[c]["hloc"], pp=r1[c]["pp"], selb=selb))
    return ins


TWO_PI = 6.283185307179586
CW1 = 6.28125
CW2 = TWO_PI - CW1
PI = 3.141592653589793


def emit_rope_tables(P, es, posb_d, invf_d):
    S = P.S
    posi = P.sb(es, "posi", [128, TL], I32)
    posf = P.sb(es, "posf", [128, TL], F32)
    invf = P.sb(es, "invf", [128, 2], F32)
    ang = P.sb(es, "ang", [128, TL], F32)
    kf = P.sb(es, "kf", [128, TL], F32)
    ki = P.sb(es, "ki", [128, TL], I32)
    mm = P.sb(es, "mm", [128, TL], F32)
    cosT = P.sb(es, "cosT", [128, 2, TL], F32)
    sinT = P.sb(es, "sinT", [128, 2, TL], F32)
    S.dma(posi[:], posb_d, w=["posi"])
    S.dma(invf[:], invf_d, w=["invf"])
    S.dve(lambda e: e.tensor_copy(out=posf[:], in_=posi[:]), r=["posi"], w=["posf"])

    def wrap(buf, key):
        S.dve(lambda e: e.tensor_single_scalar(out=mm[:], in_=buf, scalar=PI, op=ALU.is_gt), r=[key], w=["mm"])
        S.dve(lambda e: e.scalar_tensor_tensor(out=buf, in0=mm[:], scalar=-TWO_PI, in1=buf, op0=ALU.mult, op1=ALU.add),
              r=["mm", key], w=[key])
        S.dve(lambda e: e.tensor_single_scalar(out=mm[:], in_=buf, scalar=-PI, op=ALU.is_lt), r=[key], w=["mm"])
        S.dve(lambda e: e.scalar_tensor_tensor(out=buf, in0=mm[:], scalar=TWO_PI, in1=buf, op0=ALU.mult, op1=ALU.add),
              r=["mm", key], w=[key])

    for t in range(2):
        S.dve(lambda e, t=t: e.tensor_scalar(ang[:], posf[:], invf[:, t:t + 1], None, ALU.mult),
              r=["posf", "invf"], w=["ang"])
        S.dve(lambda e: e.tensor_scalar(kf[:], ang[:], 1.0 / TWO_PI, None, ALU.mult), r=["ang"], w=["kf"])
        S.dve(lambda e: e.tensor_copy(out=ki[:], in_=kf[:]), r=["kf"], w=["ki"])
        S.dve(lambda e: e.tensor_copy(out=kf[:], in_=ki[:]), r=["ki"], w=["kf"])
        S.dve(lambda e: e.scalar_tensor_tensor(out=ang[:], in0=kf[:], scalar=-CW1, in1=ang[:], op0=ALU.mult, op1=ALU.add),
              r=["kf", "ang"], w=["ang"])
        S.dve(lambda e: e.scalar_tensor_tensor(out=ang[:], in0=kf[:], scalar=-CW2, in1=ang[:], op0=ALU.mult, op1=ALU.add),
              r=["kf", "ang"], w=["ang"])
        wrap(ang[:], "ang")
        S.act(lambda e, t=t: e.activation(out=sinT[:, t, :], in_=ang[:], func=AF.Sin), r=["ang"], w=["sinT"])
        S.dve(lambda e: e.tensor_scalar(ang[:], ang[:], PI / 2, None, ALU.add), r=["ang"], w=["ang"])
        wrap(ang[:], "ang")
        S.act(lambda e, t=t: e.activation(out=cosT[:, t, :], in_=ang[:], func=AF.Sin), r=["ang"], w=["cosT"])
    return cosT, sinT


def emit_attn_inproj(P, c, es, hT, w_in, d_in, d_out):
    S = P.S
    cosT, sinT = emit_rope_tables(P, es, d_in["posb"], d_in["invf"])
    rt = P.sb(es, "rt", [128, 2, 128], BF16)
    S.dma(rt[:, 0, :], d_in["rt"][0:128, :], w=["rt"], q="pool")
    S.dma(rt[:, 1, :], d_in["rt"][128:256, :], w=["rt"], q="pool")
    qkg = P.sb(es, "qkg", [128, 2], F32)
    S.dma(qkg[:], d_in["qkg"], w=["qkg"])
    epsh = P.sb(es, "epsh", [128, 1], F32)
    S.pool(lambda e: e.memset(epsh[:], EPS), w=["epsh"])
    sqb2 = [P.sb(es, "sqb", [128, 512], BF16) for _ in range(2)]
    rsb2 = [P.sb(es, "rsb", [128, 512], F32) for _ in range(2)]
    xnb2 = [P.sb(es, "xnb", [128, 512], BF16) for _ in range(2)]
    t12 = [P.sb(es, "t1", [128, 512], F32) for _ in range(2)]
    t22 = [P.sb(es, "t2", [128, 512], F32) for _ in range(2)]
    ob = [P.sb(es, "ob", [128, 512], BF16) for _ in range(2)]
    ws = WStream(P, es, "wain", [128, 4096], nbuf=3)

    def head_chunk(wv, bkey, s, kind, dst_of_tc):
        tab = 0 if kind in ("q", "k") else 1
        for tc in range(2):
            pb = P.ps[tc]
            sqb, rsb, xnb, t1, t2 = sqb2[tc], rsb2[tc], xnb2[tc], t12[tc], t22[tc]
            ksq, krs, kxn, kt1, kt2 = ("sqb", tc), ("rsb", tc), ("xnb", tc), ("t1", tc), ("t2", tc)
            pss, psr = P.ps[2 + tc], P.ps[4 + tc]
            kss, ksr = ("ps", 2 + tc), ("ps", 4 + tc)
            for kc in range(KC):
                S.pe(lambda e, kc=kc, pb=pb: e.matmul(
                    pb[:], lhsT=wv[:, kc, s * 128:(s + 1) * 128], rhs=hT[:, kc, tc * 512:(tc + 1) * 512],
                    start=(kc == 0), stop=(kc == KC - 1)), r=[bkey, "hT"], w=[("ps", tc)])
            tsl = slice(tc * 512, (tc + 1) * 512)
            if kind in ("q", "k"):
                gcol = 0 if kind == "q" else 1
                S.act(lambda e, pb=pb, sqb=sqb: e.activation(out=sqb[:], in_=pb[:], func=AF.Square), r=[("ps", tc)], w=[ksq])
                S.pe(lambda e, sqb=sqb, pss=pss: e.matmul(pss[:], lhsT=c["ones"][:], rhs=sqb[:], start=True, stop=True),
                     r=["ones", ksq], w=[kss])
                S.act(lambda e, rsb=rsb, pss=pss: e.activation(out=rsb[:], in_=pss[:], func=AF.Sqrt, bias=epsh[:], scale=1.0 / 128),
                      r=[kss, "epsh"], w=[krs])
                S.dve(lambda e, rsb=rsb: e.reciprocal(out=rsb[:], in_=rsb[:]), r=[krs], w=[krs])
                S.dve(lambda e, pb=pb, gcol=gcol, xnb=xnb, rsb=rsb: e.scalar_tensor_tensor(
                    out=xnb[:], in0=pb[:], scalar=qkg[:, gcol:gcol + 1], in1=rsb[:], op0=ALU.mult, op1=ALU.mult),
                    r=[("ps", tc), "qkg", krs], w=[kxn])
            else:
                S.act(lambda e, pb=pb, xnb=xnb: e.copy(out=xnb[:], in_=pb[:]), r=[("ps", tc)], w=[kxn])
            S.pe(lambda e, tab=tab, xnb=xnb, psr=psr: e.matmul(psr[:], lhsT=rt[:, tab, :], rhs=xnb[:], start=True, stop=True),
                 r=["rt", kxn], w=[ksr])
            S.dve(lambda e, tab=tab, tsl=tsl, t1=t1, xnb=xnb: e.tensor_tensor(out=t1[:], in0=xnb[:], in1=cosT[:, tab, tsl], op=ALU.mult),
                  r=[kxn, "cosT"], w=[kt1])
            S.dve(lambda e, tab=tab, tsl=tsl, t2=t2, psr=psr: e.tensor_tensor(out=t2[:], in0=psr[:], in1=sinT[:, tab, tsl], op=ALU.mult),
                  r=[ksr, "sinT"], w=[kt2])
            o = ob[tc]
            S.pool(lambda e, o=o, t1=t1, t2=t2: e.tensor_tensor(out=o[:], in0=t1[:], in1=t2[:], op=ALU.add),
                   r=[kt1, kt2], w=[("ob", tc)])
            S.dma(dst_of_tc(tc), o[:], r=[("ob", tc)], w=[("hp_out", P.uid)])
            P.uid += 1

    def piece(col0, ncols):
        def f(buf):
            dst = buf[:, 0:KC * ncols].rearrange("p (k n) -> p k n", n=ncols)
            src = w_in[:, col0:col0 + ncols].rearrange("(k p) n -> p k n", p=128)
            return dst, src
        return f

    groups = [("q", 0, 16, d_out["qT"]), ("k", 2048, 4, d_out["kT"]), ("iq", 3072, 16, d_out["iqT"])]
    pcs = []
    for kind, col0, nch, _ in groups:
        for i in range(nch // 2):
            pcs.append(piece(col0 + i * 256, 256))
    pcs.append(piece(5120, 144))
    ws.plan(pcs)
    for kind, col0, nch, dst in groups:
        for i in range(nch // 2):
            buf, bkey = ws.next()
            wv = buf[:].rearrange("p (k n) -> p k n", n=256)
            for s in range(2):
                hh = 2 * i + s
                head_chunk(wv, bkey, s, kind,
                           lambda tc, hh=hh, dst=dst: dst[:, hh * TL + tc * 512: hh * TL + (tc + 1) * 512])
    buf, bkey = ws.next()
    wv = buf[:, 0:KC * 144].rearrange("p (k n) -> p k n", n=144)
    head_chunk(wv, bkey, 0, "ik", lambda tc: d_out["ikT"][:, tc * 512:(tc + 1) * 512])
    iwsb = P.sb(es, "iwsb", [128, NT, 16], F32)
    for t in range(NT):
        for kc in range(KC):
            S.pe(lambda e, kc=kc, t=t: e.matmul(P.ps[4][:, t * 16:(t + 1) * 16], lhsT=hT[:, kc, t * 128:(t + 1) * 128],
                                                rhs=wv[:, kc, 128:144], start=(kc == 0), stop=(kc == KC - 1)),
                 r=[bkey, "hT"], w=[("ps", 4)])
    S.act(lambda e: e.copy(out=iwsb[:], in_=P.ps[4][:, 0:NT * 16].rearrange("p (a b) -> p a b", b=16)),
          r=[("ps", 4)], w=["iwsb"])
    S.dma(d_out["iw"], iwsb[:].rearrange("p a b -> p (a b)"), r=["iwsb"], w=["iw_out"])
    vsb = [P.sb(es, "vsb", [128, 512], BF16) for _ in range(2)]

    def vpiece(kp):
        def f(buf):
            dst = buf[:].rearrange("p (k n) -> p k n", n=512)
            src = w_in[kp * 1024:(kp + 1) * 1024, 2560:3072].rearrange("(k p) n -> p k n", p=128)
            return dst, src
        return f
    for half in range(2):
        ws.plan([vpiece(0), vpiece(1)])
        for kp in range(2):
            buf, bkey = ws.next()
            wv = buf[:].rearrange("p (k n) -> p k n", n=512)
            for t in range(4):
                j = half * 4 + t
                for k in range(8):
                    kc = kp * 8 + k
                    S.pe(lambda e, t=t, j=j, k=k, kc=kc, wv=wv: e.matmul(
                        P.ps[t][:], lhsT=hT[:, kc, j * 128:(j + 1) * 128], rhs=wv[:, k, :],
                        start=(kc == 0), stop=(kc == KC - 1)), r=[bkey, "hT"], w=[("ps", t)])
        for t in range(4):
            j = half * 4 + t
            S.act(lambda e, t=t: e.copy(out=vsb[t % 2][:], in_=P.ps[t][:]), r=[("ps", t)], w=[("vsb", t % 2)])
            S.dma(d_out["v"][j * 128:(j + 1) * 128, :], vsb[t % 2][:], r=[("vsb", t % 2)], w=[("v_out", j)])
    keys = ["iw_out"] + [("v_out", j) for j in range(NT)]
    return keys


def attn_a1_io(P):
    d_in = {"posb": P.inp("posb", [128, TL], I32), "invf": P.inp("invf", [128, 2]),
            "rt": P.inp("rt", [256, 128]), "qkg": P.inp("qkg", [128, 2])}
    d_out = {"qT": P.out("qT", [128, 16 * TL], BF16), "kT": P.out("kT", [128, 4 * TL], BF16),
             "iqT": P.out("iqT", [128, 16 * TL], BF16), "ikT": P.out("ikT", [128, TL], BF16),
             "iw": P.out("iw", [128, NT * 16]), "v": P.out("v", [TL, 512], BF16)}
    return d_in, d_out


def build_A1():
    P = Prog()
    S = P.S
    x_in = P.inp("x", [TL, D])
    g = P.inp("g_bc", [128, D])
    w_in = P.inp("w_in", [D, A_IN])
    d_in, d_out = attn_a1_io(P)
    with ExitStack() as es:
        c = load_consts(P, es)
        gbc = P.sb(es, "gbc", [128, D], F32)
        S.dma(gbc[:], g, w=["gbc"])
        wk = norm_work(P, es)
        hT = P.sb(es, "hT", [128, KC, TL], BF16)
        xt = [P.sb(es, "xt", [128, D], F32) for _ in range(2)]
        for t in range(NT):
            S.dma(xt[t % 2][:], x_in[t * 128:(t + 1) * 128, :], w=[("xt", t % 2)])
            emit_norm_T(P, c, xt[t % 2][:], ("xt", t % 2), gbc[:], hT, "hT", t * 128, wk)
        keys = emit_attn_inproj(P, c, es, hT, w_in, d_in, d_out)
        S.barrier()
        nc = P.close(keys)
    return nc


def rope_consts():
    inv_h = (1.0 / (10000.0 ** (np.arange(0, 128, 2, dtype=np.float32) / np.float32(128)))).astype(np.float32)
    inv_i = (1.0 / (10000.0 ** (np.arange(0, 64, 2, dtype=np.float32) / np.float32(64)))).astype(np.float32)
    invf = np.zeros((128, 2), np.float32)
    invf[:, 0] = np.concatenate([inv_h, inv_h])
    invf[:64, 1] = np.concatenate([inv_i, inv_i])
    R = np.zeros((128, 128), np.float32)
    for i in range(64):
        R[i, i + 64] = -1.0
        R[i + 64, i] = 1.0
    R2 = np.zeros((128, 128), np.float32)
    for i in range(32):
        R2[i, i + 32] = -1.0
        R2[i + 32, i] = 1.0
    rt = np.concatenate([R.T, R2.T], 0)
    return invf, np.ascontiguousarray(rt)


def a1_inputs(xs, pos, d, j, with_x=True):
    invf, rt = rope_consts()
    ps = shard_tokens(np.asarray(pos).reshape(T))
    qkg = np.ascontiguousarray(np.stack([d["attn_q_norm"][j], d["attn_k_norm"][j]], -1).astype(np.float32))
    ins = []
    for c in range(NCORES):
        m = {"posb": np.ascontiguousarray(np.broadcast_to(ps[c].astype(np.int32).reshape(1, TL), (128, TL))),
             "invf": invf, "rt": rt, "qkg": qkg, "w_in": np.ascontiguousarray(d["attn_w_in"][j]), "c_ident": IDENT}
        if with_x:
            m["x"] = xs[c]
            m["g_bc"] = bc128(d["attn_norm"][j])
        ins.append(m)
    return ins


def emit_attention(P, c, es, d, oT_s):
    S = P.S
    ikT = P.sb(es, "ikT", [128, T], BF16)
    for q in range(4):
        S.dma(ikT[:, q * 2048:(q + 1) * 2048], d["ikTa"][:, q * 2048:(q + 1) * 2048], w=["ikT"])
    iwsb = P.sb(es, "iwsb", [128, NT, 16], F32)
    S.dma(iwsb[:], d["iw"].rearrange("p (a b) -> p a b", b=16), w=["iwsb"])
    cm = P.sb(es, "cm", [128, 1024], F32)
    pen = P.sb(es, "pen", [128, 1024], F32)
    S.dma(cm[:], d["cm"], w=["cm"])
    S.dma(pen[:], d["pen"], w=["pen"])
    score = P.sb(es, "score", [128, T], F32)
    junk = P.sb(es, "junkb", [128, T], BF16)
    maskT = P.sb(es, "maskT", [128, T // 128, 128], BF16)
    mkb = [P.sb(es, "mkb", [128, 512], BF16) for _ in range(2)]
    rl = [P.sb(es, "rl", [128, 512], F32) for _ in range(2)]
    iqtb = [P.sb(es, "iqt", [128, 16, 128], BF16) for _ in range(2)]
    qt = [P.sb(es, "qt", [128, 4, 128], BF16) for _ in range(2)]
    kTb = [P.sb(es, "kTg", [128, T], BF16) for _ in range(2)]
    vgb = [P.sb(es, "vg", [128, T // 128, 128], BF16) for _ in range(2)]
    ptb = [P.sb(es, "ptb", [128, 512], BF16) for _ in range(3)]
    rden = P.sb(es, "rden", [128, 512], F32)
    ot = [P.sb(es, "ot", [128, 512], F32) for _ in range(2)]
    sm = {n: P.sb(es, n, [128, 1], F32) for n in ("M", "lo", "hi", "mid", "cnt", "pred", "dl")}
    iq3 = d["iqT"].rearrange("p (h t) -> p h t", t=TL)
    q3 = d["qT"].rearrange("p (h t) -> p h t", t=TL)
    o3 = oT_s.rearrange("p (h t) -> p h t", t=TL)
    SC = 128.0 ** -0.5
    okeys = []

    def gen_indexer(j):
        Kc = 1024 * (j + 1)
        iqt = iqtb[j % 2]
        S.dma(iqt[:], iq3[:, :, j * 128:(j + 1) * 128], w=[("iqt", j % 2)])
        for k5 in range(Kc // 512):
            sc = score[:, k5 * 512:(k5 + 1) * 512]
            for h in range(16):
                pb = P.ps[h % 2]
                S.pe(lambda e, h=h, pb=pb, k5=k5: e.matmul(pb[:], lhsT=iqt[:, h, :], rhs=ikT[:, k5 * 512:(k5 + 1) * 512],
                                                         start=True, stop=True), r=[("iqt", j % 2), "ikT"], w=[("ps", h % 2)])
                r_ = rl[h % 2]
                S.act(lambda e, pb=pb, r_=r_: e.activation(out=r_[:], in_=pb[:], func=AF.Relu),
                      r=[("ps", h % 2)], w=[("rl", h % 2)])
                if h == 0:
                    S.dve(lambda e, r_=r_, sc=sc: e.tensor_scalar(sc, r_[:], iwsb[:, j, 0:1], None, ALU.mult),
                          r=[("rl", h % 2), "iwsb"], w=["score"])
                else:
                    S.dve(lambda e, r_=r_, sc=sc, h=h: e.scalar_tensor_tensor(
                        out=sc, in0=r_[:], scalar=iwsb[:, j, h:h + 1], in1=sc, op0=ALU.mult, op1=ALU.add),
                        r=[("rl", h % 2), "iwsb", "score"], w=["score"])
                yield

    def post_indexer(j):
        Kc = 1024 * (j + 1)
        S.dve(lambda e: e.tensor_reduce(out=sm["M"][:], in_=score[:, 0:Kc], axis=AX.X, op=ALU.max), r=["score"], w=["M"])
        S.dve(lambda e: e.tensor_reduce(out=sm["cnt"][:], in_=score[:, 0:Kc], axis=AX.X, op=ALU.min), r=["score"], w=["cnt"])
        S.dve(lambda e: e.tensor_tensor(out=sm["dl"][:], in0=sm["M"][:], in1=sm["cnt"][:], op=ALU.subtract),
              r=["M", "cnt"], w=["dl"])
        win = score[:, Kc - 1024:Kc]
        S.dve(lambda e: e.tensor_tensor(out=win, in0=win, in1=cm[:], op=ALU.mult), r=["score", "cm"], w=["score"])
        S.dve(lambda e: e.tensor_tensor(out=win, in0=win, in1=pen[:], op=ALU.add), r=["score", "pen"], w=["score"])
        S.dve(lambda e: e.scalar_tensor_tensor(out=sm["lo"][:], in0=sm["dl"][:], scalar=-0.001, in1=sm["cnt"][:],
                                               op0=ALU.mult, op1=ALU.add), r=["dl", "cnt"], w=["lo"])
        S.dve(lambda e: e.tensor_scalar(sm["lo"][:], sm["lo"][:], -1e-6, None, ALU.add), r=["lo"], w=["lo"])
        S.dve(lambda e: e.tensor_scalar(sm["hi"][:], sm["dl"][:], 1.002, 2e-6, ALU.mult, ALU.add), r=["dl"], w=["hi"])
        Ka = max(128, int(round(Kc * 0.45 / 128)) * 128)
        for it in range(NBIS):
            S.dve(lambda e: e.tensor_scalar(sm["hi"][:], sm["hi"][:], 0.5, None, ALU.mult), r=["hi"], w=["hi"])
            S.dve(lambda e: e.tensor_tensor(out=sm["mid"][:], in0=sm["lo"][:], in1=sm["hi"][:], op=ALU.add),
                  r=["lo", "hi"], w=["mid"])
            S.dve(lambda e: e.tensor_scalar(junk[:, 0:Ka], score[:, 0:Ka], sm["mid"][:], 0.0, ALU.is_ge, ALU.add,
                                            accum_out=sm["cnt"][:]), r=["score", "mid"], w=["junkD", "cnt"])
            S.act(lambda e: e.activation(out=junk[:, Ka:Kc], in_=score[:, Ka:Kc], func=AF.Sign, bias=sm["mid"][:], scale=-1.0,
                                         accum_out=sm["M"][:]), r=["score", "mid"], w=["junkA", "M"])
            S.dve(lambda e: e.scalar_tensor_tensor(out=sm["pred"][:], in0=sm["cnt"][:], scalar=2.0, in1=sm["M"][:],
                                                   op0=ALU.mult, op1=ALU.subtract), r=["cnt", "M"], w=["pred"])
            S.dve(lambda e: e.tensor_scalar(sm["pred"][:], sm["pred"][:], float(2 * TOPK - (Kc - Ka)), None, ALU.is_ge),
                  r=["pred"], w=["pred"])
            S.dve(lambda e: e.scalar_tensor_tensor(out=sm["lo"][:], in0=sm["hi"][:], scalar=sm["pred"][:], in1=sm["lo"][:],
                                                   op0=ALU.mult, op1=ALU.add), r=["hi", "pred", "lo"], w=["lo"])
        for k5 in range(Kc // 512):
            mk = mkb[k5 % 2]
            S.dve(lambda e, mk=mk, k5=k5: e.tensor_scalar(mk[:], score[:, k5 * 512:(k5 + 1) * 512], sm["lo"][:], -30000.0,
                                                          ALU.is_lt, ALU.mult), r=["score", "lo"], w=[("mkb", k5 % 2)])
            pbb = P.psb[k5 % 2]
            for i in range(4):
                S.pe(lambda e, mk=mk, i=i, pbb=pbb: e.transpose(out=pbb[:, i * 128:(i + 1) * 128],
                                                                in_=mk[:, i * 128:(i + 1) * 128], identity=c["ident"][:]),
                     r=[("mkb", k5 % 2), "ident"], w=[("psb", k5 % 2)])
            S.act(lambda e, k5=k5, pbb=pbb: e.copy(out=maskT[:, k5 * 4:(k5 + 1) * 4, :],
                                                   in_=pbb[:, 0:512].rearrange("p (a b) -> p a b", b=128)),
                  r=[("psb", k5 % 2)], w=["maskT"])

    def gen_attention(j):
        Kc = 1024 * (j + 1)
        n1 = Kc // 128
        for g in range(4):
            gi = j * 4 + g
            qg = qt[gi % 2]
            kT = kTb[gi % 2]
            vg = vgb[gi % 2]
            kk, vk = ("kTg", gi % 2), ("vg", gi % 2)
            S.dma(qg[:], q3[:, 4 * g:4 * g + 4, j * 128:(j + 1) * 128], w=[("qt", gi % 2)])
            S.dma(kT[:, 0:Kc], d["kTa"][:, g * T:g * T + Kc], w=[kk])
            for k0 in range(0, n1, 16):
                S.dma(vg[:, k0:k0 + 16, :],
                      d["va"][k0 * 128:(k0 + 16) * 128, g * 128:(g + 1) * 128].rearrange("(k p) e -> p k e", p=128),
                      w=[vk])
            qg2 = qg[:].rearrange("p a b -> p (a b)")

            def st(kc, kT=kT, kk=kk, qg2=qg2, gi=gi):
                pb = P.ps[2 + kc % 2]
                S.pe(lambda e, pb=pb: e.matmul(pb[:], lhsT=kT[:, kc * 128:(kc + 1) * 128], rhs=qg2, start=True, stop=False),
                     r=[kk, ("qt", gi % 2)], w=[("ps", 2 + kc % 2)])
                S.pe(lambda e, pb=pb: e.matmul(pb[:].rearrange("p (a b) -> p a b", b=128), lhsT=c["ident"][:],
                                               rhs=maskT[:, kc:kc + 1, :].to_broadcast([128, 4, 128]), start=False, stop=True),
                     r=["ident", "maskT"], w=[("ps", 2 + kc % 2)])
            st(0)
            for kc in range(n1):
                pb = P.ps[2 + kc % 2]
                pt = ptb[kc % 3]
                S.act(lambda e, pb=pb, pt=pt: e.activation(out=pt[:], in_=pb[:], func=AF.Exp, scale=SC),
                      r=[("ps", 2 + kc % 2)], w=[("ptb", kc % 3)])
                if kc + 1 < n1:
                    st(kc + 1)
                yield
                S.pe(lambda e, kc=kc, pt=pt, vg=vg: e.matmul(P.ps[4][:], lhsT=vg[:, kc, :], rhs=pt[:],
                                                             start=(kc == 0), stop=(kc == n1 - 1)),
                     r=[vk, ("ptb", kc % 3)], w=[("ps", 4)])
                S.pe(lambda e, kc=kc, pt=pt: e.matmul(P.ps[5][:], lhsT=c["ones"][:], rhs=pt[:],
                                                      start=(kc == 0), stop=(kc == n1 - 1)),
                     r=["ones", ("ptb", kc % 3)], w=[("ps", 5)])
                yield
            S.dve(lambda e: e.reciprocal(out=rden[:], in_=P.ps[5][:]), r=[("ps", 5)], w=["rden"])
            o = ot[g % 2]
            S.dve(lambda e, o=o: e.tensor_tensor(out=o[:], in0=P.ps[4][:], in1=rden[:], op=ALU.mult),
                  r=[("ps", 4), "rden"], w=[("ot", g % 2)])
            S.dma(o3[:, 4 * g:4 * g + 4, j * 128:(j + 1) * 128], o[:].rearrange("p (a b) -> p a b", b=128),
                  r=[("ot", g % 2)], w=[("oT_s", j, g)])
            okeys.append(("oT_s", j, g))

    for _ in gen_indexer(0):
        pass
    post_indexer(0)
    for j in range(NT):
        ga = gen_attention(j)
        gx = gen_indexer(j + 1) if j + 1 < NT else iter(())
        da = dx = False
        while not (da and dx):
            if not da:
                try:
                    next(ga)
                except StopIteration:
                    da = True
            if not dx:
                try:
                    next(gx)
                except StopIteration:
                    dx = True
            if not da:
                try:
                    next(ga)
                except StopIteration:
                    da = True
        if j + 1 < NT:
            post_indexer(j + 1)
    return okeys


def build_A2(dbg=False):
    P = Prog()
    S = P.S
    x_in = P.inp("x", [TL, D])
    d = {"qT": P.inp("qT", [128, 16 * TL], BF16), "iqT": P.inp("iqT", [128, 16 * TL], BF16),
         "iw": P.inp("iw", [128, NT * 16]), "kTa": P.inp("kTa", [128, 4 * T], BF16), "va": P.inp("va", [T, 512], BF16),
         "ikTa": P.inp("ikTa", [128, T], BF16),
         "cm": P.inp("cm", [128, 1024]), "pen": P.inp("pen", [128, 1024])}
    w_out = P.inp("w_out", [D, D])
    g2 = P.inp("g_bc", [128, D])
    w_up = P.inp("w_up", [D, DFF])
    w_down = P.inp("w_down", [DFF, D])
    y = P.out("y", [TL, D])
    oT_s = P.out("oT_dbg", [128, 16 * TL]) if dbg else P.scratch("oT_s", [128, 16 * TL])
    with ExitStack() as es:
        c = load_consts(P, es)
        with ExitStack() as es1:
            emit_attention(P, c, es1, d, oT_s)
            S.barrier()
        x_res = P.sb(es, "x_res", [128, NT, D], F32)
        load_x(P, x_res, x_in)
        with ExitStack() as es2:
            oT = P.sb(es2, "oT", [128, KC, TL], BF16)
            S.dma(oT[:], oT_s.rearrange("p (h t) -> p h t", t=TL), w=["oT"], q="pool")
            ws = WStream(P, es2, "wout", [128, 4096], nbuf=3)
            emit_linear_tm_res(P, ws, oT, "oT", KC, w_out, x_res)
            S.barrier()
        emit_mlp(P, c, es, x_res, g2, w_up, w_down)
        keys = store_x(P, y, x_res)
        nc = P.close(keys)
    return nc


def a2_inputs(xs, a1, d, j, li):
    kT = np.stack([a1[c]["kT"].reshape(128, 4, NT, 128) for c in range(NCORES)], 3)
    kTa = np.ascontiguousarray(kT.reshape(128, 4 * T))
    ik = np.stack([a1[c]["ikT"].reshape(128, NT, 128) for c in range(NCORES)], 2)
    ikTa = np.ascontiguousarray(ik.reshape(128, T))
    va = unshard_tokens([a1[c]["v"] for c in range(NCORES)])
    common = {"kTa": kTa, "va": va, "ikTa": ikTa, "w_out": np.ascontiguousarray(d["attn_w_out"][j]),
              "g_bc": bc128(d["mlp_norm"][li]), "w_up": np.ascontiguousarray(d["mlp_w_up"][li]),
              "w_down": np.ascontiguousarray(d["mlp_w_down"][li]), "c_ident": IDENT}
    ins = []
    f = np.arange(1024).reshape(1, 1024)
    p = np.arange(128).reshape(128, 1)
    for c in range(NCORES):
        cm = (f <= 128 * c + p)
        ins.append(dict(common, x=xs[c], qT=a1[c]["qT"], iqT=a1[c]["iqT"], iw=a1[c]["iw"],
                        cm=np.where(cm, np.float32(1.0), np.float32(0.0)).astype(np.float32),
                        pen=np.where(cm, np.float32(0.0), np.float32(-BIG)).astype(np.float32)))
    return ins


POOL_W = (2, 4, 8, 16)


def emit_pool(P, c, es_outer, x_res, hT9, d):
    S = P.S
    with ExitStack() as es:
        hx = P.sb(es, "hx", [128, 8, 144], F32)
        sA = P.sb(es, "sA", [128, 8, 144], F32)
        sB = P.sb(es, "sB", [128, 8, 144], F32)
        yT = P.sb(es, "yTg", [128, 4, TL], BF16)
        rd = P.sb(es, "rd", [128, TL], F32)
        wp = P.sb(es, "wp", [128, 4, 4, 512], BF16)
        bbc = P.sb(es, "bbc", [128, D], F32)
        sbc = P.sb(es, "sbc", [128, D], F32)
        tt = [P.sb(es, "ptt", [128, 512], F32) for _ in range(2)]
        for g in range(4):
            S.dma(wp[:, g], d["pool_w"][g * 512:(g + 1) * 512, :].rearrange("(k p) n -> p k n", p=128), w=["wp"], q="pool")
        S.dma(bbc[:], d["pool_b"], w=["bbc"])
        S.dma(sbc[:], d["pool_s"], w=["sbc"])
        for g in range(4):
            w = POOL_W[g]
            S.dma(rd[:], d["mind"][:, g * TL:(g + 1) * TL], w=["rd"])
            S.dve(lambda e: e.reciprocal(out=rd[:], in_=rd[:]), r=["rd"], w=["rd"])
            rd3 = rd[:].rearrange("p (a b) -> p a b", b=128)
            for ci in range(4):
                ch = 4 * g + ci
                S.act(lambda e, ch=ch: e.copy(out=hx[:, :, 16:144], in_=hT9[:, ch, 0:TL].rearrange("p (a b) -> p a b", b=128)),
                      r=["hT"], w=["hx"])
                S.act(lambda e, ch=ch: e.copy(out=hx[:, :, 0:16], in_=hT9[:, ch, TL:TL + 128].rearrange("p (a b) -> p a b", b=16)),
                      r=["hT"], w=["hx"])
                cur, ck = hx, "hx"
                nxt = [(sA, "sA"), (sB, "sB")]
                step = 1
                k = 0
                while step < w:
                    o, ok = nxt[k % 2]
                    S.dve(lambda e, o=o, cur=cur, step=step: e.tensor_tensor(
                        out=o[:, :, step:144], in0=cur[:, :, step:144], in1=cur[:, :, 0:144 - step], op=ALU.add),
                        r=[ck], w=[ok])
                    cur, ck = o, ok
                    step *= 2
                    k += 1
                S.dve(lambda e, cur=cur, rd3=rd3: e.tensor_tensor(out=cur[:, :, 16:144], in0=cur[:, :, 16:144], in1=rd3,
                                                                 op=ALU.mult), r=[ck, "rd"], w=[ck])
                S.dve(lambda e, cur=cur, ci=ci: e.tensor_tensor(
                    out=yT[:, ci, :].rearrange("p (a b) -> p a b", b=128), in0=cur[:, :, 16:144], in1=hx[:, :, 16:144],
                    op=ALU.subtract), r=[ck, "hx"], w=["yTg"])
            for j in range(NT):
                pb = P.ps[j % 4]
                for kc in range(4):
                    S.pe(lambda e, kc=kc, j=j, pb=pb, g=g: e.matmul(pb[:], lhsT=yT[:, kc, j * 128:(j + 1) * 128],
                                                                   rhs=wp[:, g, kc, :], start=(kc == 0), stop=(kc == 3)),
                         r=["yTg", "wp"], w=[("ps", j % 4)])
                t_ = tt[j % 2]
                gs = slice(g * 512, (g + 1) * 512)
                S.dve(lambda e, t_=t_, pb=pb, gs=gs: e.tensor_tensor(out=t_[:], in0=pb[:], in1=bbc[:, gs], op=ALU.add),
                      r=[("ps", j % 4), "bbc"], w=[("ptt", j % 2)])
                S.pool(lambda e, t_=t_, gs=gs: e.tensor_tensor(out=t_[:], in0=t_[:], in1=sbc[:, gs], op=ALU.mult),
                       r=[("ptt", j % 2), "sbc"], w=[("ptt", j % 2)])
                xs_ = x_res[:, j, gs]
                S.dve(lambda e, t_=t_, xs_=xs_: e.tensor_tensor(out=xs_, in0=xs_, in1=t_[:], op=ALU.add),
                      r=[("ptt", j % 2), ("x", j)], w=[("x", j)])
        S.barrier()


def build_PA1():
    P = Prog()
    S = P.S
    x9 = P.inp("x9", [9 * 128, D])
    gp = P.inp("gp_bc", [128, D])
    dp = {"pool_w": P.inp("pool_w", [D, 512]), "pool_b": P.inp("pool_b", [128, D]), "pool_s": P.inp("pool_s", [128, D]),
          "mind": P.inp("mind", [128, 4 * TL])}
    g2 = P.inp("g_bc", [128, D])
    w_up = P.inp("w_up", [D, DFF])
    w_down = P.inp("w_down", [DFF, D])
    ga = P.inp("ga_bc", [128, D])
    w_in = P.inp("w_in", [D, A_IN])
    d_in, d_out = attn_a1_io(P)
    y = P.out("y", [TL, D])
    with ExitStack() as es:
        c = load_consts(P, es)
        x_res = P.sb(es, "x_res", [128, NT, D], F32)
        load_x(P, x_res, x9)
        with ExitStack() as es1:
            gbc = P.sb(es1, "gbc", [128, D], F32)
            S.dma(gbc[:], gp, w=["gbc"])
            wk = norm_work(P, es1)
            hT9 = P.sb(es1, "hT9", [128, KC, 9 * 128], BF16)
            xt = P.sb(es1, "xt", [128, D], F32)
            S.dma(xt[:], x9[TL:TL + 128, :], w=["xt"])
            for t in range(NT):
                emit_norm_T(P, c, x_res[:, t, :], ("x", t), gbc[:], hT9, "hT", t * 128, wk)
            emit_norm_T(P, c, xt[:], "xt", gbc[:], hT9, "hT", TL, wk)
            emit_pool(P, c, es1, x_res, hT9, dp)
        emit_mlp(P, c, es, x_res, g2, w_up, w_down)
        keys = store_x(P, y, x_res)
        with ExitStack() as es2:
            gbc = P.sb(es2, "gbc", [128, D], F32)
            S.dma(gbc[:], ga, w=["gbc"])
            wk = norm_work(P, es2)
            hT = P.sb(es2, "hT", [128, KC, TL], BF16)
            for t in range(NT):
                emit_norm_T(P, c, x_res[:, t, :], ("x", t), gbc[:], hT, "hT", t * 128, wk)
            keys += emit_attn_inproj(P, c, es2, hT, w_in, d_in, d_out)
            S.barrier()
        nc = P.close(keys)
    return nc


def pa1_inputs(xg, pos, d, li, ja):
    xs = shard_tokens(xg)
    a1 = a1_inputs(xs, pos, d, ja, with_x=False)
    common = {"gp_bc": bc128(d["pool_norm"][0]), "pool_w": np.ascontiguousarray(d["pool_w"][0].reshape(D, 512)),
              "pool_b": bc128(d["pool_b"][0].reshape(-1)), "pool_s": bc128(d["pool_scale"][0]),
              "g_bc": bc128(d["mlp_norm"][li]), "w_up": np.ascontiguousarray(d["mlp_w_up"][li]),
              "w_down": np.ascontiguousarray(d["mlp_w_down"][li]), "ga_bc": bc128(d["attn_norm"][ja])}
    ins = []
    for c in range(NCORES):
        idx = (np.arange(NT).reshape(NT, 1) * 8 + c) * 128 + np.arange(128).reshape(1, 128)
        mind = np.stack([np.minimum(idx + 1, w) for w in POOL_W], 0).reshape(1, 4 * TL).astype(np.float32)
        m = dict(common, **a1[c])
        m["x9"] = np.concatenate([xs[c], halo_tile(xg, c)], 0)
        m["mind"] = np.ascontiguousarray(np.broadcast_to(mind, (128, 4 * TL)))
        ins.append(m)
    return ins


_CACHE = {}


def _prog(name, fn):
    if name not in _CACHE:
        _CACHE[name] = fn()
    return _CACHE[name]


def _run(nc, ins):
    return run_bass_kernel_spmd(nc, ins, core_ids=list(range(NCORES))).results


def kernel(**inp):
    d = {k: np.asarray(v) for k, v in inp.items()}
    xg = np.ascontiguousarray(d["x"].reshape(T, D).astype(np.float32, copy=False))
    pos = d["positions"].reshape(T)
    xs = shard_tokens(xg)
    a1 = _run(_prog("A1", build_A1), a1_inputs(xs, pos, d, 0))
    r = _run(_prog("A2", build_A2), a2_inputs(xs, a1, d, 0, 0))
    xg = unshard_tokens([r[c]["y"] for c in range(NCORES)])
    r1 = _run(_prog("R1", build_R1), r1_inputs(xg, d, 0))
    r = _run(_prog("R2", build_R2), r2_inputs(xg, r1, d, 0, 1))
    xg = unshard_tokens([r[c]["y"] for c in range(NCORES)])
    r = _run(_prog("PA1", build_PA1), pa1_inputs(xg, pos, d, 2, 1))
    xg = unshard_tokens([r[c]["y"] for c in range(NCORES)])
    xs = shard_tokens(xg)
    r = _run(_prog("A2", build_A2), a2_inputs(xs, r, d, 1, 3))
    out = unshard_tokens([r[c]["y"] for c in range(NCORES)])
    return out.reshape(1, T, D).astype(np.float32, copy=False)
```

```python
import numpy as np
from contextlib import ExitStack
import concourse.bass as bass
import concourse.mybir as mybir
from concourse.bass_utils import run_bass_kernel_spmd

F32 = mybir.dt.float32
BF16 = mybir.dt.bfloat16
I32 = mybir.dt.int32
ALU = mybir.AluOpType
AF = mybir.ActivationFunctionType
AX = mybir.AxisListType

NCORES = 8
T = 8192
D = 2048
TL = T // NCORES
NT = TL // 128
KC = D // 128
DFF = 4 * D
EPS = 1e-6
A_IN = 5264
TOPK = 256
NBIS = 21
BIG = 1.0e30


class Sched:
    NDS = 40

    def __init__(self, nc, es):
        self.nc = nc
        self.E = {"pe": nc.tensor, "act": nc.scalar, "dve": nc.vector, "pool": nc.gpsimd, "sp": nc.sync}
        self.csem = {e: es.enter_context(nc.semaphore("c_" + e)) for e in ("pe", "act", "dve", "pool")}
        self.ccnt = {e: 0 for e in self.csem}
        self.dsem = [es.enter_context(nc.semaphore("d%d" % i)) for i in range(self.NDS)]
        self.dcnt = [0] * self.NDS
        self.drr = 0
        self.drr_sw = 0
        self.known = {e: {} for e in self.E}
        self.lastw = {}
        self.readers = {}
        self.nins = 0

    def _wait(self, eng, ev):
        sid, h, val, src, isdma = ev
        if src == "pe" and eng == "pe" and not isdma:
            return
        if self.known[eng].get(sid, 0) >= val:
            return
        self.E[eng].wait_ge(h, val)
        self.known[eng][sid] = val

    def op(self, eng, fn, r=(), w=(), dma=False):
        deps = []
        for k in r:
            if k in self.lastw:
                deps.append(self.lastw[k])
        for k in w:
            if k in self.lastw:
                deps.append(self.lastw[k])
            deps.extend(self.readers.get(k, {}).values())
        for ev in deps:
            self._wait(eng, ev)
        if dma:
            half = self.NDS // 2
            if eng == "pool":
                i = self.drr_sw
                self.drr_sw = (self.drr_sw + 1) % half
            else:
                i = half + self.drr
                self.drr = (self.drr + 1) % half
            if self.dcnt[i] > 0:
                self._wait(eng, (("d", i), self.dsem[i], self.dcnt[i], eng, True))
        ins = fn(self.E[eng])
        self.nins += 1
        if dma:
            self.dcnt[i] += 16
            ins.then_inc(self.dsem[i], 16)
            ev = (("d", i), self.dsem[i], self.dcnt[i], eng, True)
        else:
            self.ccnt[eng] += 1
            ins.then_inc(self.csem[eng], 1)
            ev = (("c", eng), self.csem[eng], self.ccnt[eng], eng, False)
        for k in w:
            self.lastw[k] = ev
            self.readers[k] = {}
        for k in r:
            self.readers.setdefault(k, {})[ev[0]] = ev
        return ev

    def pe(self, fn, r=(), w=()):
        return self.op("pe", fn, r, w)

    def act(self, fn, r=(), w=()):
        return self.op("act", fn, r, w)

    def dve(self, fn, r=(), w=()):
        return self.op("dve", fn, r, w)

    def pool(self, fn, r=(), w=()):
        return self.op("pool", fn, r, w)

    def dma(self, out, in_, r=(), w=(), q="sp"):
        return self.op(q, lambda e: e.dma_start(out=out, in_=in_), r, w, dma=True)

    def barrier(self):
        evs = []
        for e in self.csem:
            if self.ccnt[e] > 0:
                evs.append((("c", e), self.csem[e], self.ccnt[e], e, False))
        for i in range(self.NDS):
            if self.dcnt[i] > 0:
                evs.append((("d", i), self.dsem[i], self.dcnt[i], "sp", True))
        for eng in self.E:
            for ev in evs:
                if ev[3] == "pe" and eng == "pe" and not ev[4]:
                    continue
                if self.known[eng].get(ev[0], 0) >= ev[2]:
                    continue
                self.E[eng].wait_ge(ev[1], ev[2])
                self.known[eng][ev[0]] = ev[2]
        self.lastw = {}
        self.readers = {}

    def finish(self, keys):
        for k in keys:
            if k in self.lastw:
                self._wait("sp", self.lastw[k])


class Prog:
    def __init__(self):
        self.nc = bass.Bass("TRN2", target_bir_lowering=False)
        self.es = ExitStack()
        self.S = Sched(self.nc, self.es)
        self.ins = {}
        self.outs = {}
        nc = self.nc
        self.ps = [self.es.enter_context(nc.psum_tensor("psf%d" % i, [128, 512], F32)) for i in range(6)]
        self.psb = [self.es.enter_context(nc.psum_tensor("psb%d" % i, [128, 1024], BF16)) for i in range(2)]
        self.uid = 0

    def inp(self, name, shape, dt=F32):
        t = self.nc.dram_tensor(name, list(shape), dt, kind="ExternalInput").ap()
        self.ins[name] = t
        return t

    def out(self, name, shape, dt=F32):
        t = self.nc.dram_tensor(name, list(shape), dt, kind="ExternalOutput").ap()
        self.outs[name] = t
        return t

    def scratch(self, name, shape, dt=F32):
        return self.nc.dram_tensor(name, list(shape), dt).ap()

    def sb(self, es, name, shape, dt=F32):
        self.uid += 1
        return es.enter_context(self.nc.sbuf_tensor("%s_%d" % (name, self.uid), list(shape), dt))

    def close(self, out_keys):
        self.S.finish(out_keys)
        self.es.close()
        return self.nc


def load_consts(P, es):
    S = P.S
    c = {}
    ident_d = P.inp("c_ident", [128, 128])
    c["ident"] = P.sb(es, "ident", [128, 128], BF16)
    c["ones"] = P.sb(es, "ones", [128, 128], BF16)
    S.dma(c["ident"][:], ident_d, w=["ident"], q="pool")
    S.pool(lambda e: e.memset(c["ones"][:], 1.0), w=["ones"])
    return c


def emit_norm_T(P, c, xsrc, xkey, g_bc, hT, hkey, col0, work):
    S = P.S
    junk, ss, rs, hbf = work["junk"], work["ss"], work["rs"], work["hbf"]
    S.dve(lambda e: e.scalar_tensor_tensor(out=junk[:], in0=xsrc, scalar=1.0, in1=xsrc,
                                           op0=ALU.mult, op1=ALU.mult, accum_out=ss[:]),
          r=[xkey], w=["n_junk", "n_ss"])
    S.act(lambda e: e.activation(out=rs[:], in_=ss[:], func=AF.Sqrt, bias=work["eps"][:], scale=1.0 / D),
          r=["n_ss", "n_eps"], w=["n_rs"])
    S.dve(lambda e: e.reciprocal(out=rs[:], in_=rs[:]), r=["n_rs"], w=["n_rs"])
    S.dve(lambda e: e.scalar_tensor_tensor(out=hbf[:], in0=xsrc, scalar=rs[:], in1=g_bc,
                                           op0=ALU.mult, op1=ALU.mult),
          r=[xkey, "n_rs", "gbc"], w=["n_hbf"])
    for q in range(KC // 8):
        pb = P.psb[q % 2]
        for i in range(8):
            kc = q * 8 + i
            S.pe(lambda e, kc=kc, i=i: e.transpose(out=pb[:, i * 128:(i + 1) * 128],
                                                   in_=hbf[:, kc * 128:(kc + 1) * 128],
                                                   identity=c["ident"][:]),
                 r=["n_hbf", "ident"], w=[("psb", q % 2)])
        dst = hT[:, q * 8:(q + 1) * 8, col0:col0 + 128]
        src = pb[:].rearrange("p (a b) -> p a b", b=128)
        if q % 2 == 0:
            S.act(lambda e, dst=dst, src=src: e.copy(out=dst, in_=src), r=[("psb", q % 2)], w=[hkey])
        else:
            S.dve(lambda e, dst=dst, src=src: e.tensor_copy(out=dst, in_=src), r=[("psb", q % 2)], w=[hkey])


def norm_work(P, es):
    w = {
        "junk": P.sb(es, "n_junk", [128, D], BF16),
        "ss": P.sb(es, "n_ss", [128, 1], F32),
        "rs": P.sb(es, "n_rs", [128, 1], F32),
        "hbf": P.sb(es, "n_hbf", [128, D], BF16),
        "eps": P.sb(es, "n_eps", [128, 1], F32),
    }
    P.S.pool(lambda e: e.memset(w["eps"][:], EPS), w=["n_eps"])
    return w


class WStream:
    def __init__(self, P, es, name, shape, nbuf=4):
        self.P = P
        self.name = name
        self.bufs = [P.sb(es, name, shape, BF16) for _ in range(nbuf)]
        self.nbuf = nbuf
        self.pieces = []
        self.issued = 0
        self.used = 0

    def plan(self, pieces):
        self.pieces = list(pieces)
        self.issued = 0
        self.used = 0

    def _issue(self):
        i = self.issued
        b = i % self.nbuf
        dst, src = self.pieces[i](self.bufs[b])
        self.P.S.dma(dst, src, w=[(self.name, b)], q="pool")
        self.issued += 1

    def next(self):
        while self.issued < len(self.pieces) and self.issued < self.used + self.nbuf - 1:
            self._issue()
        if self.issued <= self.used:
            self._issue()
        b = self.used % self.nbuf
        self.used += 1
        return self.bufs[b], (self.name, b)


def emit_mlp(P, c, es_outer, x_res, g_bc_dram, w_up, w_down):
    S = P.S
    HF = DFF // 2
    NOC = HF // 128
    acc = [P.ps[t][:] for t in range(6)] + [P.psb[t][:].bitcast(F32) for t in range(2)]
    akey = [("ps", t) for t in range(6)] + [("psb", t) for t in range(2)]
    with ExitStack() as es:
        gbc = P.sb(es, "gbc", [128, D], F32)
        S.dma(gbc[:], g_bc_dram, w=["gbc"])
        wk = norm_work(P, es)
        hT = P.sb(es, "hT", [128, KC, TL], BF16)
        actT = P.sb(es, "actT", [128, NOC, TL], BF16)
        rl = [P.sb(es, "rl", [128, 512], F32) for _ in range(2)]
        ws = WStream(P, es, "wmlp", [128, 4096], nbuf=3)
        for j in range(NT):
            emit_norm_T(P, c, x_res[:, j, :], ("x", j), gbc[:], hT, "hT", j * 128, wk)
        for fh in range(2):
            def up_piece(i):
                def f(buf):
                    dst = buf[:].rearrange("p (k n) -> p k n", n=256)
                    c0 = fh * HF + i * 256
                    src = w_up[:, c0:c0 + 256].rearrange("(k p) n -> p k n", p=128)
                    return dst, src
                return f
            ws.plan([up_piece(i) for i in range(NOC // 2)])
            cnt = 0
            for i in range(NOC // 2):
                buf, bkey = ws.next()
                wv = buf[:].rearrange("p (k n) -> p k n", n=256)
                for s in range(2):
                    oc = i * 2 + s
                    for tc in range(2):
                        bi = cnt % 4
                        pb = P.ps[bi]
                        for kc in range(KC):
                            S.pe(lambda e, kc=kc, s=s, pb=pb, wv=wv, tc=tc: e.matmul(
                                pb[:], lhsT=wv[:, kc, s * 128:(s + 1) * 128], rhs=hT[:, kc, tc * 512:(tc + 1) * 512],
                                start=(kc == 0), stop=(kc == KC - 1)),
                                r=[bkey, "hT"], w=[("ps", bi)])
                        r_ = rl[cnt % 2]
                        S.act(lambda e, pb=pb, r_=r_: e.activation(out=r_[:], in_=pb[:], func=AF.Relu),
                              r=[("ps", bi)], w=[("rl", cnt % 2)])
                        S.pool(lambda e, oc=oc, r_=r_, tc=tc: e.tensor_tensor(
                            out=actT[:, oc, tc * 512:(tc + 1) * 512], in0=r_[:], in1=r_[:], op=ALU.mult),
                            r=[("rl", cnt % 2)], w=["actT"])
                        cnt += 1
            nkp = NOC // 8

            def dn_piece(cc, kp):
                def f(buf):
                    dst = buf[:].rearrange("p (k n) -> p k n", n=512)
                    r0 = fh * HF + kp * 1024
                    src = w_down[r0:r0 + 1024, cc * 512:(cc + 1) * 512].rearrange("(k p) n -> p k n", p=128)
                    return dst, src
                return f
            ws.plan([dn_piece(cc, kp) for cc in range(4) for kp in range(nkp)])
            for cc in range(4):
                for kp in range(nkp):
                    buf, bkey = ws.next()
                    wv = buf[:].rearrange("p (k n) -> p k n", n=512)
                    for t in range(NT):
                        for k in range(8):
                            kc = kp * 8 + k
                            S.pe(lambda e, t=t, k=k, kc=kc, wv=wv: e.matmul(
                                acc[t], lhsT=actT[:, kc, t * 128:(t + 1) * 128], rhs=wv[:, k, :],
                                start=(kc == 0), stop=(kc == NOC - 1)),
                                r=[bkey, "actT"], w=[akey[t]])
                for t in range(NT):
                    xs = x_res[:, t, cc * 512:(cc + 1) * 512]
                    S.dve(lambda e, xs=xs, t=t: e.tensor_tensor(out=xs, in0=xs, in1=acc[t], op=ALU.add),
                          r=[akey[t], ("x", t)], w=[("x", t)])
        S.barrier()


def build_mlp_only():
    P = Prog()
    S = P.S
    x_in = P.inp("x", [TL, D])
    g = P.inp("g_bc", [128, D])
    w_up = P.inp("w_up", [D, DFF])
    w_down = P.inp("w_down", [DFF, D])
    y = P.out("y", [TL, D])
    with ExitStack() as es:
        c = load_consts(P, es)
        x_res = P.sb(es, "x_res", [128, NT, D], F32)
        for j in range(NT):
            S.dma(x_res[:, j, :], x_in[j * 128:(j + 1) * 128, :], w=[("x", j)])
        emit_mlp(P, c, es, x_res, g, w_up, w_down)
        for j in range(NT):
            S.dma(y[j * 128:(j + 1) * 128, :], x_res[:, j, :], r=[("x", j)], w=[("y", j)])
        nc = P.close([("y", j) for j in range(NT)])
    return nc


def shard_tokens(a):
    b = a.reshape((NT, NCORES, 128) + a.shape[1:])
    return [np.ascontiguousarray(b[:, c].reshape((TL,) + a.shape[1:])) for c in range(NCORES)]


def unshard_tokens(parts):
    a = np.stack([p.reshape((NT, 128) + p.shape[1:]) for p in parts], axis=1)
    return np.ascontiguousarray(a.reshape((T,) + parts[0].shape[1:]))


def bc128(v):
    return np.ascontiguousarray(np.broadcast_to(np.asarray(v, np.float32).reshape(1, -1), (128, v.size)))


IDENT = np.eye(128, dtype=np.float32)


def emit_linear_tm_res(P, ws, AT, atkey, nkc, w_dram, x_res):
    S = P.S
    nkp = nkc // 8
    for half in range(2):
        def piece(cc, kp):
            def f(buf):
                dst = buf[:].rearrange("p (k n) -> p k n", n=512)
                src = w_dram[kp * 1024:(kp + 1) * 1024, cc * 512:(cc + 1) * 512].rearrange(
                    "(k p) n -> p k n", p=128)
                return dst, src
            return f
        ws.plan([piece(cc, kp) for cc in range(4) for kp in range(nkp)])
        for cc in range(4):
            for kp in range(nkp):
                buf, bkey = ws.next()
                wv = buf[:].rearrange("p (k n) -> p k n", n=512)
                for t in range(4):
                    j = half * 4 + t
                    for k in range(8):
                        kc = kp * 8 + k
                        S.pe(lambda e, t=t, j=j, k=k, kc=kc, wv=wv: e.matmul(
                            P.ps[t][:], lhsT=AT[:, kc, j * 128:(j + 1) * 128], rhs=wv[:, k, :],
                            start=(kc == 0), stop=(kc == nkc - 1)),
                            r=[bkey, atkey], w=[("ps", t)])
            for t in range(4):
                j = half * 4 + t
                xs = x_res[:, j, cc * 512:(cc + 1) * 512]
                S.dve(lambda e, xs=xs, t=t: e.tensor_tensor(out=xs, in0=xs, in1=P.ps[t][:], op=ALU.add),
                      r=[("ps", t), ("x", j)], w=[("x", j)])


def load_x(P, x_res, x_in):
    for j in range(NT):
        P.S.dma(x_res[:, j, :], x_in[j * 128:(j + 1) * 128, :], w=[("x", j)])


def store_x(P, y, x_res):
    for j in range(NT):
        P.S.dma(y[j * 128:(j + 1) * 128, :], x_res[:, j, :], r=[("x", j)], w=[("y", j)])
    return [("y", j) for j in range(NT)]


GELU_C = 0.044715
GELU_S = 1.5957691216057308


def build_R1(stage=99):
    P = Prog()
    S = P.S
    x9 = P.inp("x9", [9 * 128, D])
    g = P.inp("g_bc", [128, D])
    w_in = P.inp("w_in", [D, 2 * D])
    cw_d = P.inp("cw", [128, KC * 4])
    vec_d = P.inp("vecs", [128, KC * 4])
    wa_d = P.inp("wa", [8 * 256, 256])
    wx_d = P.inp("wx", [8 * 256, 256])
    gel_o = P.out("gel", [128, KC * TL])
    hl_o = P.out("hloc", [128, KC * TL])
    pp_o = P.out("pp", [128, KC * TL])
    ab_o = P.out("ab", [128, KC * 16])
    with ExitStack() as es:
        c = load_consts(P, es)
        gbc = P.sb(es, "gbc", [128, D], F32)
        S.dma(gbc[:], g, w=["gbc"])
        wk = norm_work(P, es)
        hT = P.sb(es, "hT9", [128, KC, 9 * 128], BF16)
        xt = [P.sb(es, "xt", [128, D], F32) for _ in range(2)]
        for t in range(9):
            S.dma(xt[t % 2][:], x9[t * 128:(t + 1) * 128, :], w=[("xt", t % 2)])
            emit_norm_T(P, c, xt[t % 2][:], ("xt", t % 2), gbc[:], hT, "hT", t * 128, wk)
        cw = P.sb(es, "cw", [128, KC, 4], F32)
        vec = P.sb(es, "vec", [128, KC, 4], F32)
        S.dma(cw[:], cw_d.rearrange("p (k n) -> p k n", n=4), w=["cw"])
        S.dma(vec[:], vec_d.rearrange("p (k n) -> p k n", n=4), w=["vec"])
        wa = P.sb(es, "wa", [128, 8, 2, 256], BF16)
        wx = P.sb(es, "wx", [128, 8, 2, 256], BF16)
        for n in range(8):
            S.dma(wa[:, n], wa_d[n * 256:(n + 1) * 256, :].rearrange("(k p) c -> p k c", p=128), w=["wa"], q="pool")
            S.dma(wx[:, n], wx_d[n * 256:(n + 1) * 256, :].rearrange("(k p) c -> p k c", p=128), w=["wx"], q="pool")
        one = P.sb(es, "one", [128, 1], F32)
        S.pool(lambda e: e.memset(one[:], 1.0), w=["one"])
        cl = P.sb(es, "cl", [128, KC], F32)
        S.act(lambda e: e.activation(out=cl[:], in_=vec[:, :, 3], func=AF.Exp, scale=-1.0), r=["vec"], w=["cl"])
        S.act(lambda e: e.activation(out=cl[:], in_=cl[:], func=AF.Ln, bias=one[:], scale=1.0),
              r=["cl", "one"], w=["cl"])
        S.dve(lambda e: e.tensor_scalar(cl[:], cl[:], -8.0, None, ALU.mult), r=["cl"], w=["cl"])
        if stage == 1:
            return P.close([])
        ws = WStream(P, es, "wrin", [128, 4096], nbuf=3)
        f4 = lambda name: P.sb(es, name, [128, TL], F32)
        gelb = [f4("gelb"), f4("gelb")]
        hlb = [f4("hlb"), f4("hlb")]
        ppb = [f4("ppb"), f4("ppb")]
        rr, ii, aa, a2, bb, ap_, dd = f4("rr"), f4("ii"), f4("aa"), f4("a2"), f4("bb"), f4("ap"), f4("dd")
        t1 = [P.sb(es, "t1", [128, 512], F32) for _ in range(2)]
        xrx = P.sb(es, "xrx", [128, 2, 8, 131], F32)
        xc = P.sb(es, "xc", [128, 2, TL], F32)
        xcb = P.sb(es, "xcb", [128, 2, TL], BF16)
        ab = P.sb(es, "ab", [128, KC, 2, 8], F32)
        S.pool(lambda e: e.memset(dd[:], 0.0), w=["dd"])

        def piece(col0):
            def f(buf):
                dst = buf[:].rearrange("p (k n) -> p k n", n=256)
                src = w_in[:, col0:col0 + 256].rearrange("(k p) n -> p k n", p=128)
                return dst, src
            return f
        pcs = []
        for n in range(8):
            pcs.append(piece(n * 256))
            pcs.append(piece(D + n * 256))
        ws.plan(pcs)
        for n in range(8):
            buf, bkey = ws.next()
            wv = buf[:].rearrange("p (k n) -> p k n", n=256)
            for s in range(2):
                ch = 2 * n + s
                gb = gelb[ch % 2]
                for tc in range(2):
                    pb = P.ps[tc]
                    for kc in range(KC):
                        S.pe(lambda e, kc=kc, s=s, tc=tc, pb=pb, wv=wv: e.matmul(
                            pb[:], lhsT=wv[:, kc, s * 128:(s + 1) * 128], rhs=hT[:, kc, tc * 512:(tc + 1) * 512],
                            start=(kc == 0), stop=(kc == KC - 1)), r=[bkey, "hT"], w=[("ps", tc)])
                    tt = t1[tc]
                    S.act(lambda e, tt=tt, pb=pb: e.activation(out=tt[:], in_=pb[:], func=AF.Square),
                          r=[("ps", tc)], w=[("t1", tc)])
                    S.dve(lambda e, tt=tt: e.tensor_scalar(tt[:], tt[:], GELU_C, 1.0, ALU.mult, ALU.add),
                          r=[("t1", tc)], w=[("t1", tc)])
                    S.dve(lambda e, tt=tt, pb=pb: e.tensor_tensor(out=tt[:], in0=tt[:], in1=pb[:], op=ALU.mult),
                          r=[("t1", tc), ("ps", tc)], w=[("t1", tc)])
                    S.act(lambda e, tt=tt: e.activation(out=tt[:], in_=tt[:], func=AF.Sigmoid, scale=GELU_S),
                          r=[("t1", tc)], w=[("t1", tc)])
                    gs = gb[:, tc * 512:(tc + 1) * 512]
                    S.dve(lambda e, tt=tt, pb=pb, gs=gs: e.tensor_tensor(out=gs, in0=tt[:], in1=pb[:], op=ALU.mult),
                          r=[("t1", tc), ("ps", tc)], w=[("gelb", ch % 2)])
                S.dma(gel_o[:, ch * TL:(ch + 1) * TL], gb[:], r=[("gelb", ch % 2)], w=[("gel_o", ch)])
            if stage == 2:
                return P.close([("gel_o", 0), ("gel_o", 1)])
            buf, bkey = ws.next()
            wv = buf[:].rearrange("p (k n) -> p k n", n=256)
            for s in range(2):
                ch = 2 * n + s
                for tc in range(3):
                    pb = P.ps[2 + tc]
                    rhs_of = (lambda kc, tc=tc: hT[:, kc, tc * 512:(tc + 1) * 512]) if tc < 2 else \
                        (lambda kc: hT[:, kc, 1024:1152])
                    po = pb[:] if tc < 2 else pb[:, 0:128]
                    for kc in range(KC):
                        S.pe(lambda e, kc=kc, s=s, po=po, wv=wv, rhs_of=rhs_of: e.matmul(
                            po, lhsT=wv[:, kc, s * 128:(s + 1) * 128], rhs=rhs_of(kc),
                            start=(kc == 0), stop=(kc == KC - 1)), r=[bkey, "hT"], w=[("ps", 2 + tc)])
                    if tc < 2:
                        S.act(lambda e, s=s, tc=tc, pb=pb: e.copy(
                            out=xrx[:, s, tc * 4:(tc + 1) * 4, 3:131],
                            in_=pb[:].rearrange("p (a b) -> p a b", b=128)),
                            r=[("ps", 2 + tc)], w=["xrx"])
                    else:
                        S.act(lambda e, s=s, pb=pb: e.copy(
                            out=xrx[:, s, :, 0:3],
                            in_=pb[:, 0:128].rearrange("p (a b) -> p a b", b=16)[:, :, 13:16]),
                            r=[("ps", 2 + tc)], w=["xrx"])
                xcv = xc[:, s, :].rearrange("p (a b) -> p a b", b=128)
                S.dve(lambda e, s=s, ch=ch, xcv=xcv: e.tensor_scalar(
                    xcv, xrx[:, s, :, 0:128], cw[:, ch, 0:1], vec[:, ch, 0:1], ALU.mult, ALU.add),
                    r=["xrx", "cw", "vec"], w=["xc"])
                for i in range(1, 4):
                    S.dve(lambda e, s=s, ch=ch, i=i, xcv=xcv: e.scalar_tensor_tensor(
                        out=xcv, in0=xrx[:, s, :, i:i + 128], scalar=cw[:, ch, i:i + 1], in1=xcv,
                        op0=ALU.mult, op1=ALU.add), r=["xrx", "cw", "xc"], w=["xc"])
                S.pool(lambda e, s=s: e.tensor_copy(out=xcb[:, s, :], in_=xc[:, s, :]), r=["xc"], w=["xcb"])
            if stage == 3:
                return P.close([("gel_o", 0), ("gel_o", 1)])
            for s in range(2):
                ch = 2 * n + s
                for (wg, dst, bcol, pbase, nm) in ((wa, rr, 1, 0, "rr"), (wx, ii, 2, 2, "ii")):
                    for tc in range(2):
                        pb = P.ps[pbase + tc]
                        for k in range(2):
                            S.pe(lambda e, k=k, s=s, tc=tc, pb=pb, wg=wg: e.matmul(
                                pb[:], lhsT=wg[:, n, k, s * 128:(s + 1) * 128], rhs=xcb[:, k, tc * 512:(tc + 1) * 512],
                                start=(k == 0), stop=(k == 1)), r=["wa", "wx", "xcb"], w=[("ps", pbase + tc)])
                        S.act(lambda e, tc=tc, pb=pb, dst=dst, bcol=bcol, ch=ch: e.activation(
                            out=dst[:, tc * 512:(tc + 1) * 512], in_=pb[:], func=AF.Sigmoid,
                            bias=vec[:, ch, bcol:bcol + 1], scale=1.0), r=[("ps", pbase + tc), "vec"], w=[nm])
                S.act(lambda e, ch=ch: e.activation(out=aa[:], in_=rr[:], func=AF.Exp, scale=cl[:, ch:ch + 1]),
                      r=["rr", "cl"], w=["aa"])
                S.pool(lambda e: e.tensor_tensor(out=a2[:], in0=aa[:], in1=aa[:], op=ALU.mult), r=["aa"], w=["a2"])
                S.act(lambda e: e.activation(out=a2[:], in_=a2[:], func=AF.Sqrt, bias=one[:], scale=-1.0),
                      r=["a2", "one"], w=["a2"])
                S.dve(lambda e, s=s: e.tensor_tensor(out=bb[:], in0=xc[:, s, :], in1=ii[:], op=ALU.mult),
                      r=["xc", "ii"], w=["bb"])
                S.dve(lambda e: e.tensor_tensor(out=bb[:], in0=bb[:], in1=a2[:], op=ALU.mult),
                      r=["bb", "a2"], w=["bb"])
                a3 = aa[:].rearrange("p (a b) -> p a b", b=128)
                ap3 = ap_[:].rearrange("p (a b) -> p a b", b=128)
                d3 = dd[:].rearrange("p (a b) -> p a b", b=128)
                S.pool(lambda e: e.tensor_copy(out=ap_[:], in_=aa[:]), r=["aa"], w=["ap"])
                S.pool(lambda e, ap3=ap3: e.memset(ap3[:, :, 0:1], 0.0), w=["ap"])
                S.pool(lambda e, d3=d3, a3=a3: e.tensor_copy(out=d3[:, :, 0:1], in_=a3[:, :, 0:1]),
                       r=["aa"], w=["dd"])
                hb, pb_ = hlb[ch % 2], ppb[ch % 2]
                S.dve(lambda e, hb=hb: e.tensor_tensor_scan(out=hb[:], data0=ap_[:], data1=bb[:], initial=0.0,
                                                            op0=ALU.mult, op1=ALU.add),
                      r=["ap", "bb"], w=[("hlb", ch % 2)])
                S.dve(lambda e, pb_=pb_: e.tensor_tensor_scan(out=pb_[:], data0=ap_[:], data1=dd[:], initial=0.0,
                                                              op0=ALU.mult, op1=ALU.add),
                      r=["ap", "dd"], w=[("ppb", ch % 2)])
                h3 = hb[:].rearrange("p (a b) -> p a b", b=128)
                p3 = pb_[:].rearrange("p (a b) -> p a b", b=128)
                S.pool(lambda e, ch=ch, p3=p3: e.tensor_copy(out=ab[:, ch, 0, :], in_=p3[:, :, 127]),
                       r=[("ppb", ch % 2)], w=["ab"])
                S.pool(lambda e, ch=ch, h3=h3: e.tensor_copy(out=ab[:, ch, 1, :], in_=h3[:, :, 127]),
                       r=[("hlb", ch % 2)], w=["ab"])
                S.dma(hl_o[:, ch * TL:(ch + 1) * TL], hb[:], r=[("hlb", ch % 2)], w=[("hl_o", ch)])
                S.dma(pp_o[:, ch * TL:(ch + 1) * TL], pb_[:], r=[("ppb", ch % 2)], w=[("pp_o", ch)])
        S.dma(ab_o, ab[:].rearrange("p a b c -> p (a b c)"), r=["ab"], w=["ab_o"])
        keys = ["ab_o"] + [(k, ch) for k in ("gel_o", "hl_o", "pp_o") for ch in range(KC)]
        nc = P.close(keys)
    return nc


def build_R2(final=False):
    P = Prog()
    S = P.S
    x_in = P.inp("x", [TL, D])
    gel_d = P.inp("gel", [128, KC * TL])
    hl_d = P.inp("hloc", [128, KC * TL])
    pp_d = P.inp("pp", [128, KC * TL])
    abg_d = P.inp("abg", [128, KC * 2 * 64])
    sel_d = P.inp("selb", [128, 8 * 64])
    w_out = P.inp("w_out", [D, D])
    g2 = P.inp("g_bc", [128, D])
    w_up = P.inp("w_up", [D, DFF])
    w_down = P.inp("w_down", [DFF, D])
    y = P.out("y", [TL, D])
    with ExitStack() as es:
        c = load_consts(P, es)
        x_res = P.sb(es, "x_res", [128, NT, D], F32)
        load_x(P, x_res, x_in)
        with ExitStack() as es2:
            yT = P.sb(es2, "yT", [128, KC, TL], BF16)
            abg = P.sb(es2, "abg", [128, KC, 2, 64], F32)
            sel = P.sb(es2, "sel", [128, 8, 64], F32)
            hs = P.sb(es2, "hs", [128, KC, 64], F32)
            carry = P.sb(es2, "carry", [128, KC, 8], F32)
            junk = P.sb(es2, "junk", [128, 64], F32)
            S.dma(abg[:], abg_d.rearrange("p (a b c) -> p a b c", b=2, c=64), w=["abg"])
            S.dma(sel[:], sel_d.rearrange("p (a b) -> p a b", b=64), w=["sel"])
            for ch in range(KC):
                S.dve(lambda e, ch=ch: e.tensor_tensor_scan(out=hs[:, ch, :], data0=abg[:, ch, 0, :],
                                                            data1=abg[:, ch, 1, :], initial=0.0,
                                                            op0=ALU.mult, op1=ALU.add),
                      r=["abg"], w=["hs"])
            for ch in range(KC):
                for j in range(NT):
                    S.dve(lambda e, ch=ch, j=j: e.scalar_tensor_tensor(
                        out=junk[:], in0=hs[:, ch, :], scalar=1.0, in1=sel[:, j, :], op0=ALU.mult, op1=ALU.mult,
                        accum_out=carry[:, ch, j:j + 1]), r=["hs", "sel"], w=["junk", "carry"])
            f4 = lambda name: P.sb(es2, name, [128, TL], F32)
            gb = [f4("gb"), f4("gb")]
            hb = [f4("hb"), f4("hb")]
            pb = [f4("pb"), f4("pb")]
            for ch in range(KC):
                q = ch % 2
                S.dma(gb[q][:], gel_d[:, ch * TL:(ch + 1) * TL], w=[("gb", q)])
                S.dma(hb[q][:], hl_d[:, ch * TL:(ch + 1) * TL], w=[("hb", q)])
                S.dma(pb[q][:], pp_d[:, ch * TL:(ch + 1) * TL], w=[("pb", q)])
                for j in range(NT):
                    sl = slice(j * 128, (j + 1) * 128)
                    S.dve(lambda e, q=q, ch=ch, j=j, sl=sl: e.scalar_tensor_tensor(
                        out=hb[q][:, sl], in0=pb[q][:, sl], scalar=carry[:, ch, j:j + 1], in1=hb[q][:, sl],
                        op0=ALU.mult, op1=ALU.add), r=[("pb", q), ("hb", q), "carry"], w=[("hb", q)])
                S.pool(lambda e, q=q, ch=ch: e.tensor_tensor(out=yT[:, ch, :], in0=hb[q][:], in1=gb[q][:],
                                                             op=ALU.mult),
                       r=[("hb", q), ("gb", q)], w=["yT"])
            ws = WStream(P, es2, "wout", [128, 4096], nbuf=3)
            emit_linear_tm_res(P, ws, yT, "yT", KC, w_out, x_res)
            S.barrier()
        emit_mlp(P, c, es, x_res, g2, w_up, w_down)
        keys = store_x(P, y, x_res)
        nc = P.close(keys)
    return nc


def halo_tile(xg, c):
    out = np.zeros((128, xg.shape[1]), np.float32)
    for j in range(NT):
        t0 = (8 * j + c) * 128 - 16
        if t0 >= 0:
            out[j * 16:(j + 1) * 16] = xg[t0:t0 + 16]
    return out


def pk(v):
    return np.ascontiguousarray(np.asarray(v, np.float32).reshape(KC, 128).T)


def r1_inputs(xg, d, j):
    xs = shard_tokens(xg)
    cw = np.stack([pk(d["rnn_conv_w"][j][i]) for i in range(4)], -1).reshape(128, KC * 4)
    vecs = np.stack([pk(d["rnn_conv_b"][j]), pk(d["rnn_gate_a_b"][j]), pk(d["rnn_gate_x_b"][j]),
                     pk(d["rnn_lambda"][j])], -1).reshape(128, KC * 4)
    common = {
        "g_bc": bc128(d["rnn_norm"][j]), "w_in": np.ascontiguousarray(d["rnn_w_in"][j]),
        "cw": np.ascontiguousarray(cw), "vecs": np.ascontiguousarray(vecs),
        "wa": np.ascontiguousarray(d["rnn_gate_a_w"][j].reshape(8 * 256, 256)),
        "wx": np.ascontiguousarray(d["rnn_gate_x_w"][j].reshape(8 * 256, 256)),
        "c_ident": IDENT,
    }
    return [dict(common, x9=np.concatenate([xs[c], halo_tile(xg, c)], 0)) for c in range(NCORES)]


def r2_inputs(xg, r1, d, j, li):
    xs = shard_tokens(xg)
    ab = np.stack([r1[c]["ab"].reshape(128, KC, 2, NT) for c in range(NCORES)], -1)
    abg = np.ascontiguousarray(ab.reshape(128, KC * 2 * 64))
    common = {
        "abg": abg, "w_out": np.ascontiguousarray(d["rnn_w_out"][j]), "g_bc": bc128(d["mlp_norm"][li]),
        "w_up": np.ascontiguousarray(d["mlp_w_up"][li]), "w_down": np.ascontiguousarray(d["mlp_w_down"][li]),
        "c_ident": IDENT,
    }
    ins = []
    for c in range(NCORES):
        sel = np.zeros((NT, 64), np.float32)
        for jj in range(NT):
            b = 8 * jj + c - 1
            if b >= 0:
                sel[jj, b] = 1.0
        selb = np.ascontiguousarray(np.broadcast_to(sel.reshape(1, NT * 64), (128, NT * 64)))
        ins.append(dict(common, x=xs[c], gel=r1[c]["gel"], hloc=r1[c]["hloc"], pp=r1[c]["pp"], selb=selb))
    return ins


TWO_PI = 6.283185307179586
CW1 = 6.28125
CW2 = TWO_PI - CW1
PI = 3.141592653589793


def emit_rope_tables(P, es, posb_d, invf_d):
    S = P.S
    posi = P.sb(es, "posi", [128, TL], I32)
    posf = P.sb(es, "posf", [128, TL], F32)
    invf = P.sb(es, "invf", [128, 2], F32)
    ang = P.sb(es, "ang", [128, TL], F32)
    kf = P.sb(es, "kf", [128, TL], F32)
    ki = P.sb(es, "ki", [128, TL], I32)
    mm = P.sb(es, "mm", [128, TL], F32)
    cosT = P.sb(es, "cosT", [128, 2, TL], F32)
    sinT = P.sb(es, "sinT", [128, 2, TL], F32)
    S.dma(posi[:], posb_d, w=["posi"])
    S.dma(invf[:], invf_d, w=["invf"])
    S.dve(lambda e: e.tensor_copy(out=posf[:], in_=posi[:]), r=["posi"], w=["posf"])

    def wrap(buf, key):
        S.dve(lambda e: e.tensor_single_scalar(out=mm[:], in_=buf, scalar=PI, op=ALU.is_gt), r=[key], w=["mm"])
        S.dve(lambda e: e.scalar_tensor_tensor(out=buf, in0=mm[:], scalar=-TWO_PI, in1=buf, op0=ALU.mult, op1=ALU.add),
              r=["mm", key], w=[key])
        S.dve(lambda e: e.tensor_single_scalar(out=mm[:], in_=buf, scalar=-PI, op=ALU.is_lt), r=[key], w=["mm"])
        S.dve(lambda e: e.scalar_tensor_tensor(out=buf, in0=mm[:], scalar=TWO_PI, in1=buf, op0=ALU.mult, op1=ALU.add),
              r=["mm", key], w=[key])

    for t in range(2):
        S.dve(lambda e, t=t: e.tensor_scalar(ang[:], posf[:], invf[:, t:t + 1], None, ALU.mult),
              r=["posf", "invf"], w=["ang"])
        S.dve(lambda e: e.tensor_scalar(kf[:], ang[:], 1.0 / TWO_PI, None, ALU.mult), r=["ang"], w=["kf"])
        S.dve(lambda e: e.tensor_copy(out=ki[:], in_=kf[:]), r=["kf"], w=["ki"])
        S.dve(lambda e: e.tensor_copy(out=kf[:], in_=ki[:]), r=["ki"], w=["kf"])
        S.dve(lambda e: e.scalar_tensor_tensor(out=ang[:], in0=kf[:], scalar=-CW1, in1=ang[:], op0=ALU.mult, op1=ALU.add),
              r=["kf", "ang"], w=["ang"])
        S.dve(lambda e: e.scalar_tensor_tensor(out=ang[:], in0=kf[:], scalar=-CW2, in1=ang[:], op0=ALU.mult, op1=ALU.add),
              r=["kf", "ang"], w=["ang"])
        wrap(ang[:], "ang")
        S.act(lambda e, t=t: e.activation(out=sinT[:, t, :], in_=ang[:], func=AF.Sin), r=["ang"], w=["sinT"])
        S.dve(lambda e: e.tensor_scalar(ang[:], ang[:], PI / 2, None, ALU.add), r=["ang"], w=["ang"])
        wrap(ang[:], "ang")
        S.act(lambda e, t=t: e.activation(out=cosT[:, t, :], in_=ang[:], func=AF.Sin), r=["ang"], w=["cosT"])
    return cosT, sinT


def emit_attn_inproj(P, c, es, hT, w_in, d_in, d_out):
    S = P.S
    cosT, sinT = emit_rope_tables(P, es, d_in["posb"], d_in["invf"])
    rt = P.sb(es, "rt", [128, 2, 128], BF16)
    S.dma(rt[:, 0, :], d_in["rt"][0:128, :], w=["rt"], q="pool")
    S.dma(rt[:, 1, :], d_in["rt"][128:256, :], w=["rt"], q="pool")
    qkg = P.sb(es, "qkg", [128, 2], F32)
    S.dma(qkg[:], d_in["qkg"], w=["qkg"])
    epsh = P.sb(es, "epsh", [128, 1], F32)
    S.pool(lambda e: e.memset(epsh[:], EPS), w=["epsh"])
    sqb2 = [P.sb(es, "sqb", [128, 512], BF16) for _ in range(2)]
    rsb2 = [P.sb(es, "rsb", [128, 512], F32) for _ in range(2)]
    xnb2 = [P.sb(es, "xnb", [128, 512], BF16) for _ in range(2)]
    t12 = [P.sb(es, "t1", [128, 512], F32) for _ in range(2)]
    t22 = [P.sb(es, "t2", [128, 512], F32) for _ in range(2)]
    ob = [P.sb(es, "ob", [128, 512], BF16) for _ in range(2)]
    ws = WStream(P, es, "wain", [128, 4096], nbuf=3)

    def head_chunk(wv, bkey, s, kind, dst_of_tc):
        tab = 0 if kind in ("q", "k") else 1
        for tc in range(2):
            pb = P.ps[tc]
            sqb, rsb, xnb, t1, t2 = sqb2[tc], rsb2[tc], xnb2[tc], t12[tc], t22[tc]
            ksq, krs, kxn, kt1, kt2 = ("sqb", tc), ("rsb", tc), ("xnb", tc), ("t1", tc), ("t2", tc)
            pss, psr = P.ps[2 + tc], P.ps[4 + tc]
            kss, ksr = ("ps", 2 + tc), ("ps", 4 + tc)
            for kc in range(KC):
                S.pe(lambda e, kc=kc, pb=pb: e.matmul(
                    pb[:], lhsT=wv[:, kc, s * 128:(s + 1) * 128], rhs=hT[:, kc, tc * 512:(tc + 1) * 512],
                    start=(kc == 0), stop=(kc == KC - 1)), r=[bkey, "hT"], w=[("ps", tc)])
            tsl = slice(tc * 512, (tc + 1) * 512)
            if kind in ("q", "k"):
                gcol = 0 if kind == "q" else 1
                S.act(lambda e, pb=pb, sqb=sqb: e.activation(out=sqb[:], in_=pb[:], func=AF.Square), r=[("ps", tc)], w=[ksq])
                S.pe(lambda e, sqb=sqb, pss=pss: e.matmul(pss[:], lhsT=c["ones"][:], rhs=sqb[:], start=True, stop=True),
                     r=["ones", ksq], w=[kss])
                S.act(lambda e, rsb=rsb, pss=pss: e.activation(out=rsb[:], in_=pss[:], func=AF.Sqrt, bias=epsh[:], scale=1.0 / 128),
                      r=[kss, "epsh"], w=[krs])
                S.dve(lambda e, rsb=rsb: e.reciprocal(out=rsb[:], in_=rsb[:]), r=[krs], w=[krs])
                S.dve(lambda e, pb=pb, gcol=gcol, xnb=xnb, rsb=rsb: e.scalar_tensor_tensor(
                    out=xnb[:], in0=pb[:], scalar=qkg[:, gcol:gcol + 1], in1=rsb[:], op0=ALU.mult, op1=ALU.mult),
                    r=[("ps", tc), "qkg", krs], w=[kxn])
            else:
                S.act(lambda e, pb=pb, xnb=xnb: e.copy(out=xnb[:], in_=pb[:]), r=[("ps", tc)], w=[kxn])
            S.pe(lambda e, tab=tab, xnb=xnb, psr=psr: e.matmul(psr[:], lhsT=rt[:, tab, :], rhs=xnb[:], start=True, stop=True),
                 r=["rt", kxn], w=[ksr])
            S.dve(lambda e, tab=tab, tsl=tsl, t1=t1, xnb=xnb: e.tensor_tensor(out=t1[:], in0=xnb[:], in1=cosT[:, tab, tsl], op=ALU.mult),
                  r=[kxn, "cosT"], w=[kt1])
            S.dve(lambda e, tab=tab, tsl=tsl, t2=t2, psr=psr: e.tensor_tensor(out=t2[:], in0=psr[:], in1=sinT[:, tab, tsl], op=ALU.mult),
                  r=[ksr, "sinT"], w=[kt2])
            o = ob[tc]
            S.pool(lambda e, o=o, t1=t1, t2=t2: e.tensor_tensor(out=o[:], in0=t1[:], in1=t2[:], op=ALU.add),
                   r=[kt1, kt2], w=[("ob", tc)])
            S.dma(dst_of_tc(tc), o[:], r=[("ob", tc)], w=[("hp_out", P.uid)])
            P.uid += 1

    def piece(col0, ncols):
        def f(buf):
            dst = buf[:, 0:KC * ncols].rearrange("p (k n) -> p k n", n=ncols)
            src = w_in[:, col0:col0 + ncols].rearrange("(k p) n -> p k n", p=128)
            return dst, src
        return f

    groups = [("q", 0, 16, d_out["qT"]), ("k", 2048, 4, d_out["kT"]), ("iq", 3072, 16, d_out["iqT"])]
    pcs = []
    for kind, col0, nch, _ in groups:
        for i in range(nch // 2):
            pcs.append(piece(col0 + i * 256, 256))
    pcs.append(piece(5120, 144))
    ws.plan(pcs)
    for kind, col0, nch, dst in groups:
        for i in range(nch // 2):
            buf, bkey = ws.next()
            wv = buf[:].rearrange("p (k n) -> p k n", n=256)
            for s in range(2):
                hh = 2 * i + s
                head_chunk(wv, bkey, s, kind,
                           lambda tc, hh=hh, dst=dst: dst[:, hh * TL + tc * 512: hh * TL + (tc + 1) * 512])
    buf, bkey = ws.next()
    wv = buf[:, 0:KC * 144].rearrange("p (k n) -> p k n", n=144)
    head_chunk(wv, bkey, 0, "ik", lambda tc: d_out["ikT"][:, tc * 512:(tc + 1) * 512])
    iwsb = P.sb(es, "iwsb", [128, NT, 16], F32)
    for t in range(NT):
        for kc in range(KC):
            S.pe(lambda e, kc=kc, t=t: e.matmul(P.ps[4][:, t * 16:(t + 1) * 16], lhsT=hT[:, kc, t * 128:(t + 1) * 128],
                                                rhs=wv[:, kc, 128:144], start=(kc == 0), stop=(kc == KC - 1)),
                 r=[bkey, "hT"], w=[("ps", 4)])
    S.act(lambda e: e.copy(out=iwsb[:], in_=P.ps[4][:, 0:NT * 16].rearrange("p (a b) -> p a b", b=16)),
          r=[("ps", 4)], w=["iwsb"])
    S.dma(d_out["iw"], iwsb[:].rearrange("p a b -> p (a b)"), r=["iwsb"], w=["iw_out"])
    vsb = [P.sb(es, "vsb", [128, 512], BF16) for _ in range(2)]

    def vpiece(kp):
        def f(buf):
            dst = buf[:].rearrange("p (k n) -> p k n", n=512)
            src = w_in[kp * 1024:(kp + 1) * 1024, 2560:3072].rearrange("(k p) n -> p k n", p=128)
            return dst, src
        return f
    for half in range(2):
        ws.plan([vpiece(0), vpiece(1)])
        for kp in range(2):
            buf, bkey = ws.next()
            wv = buf[:].rearrange("p (k n) -> p k n", n=512)
            for t in range(4):
                j = half * 4 + t
                for k in range(8):
                    kc = kp * 8 + k
                    S.pe(lambda e, t=t, j=j, k=k, kc=kc, wv=wv: e.matmul(
                        P.ps[t][:], lhsT=hT[:, kc, j * 128:(j + 1) * 128], rhs=wv[:, k, :],
                        start=(kc == 0), stop=(kc == KC - 1)), r=[bkey, "hT"], w=[("ps", t)])
        for t in range(4):
            j = half * 4 + t
            S.act(lambda e, t=t: e.copy(out=vsb[t % 2][:], in_=P.ps[t][:]), r=[("ps", t)], w=[("vsb", t % 2)])
            S.dma(d_out["v"][j * 128:(j + 1) * 128, :], vsb[t % 2][:], r=[("vsb", t % 2)], w=[("v_out", j)])
    keys = ["iw_out"] + [("v_out", j) for j in range(NT)]
    return keys


def attn_a1_io(P):
    d_in = {"posb": P.inp("posb", [128, TL], I32), "invf": P.inp("invf", [128, 2]),
            "rt": P.inp("rt", [256, 128]), "qkg": P.inp("qkg", [128, 2])}
    d_out = {"qT": P.out("qT", [128, 16 * TL], BF16), "kT": P.out("kT", [128, 4 * TL], BF16),
             "iqT": P.out("iqT", [128, 16 * TL], BF16), "ikT": P.out("ikT", [128, TL], BF16),
             "iw": P.out("iw", [128, NT * 16]), "v": P.out("v", [TL, 512], BF16)}
    return d_in, d_out


def build_A1():
    P = Prog()
    S = P.S
    x_in = P.inp("x", [TL, D])
    g = P.inp("g_bc", [128, D])
    w_in = P.inp("w_in", [D, A_IN])
    d_in, d_out = attn_a1_io(P)
    with ExitStack() as es:
        c = load_consts(P, es)
        gbc = P.sb(es, "gbc", [128, D], F32)
        S.dma(gbc[:], g, w=["gbc"])
        wk = norm_work(P, es)
        hT = P.sb(es, "hT", [128, KC, TL], BF16)
        xt = [P.sb(es, "xt", [128, D], F32) for _ in range(2)]
        for t in range(NT):
            S.dma(xt[t % 2][:], x_in[t * 128:(t + 1) * 128, :], w=[("xt", t % 2)])
            emit_norm_T(P, c, xt[t % 2][:], ("xt", t % 2), gbc[:], hT, "hT", t * 128, wk)
        keys = emit_attn_inproj(P, c, es, hT, w_in, d_in, d_out)
        S.barrier()
        nc = P.close(keys)
    return nc


def rope_consts():
    inv_h = (1.0 / (10000.0 ** (np.arange(0, 128, 2, dtype=np.float32) / np.float32(128)))).astype(np.float32)
    inv_i = (1.0 / (10000.0 ** (np.arange(0, 64, 2, dtype=np.float32) / np.float32(64)))).astype(np.float32)
    invf = np.zeros((128, 2), np.float32)
    invf[:, 0] = np.concatenate([inv_h, inv_h])
    invf[:64, 1] = np.concatenate([inv_i, inv_i])
    R = np.zeros((128, 128), np.float32)
    for i in range(64):
        R[i, i + 64] = -1.0
        R[i + 64, i] = 1.0
    R2 = np.zeros((128, 128), np.float32)
    for i in range(32):
        R2[i, i + 32] = -1.0
        R2[i + 32, i] = 1.0
    rt = np.concatenate([R.T, R2.T], 0)
    return invf, np.ascontiguousarray(rt)


def a1_inputs(xs, pos, d, j, with_x=True):
    invf, rt = rope_consts()
    ps = shard_tokens(np.asarray(pos).reshape(T))
    qkg = np.ascontiguousarray(np.stack([d["attn_q_norm"][j], d["attn_k_norm"][j]], -1).astype(np.float32))
    ins = []
    for c in range(NCORES):
        m = {"posb": np.ascontiguousarray(np.broadcast_to(ps[c].astype(np.int32).reshape(1, TL), (128, TL))),
             "invf": invf, "rt": rt, "qkg": qkg, "w_in": np.ascontiguousarray(d["attn_w_in"][j]), "c_ident": IDENT}
        if with_x:
            m["x"] = xs[c]
            m["g_bc"] = bc128(d["attn_norm"][j])
        ins.append(m)
    return ins


def emit_attention(P, c, es, d, oT_s):
    S = P.S
    ikT = P.sb(es, "ikT", [128, T], BF16)
    for q in range(4):
        S.dma(ikT[:, q * 2048:(q + 1) * 2048], d["ikTa"][:, q * 2048:(q + 1) * 2048], w=["ikT"])
    iwsb = P.sb(es, "iwsb", [128, NT, 16], F32)
    S.dma(iwsb[:], d["iw"].rearrange("p (a b) -> p a b", b=16), w=["iwsb"])
    cm = P.sb(es, "cm", [128, 1024], F32)
    pen = P.sb(es, "pen", [128, 1024], F32)
    S.dma(cm[:], d["cm"], w=["cm"])
    S.dma(pen[:], d["pen"], w=["pen"])
    score = P.sb(es, "score", [128, T], F32)
    junk = P.sb(es, "junkb", [128, T], BF16)
    maskT = P.sb(es, "maskT", [128, T // 128, 128], BF16)
    mkb = [P.sb(es, "mkb", [128, 512], BF16) for _ in range(2)]
    rl = [P.sb(es, "rl", [128, 512], F32) for _ in range(2)]
    iqtb = [P.sb(es, "iqt", [128, 16, 128], BF16) for _ in range(2)]
    qt = [P.sb(es, "qt", [128, 4, 128], BF16) for _ in range(2)]
    kTb = [P.sb(es, "kTg", [128, T], BF16) for _ in range(2)]
    vgb = [P.sb(es, "vg", [128, T // 128, 128], BF16) for _ in range(2)]
    ptb = [P.sb(es, "ptb", [128, 512], BF16) for _ in range(3)]
    rden = P.sb(es, "rden", [128, 512], F32)
    ot = [P.sb(es, "ot", [128, 512], F32) for _ in range(2)]
    sm = {n: P.sb(es, n, [128, 1], F32) for n in ("M", "lo", "hi", "mid", "cnt", "pred", "dl")}
    iq3 = d["iqT"].rearrange("p (h t) -> p h t", t=TL)
    q3 = d["qT"].rearrange("p (h t) -> p h t", t=TL)
    o3 = oT_s.rearrange("p (h t) -> p h t", t=TL)
    SC = 128.0 ** -0.5
    okeys = []

    def gen_indexer(j):
        Kc = 1024 * (j + 1)
        iqt = iqtb[j % 2]
        S.dma(iqt[:], iq3[:, :, j * 128:(j + 1) * 128], w=[("iqt", j % 2)])
        for k5 in range(Kc // 512):
            sc = score[:, k5 * 512:(k5 + 1) * 512]
            for h in range(16):
                pb = P.ps[h % 2]
                S.pe(lambda e, h=h, pb=pb, k5=k5: e.matmul(pb[:], lhsT=iqt[:, h, :], rhs=ikT[:, k5 * 512:(k5 + 1) * 512],
                                                         start=True, stop=True), r=[("iqt", j % 2), "ikT"], w=[("ps", h % 2)])
                r_ = rl[h % 2]
                S.act(lambda e, pb=pb, r_=r_: e.activation(out=r_[:], in_=pb[:], func=AF.Relu),
                      r=[("ps", h % 2)], w=[("rl", h % 2)])
                if h == 0:
                    S.dve(lambda e, r_=r_, sc=sc: e.tensor_scalar(sc, r_[:], iwsb[:, j, 0:1], None, ALU.mult),
                          r=[("rl", h % 2), "iwsb"], w=["score"])
                else:
                    S.dve(lambda e, r_=r_, sc=sc, h=h: e.scalar_tensor_tensor(
                        out=sc, in0=r_[:], scalar=iwsb[:, j, h:h + 1], in1=sc, op0=ALU.mult, op1=ALU.add),
                        r=[("rl", h % 2), "iwsb", "score"], w=["score"])
                yield

    def post_indexer(j):
        Kc = 1024 * (j + 1)
        S.dve(lambda e: e.tensor_reduce(out=sm["M"][:], in_=score[:, 0:Kc], axis=AX.X, op=ALU.max), r=["score"], w=["M"])
        S.dve(lambda e: e.tensor_reduce(out=sm["cnt"][:], in_=score[:, 0:Kc], axis=AX.X, op=ALU.min), r=["score"], w=["cnt"])
        S.dve(lambda e: e.tensor_tensor(out=sm["dl"][:], in0=sm["M"][:], in1=sm["cnt"][:], op=ALU.subtract),
              r=["M", "cnt"], w=["dl"])
        win = score[:, Kc - 1024:Kc]
        S.dve(lambda e: e.tensor_tensor(out=win, in0=win, in1=cm[:], op=ALU.mult), r=["score", "cm"], w=["score"])
        S.dve(lambda e: e.tensor_tensor(out=win, in0=win, in1=pen[:], op=ALU.add), r=["score", "pen"], w=["score"])
        S.dve(lambda e: e.scalar_tensor_tensor(out=sm["lo"][:], in0=sm["dl"][:], scalar=-0.001, in1=sm["cnt"][:],
                                               op0=ALU.mult, op1=ALU.add), r=["dl", "cnt"], w=["lo"])
        S.dve(lambda e: e.tensor_scalar(sm["lo"][:], sm["lo"][:], -1e-6, None, ALU.add), r=["lo"], w=["lo"])
        S.dve(lambda e: e.tensor_scalar(sm["hi"][:], sm["dl"][:], 1.002, 2e-6, ALU.mult, ALU.add), r=["dl"], w=["hi"])
        Ka = max(128, int(round(Kc * 0.45 / 128)) * 128)
        for it in range(NBIS):
            S.dve(lambda e: e.tensor_scalar(sm["hi"][:], sm["hi"][:], 0.5, None, ALU.mult), r=["hi"], w=["hi"])
            S.dve(lambda e: e.tensor_tensor(out=sm["mid"][:], in0=sm["lo"][:], in1=sm["hi"][:], op=ALU.add),
                  r=["lo", "hi"], w=["mid"])
            S.dve(lambda e: e.tensor_scalar(junk[:, 0:Ka], score[:, 0:Ka], sm["mid"][:], 0.0, ALU.is_ge, ALU.add,
                                            accum_out=sm["cnt"][:]), r=["score", "mid"], w=["junkD", "cnt"])
            S.act(lambda e: e.activation(out=junk[:, Ka:Kc], in_=score[:, Ka:Kc], func=AF.Sign, bias=sm["mid"][:], scale=-1.0,
                                         accum_out=sm["M"][:]), r=["score", "mid"], w=["junkA", "M"])
            S.dve(lambda e: e.scalar_tensor_tensor(out=sm["pred"][:], in0=sm["cnt"][:], scalar=2.0, in1=sm["M"][:],
                                                   op0=ALU.mult, op1=ALU.subtract), r=["cnt", "M"], w=["pred"])
            S.dve(lambda e: e.tensor_scalar(sm["pred"][:], sm["pred"][:], float(2 * TOPK - (Kc - Ka)), None, ALU.is_ge),
                  r=["pred"], w=["pred"])
            S.dve(lambda e: e.scalar_tensor_tensor(out=sm["lo"][:], in0=sm["hi"][:], scalar=sm["pred"][:], in1=sm["lo"][:],
                                                   op0=ALU.mult, op1=ALU.add), r=["hi", "pred", "lo"], w=["lo"])
        for k5 in range(Kc // 512):
            mk = mkb[k5 % 2]
            S.dve(lambda e, mk=mk, k5=k5: e.tensor_scalar(mk[:], score[:, k5 * 512:(k5 + 1) * 512], sm["lo"][:], -30000.0,
                                                          ALU.is_lt, ALU.mult), r=["score", "lo"], w=[("mkb", k5 % 2)])
            pbb = P.psb[k5 % 2]
            for i in range(4):
                S.pe(lambda e, mk=mk, i=i, pbb=pbb: e.transpose(out=pbb[:, i * 128:(i + 1) * 128],
                                                                in_=mk[:, i * 128:(i + 1) * 128], identity=c["ident"][:]),
                     r=[("mkb", k5 % 2), "ident"], w=[("psb", k5 % 2)])
            S.act(lambda e, k5=k5, pbb=pbb: e.copy(out=maskT[:, k5 * 4:(k5 + 1) * 4, :],
                                                   in_=pbb[:, 0:512].rearrange("p (a b) -> p a b", b=128)),
                  r=[("psb", k5 % 2)], w=["maskT"])

    def gen_attention(j):
        Kc = 1024 * (j + 1)
        n1 = Kc // 128
        for g in range(4):
            gi = j * 4 + g
            qg = qt[gi % 2]
            kT = kTb[gi % 2]
            vg = vgb[gi % 2]
            kk, vk = ("kTg", gi % 2), ("vg", gi % 2)
            S.dma(qg[:], q3[:, 4 * g:4 * g + 4, j * 128:(j + 1) * 128], w=[("qt", gi % 2)])
            S.dma(kT[:, 0:Kc], d["kTa"][:, g * T:g * T + Kc], w=[kk])
            for k0 in range(0, n1, 16):
                S.dma(vg[:, k0:k0 + 16, :],
                      d["va"][k0 * 128:(k0 + 16) * 128, g * 128:(g + 1) * 128].rearrange("(k p) e -> p k e", p=128),
                      w=[vk])
            qg2 = qg[:].rearrange("p a b -> p (a b)")

            def st(kc, kT=kT, kk=kk, qg2=qg2, gi=gi):
                pb = P.ps[2 + kc % 2]
                S.pe(lambda e, pb=pb: e.matmul(pb[:], lhsT=kT[:, kc * 128:(kc + 1) * 128], rhs=qg2, start=True, stop=False),
                     r=[kk, ("qt", gi % 2)], w=[("ps", 2 + kc % 2)])
                S.pe(lambda e, pb=pb: e.matmul(pb[:].rearrange("p (a b) -> p a b", b=128), lhsT=c["ident"][:],
                                               rhs=maskT[:, kc:kc + 1, :].to_broadcast([128, 4, 128]), start=False, stop=True),
                     r=["ident", "maskT"], w=[("ps", 2 + kc % 2)])
            st(0)
            for kc in range(n1):
                pb = P.ps[2 + kc % 2]
                pt = ptb[kc % 3]
                S.act(lambda e, pb=pb, pt=pt: e.activation(out=pt[:], in_=pb[:], func=AF.Exp, scale=SC),
                      r=[("ps", 2 + kc % 2)], w=[("ptb", kc % 3)])
                if kc + 1 < n1:
                    st(kc + 1)
                yield
                S.pe(lambda e, kc=kc, pt=pt, vg=vg: e.matmul(P.ps[4][:], lhsT=vg[:, kc, :], rhs=pt[:],
                                                             start=(kc == 0), stop=(kc == n1 - 1)),
                     r=[vk, ("ptb", kc % 3)], w=[("ps", 4)])
                S.pe(lambda e, kc=kc, pt=pt: e.matmul(P.ps[5][:], lhsT=c["ones"][:], rhs=pt[:],
                                                      start=(kc == 0), stop=(kc == n1 - 1)),
                     r=["ones", ("ptb", kc % 3)], w=[("ps", 5)])
                yield
            S.dve(lambda e: e.reciprocal(out=rden[:], in_=P.ps[5][:]), r=[("ps", 5)], w=["rden"])
            o = ot[g % 2]
            S.dve(lambda e, o=o: e.tensor_tensor(out=o[:], in0=P.ps[4][:], in1=rden[:], op=ALU.mult),
                  r=[("ps", 4), "rden"], w=[("ot", g % 2)])
            S.dma(o3[:, 4 * g:4 * g + 4, j * 128:(j + 1) * 128], o[:].rearrange("p (a b) -> p a b", b=128),
                  r=[("ot", g % 2)], w=[("oT_s", j, g)])
            okeys.append(("oT_s", j, g))

    for _ in gen_indexer(0):
        pass
    post_indexer(0)
    for j in range(NT):
        ga = gen_attention(j)
        gx = gen_indexer(j + 1) if j + 1 < NT else iter(())
        da = dx = False
        while not (da and dx):
            if not da:
                try:
                    next(ga)
                except StopIteration:
                    da = True
            if not dx:
                try:
                    next(gx)
                except StopIteration:
                    dx = True
            if not da:
                try:
                    next(ga)
                except StopIteration:
                    da = True
        if j + 1 < NT:
            post_indexer(j + 1)
    return okeys


def build_A2(dbg=False):
    P = Prog()
    S = P.S
    x_in = P.inp("x", [TL, D])
    d = {"qT": P.inp("qT", [128, 16 * TL], BF16), "iqT": P.inp("iqT", [128, 16 * TL], BF16),
         "iw": P.inp("iw", [128, NT * 16]), "kTa": P.inp("kTa", [128, 4 * T], BF16), "va": P.inp("va", [T, 512], BF16),
         "ikTa": P.inp("ikTa", [128, T], BF16),
         "cm": P.inp("cm", [128, 1024]), "pen": P.inp("pen", [128, 1024])}
    w_out = P.inp("w_out", [D, D])
    g2 = P.inp("g_bc", [128, D])
    w_up = P.inp("w_up", [D, DFF])
    w_down = P.inp("w_down", [DFF, D])
    y = P.out("y", [TL, D])
    oT_s = P.out("oT_dbg", [128, 16 * TL]) if dbg else P.scratch("oT_s", [128, 16 * TL])
    with ExitStack() as es:
        c = load_consts(P, es)
        with ExitStack() as es1:
            emit_attention(P, c, es1, d, oT_s)
            S.barrier()
        x_res = P.sb(es, "x_res", [128, NT, D], F32)
        load_x(P, x_res, x_in)
        with ExitStack() as es2:
            oT = P.sb(es2, "oT", [128, KC, TL], BF16)
            S.dma(oT[:], oT_s.rearrange("p (h t) -> p h t", t=TL), w=["oT"], q="pool")
            ws = WStream(P, es2, "wout", [128, 4096], nbuf=3)
            emit_linear_tm_res(P, ws, oT, "oT", KC, w_out, x_res)
            S.barrier()
        emit_mlp(P, c, es, x_res, g2, w_up, w_down)
        keys = store_x(P, y, x_res)
        nc = P.close(keys)
    return nc


def a2_inputs(xs, a1, d, j, li):
    kT = np.stack([a1[c]["kT"].reshape(128, 4, NT, 128) for c in range(NCORES)], 3)
    kTa = np.ascontiguousarray(kT.reshape(128, 4 * T))
    ik = np.stack([a1[c]["ikT"].reshape(128, NT, 128) for c in range(NCORES)], 2)
    ikTa = np.ascontiguousarray(ik.reshape(128, T))
    va = unshard_tokens([a1[c]["v"] for c in range(NCORES)])
    common = {"kTa": kTa, "va": va, "ikTa": ikTa, "w_out": np.ascontiguousarray(d["attn_w_out"][j]),
              "g_bc": bc128(d["mlp_norm"][li]), "w_up": np.ascontiguousarray(d["mlp_w_up"][li]),
              "w_down": np.ascontiguousarray(d["mlp_w_down"][li]), "c_ident": IDENT}
    ins = []
    f = np.arange(1024).reshape(1, 1024)
    p = np.arange(128).reshape(128, 1)
    for c in range(NCORES):
        cm = (f <= 128 * c + p)
        ins.append(dict(common, x=xs[c], qT=a1[c]["qT"], iqT=a1[c]["iqT"], iw=a1[c]["iw"],
                        cm=np.where(cm, np.float32(1.0), np.float32(0.0)).astype(np.float32),
                        pen=np.where(cm, np.float32(0.0), np.float32(-BIG)).astype(np.float32)))
    return ins


POOL_W = (2, 4, 8, 16)


def emit_pool(P, c, es_outer, x_res, hT9, d):
    S = P.S
    with ExitStack() as es:
        hx = P.sb(es, "hx", [128, 8, 144], F32)
        sA = P.sb(es, "sA", [128, 8, 144], F32)
        sB = P.sb(es, "sB", [128, 8, 144], F32)
        yT = P.sb(es, "yTg", [128, 4, TL], BF16)
        rd = P.sb(es, "rd", [128, TL], F32)
        wp = P.sb(es, "wp", [128, 4, 4, 512], BF16)
        bbc = P.sb(es, "bbc", [128, D], F32)
        sbc = P.sb(es, "sbc", [128, D], F32)
        tt = [P.sb(es, "ptt", [128, 512], F32) for _ in range(2)]
        for g in range(4):
            S.dma(wp[:, g], d["pool_w"][g * 512:(g + 1) * 512, :].rearrange("(k p) n -> p k n", p=128), w=["wp"], q="pool")
        S.dma(bbc[:], d["pool_b"], w=["bbc"])
        S.dma(sbc[:], d["pool_s"], w=["sbc"])
        for g in range(4):
            w = POOL_W[g]
            S.dma(rd[:], d["mind"][:, g * TL:(g + 1) * TL], w=["rd"])
            S.dve(lambda e: e.reciprocal(out=rd[:], in_=rd[:]), r=["rd"], w=["rd"])
            rd3 = rd[:].rearrange("p (a b) -> p a b", b=128)
            for ci in range(4):
                ch = 4 * g + ci
                S.act(lambda e, ch=ch: e.copy(out=hx[:, :, 16:144], in_=hT9[:, ch, 0:TL].rearrange("p (a b) -> p a b", b=128)),
                      r=["hT"], w=["hx"])
                S.act(lambda e, ch=ch: e.copy(out=hx[:, :, 0:16], in_=hT9[:, ch, TL:TL + 128].rearrange("p (a b) -> p a b", b=16)),
                      r=["hT"], w=["hx"])
                cur, ck = hx, "hx"
                nxt = [(sA, "sA"), (sB, "sB")]
                step = 1
                k = 0
                while step < w:
                    o, ok = nxt[k % 2]
                    S.dve(lambda e, o=o, cur=cur, step=step: e.tensor_tensor(
                        out=o[:, :, step:144], in0=cur[:, :, step:144], in1=cur[:, :, 0:144 - step], op=ALU.add),
                        r=[ck], w=[ok])
                    cur, ck = o, ok
                    step *= 2
                    k += 1
                S.dve(lambda e, cur=cur, rd3=rd3: e.tensor_tensor(out=cur[:, :, 16:144], in0=cur[:, :, 16:144], in1=rd3,
                                                                 op=ALU.mult), r=[ck, "rd"], w=[ck])
                S.dve(lambda e, cur=cur, ci=ci: e.tensor_tensor(
                    out=yT[:, ci, :].rearrange("p (a b) -> p a b", b=128), in0=cur[:, :, 16:144], in1=hx[:, :, 16:144],
                    op=ALU.subtract), r=[ck, "hx"], w=["yTg"])
            for j in range(NT):
                pb = P.ps[j % 4]
                for kc in range(4):
                    S.pe(lambda e, kc=kc, j=j, pb=pb, g=g: e.matmul(pb[:], lhsT=yT[:, kc, j * 128:(j + 1) * 128],
                                                                   rhs=wp[:, g, kc, :], start=(kc == 0), stop=(kc == 3)),
                         r=["yTg", "wp"], w=[("ps", j % 4)])
                t_ = tt[j % 2]
                gs = slice(g * 512, (g + 1) * 512)
                S.dve(lambda e, t_=t_, pb=pb, gs=gs: e.tensor_tensor(out=t_[:], in0=pb[:], in1=bbc[:, gs], op=ALU.add),
                      r=[("ps", j % 4), "bbc"], w=[("ptt", j % 2)])
                S.pool(lambda e, t_=t_, gs=gs: e.tensor_tensor(out=t_[:], in0=t_[:], in1=sbc[:, gs], op=ALU.mult),
                       r=[("ptt", j % 2), "sbc"], w=[("ptt", j % 2)])
                xs_ = x_res[:, j, gs]
                S.dve(lambda e, t_=t_, xs_=xs_: e.tensor_tensor(out=xs_, in0=xs_, in1=t_[:], op=ALU.add),
                      r=[("ptt", j % 2), ("x", j)], w=[("x", j)])
        S.barrier()


def build_PA1():
    P = Prog()
    S = P.S
    x9 = P.inp("x9", [9 * 128, D])
    gp = P.inp("gp_bc", [128, D])
    dp = {"pool_w": P.inp("pool_w", [D, 512]), "pool_b": P.inp("pool_b", [128, D]), "pool_s": P.inp("pool_s", [128, D]),
          "mind": P.inp("mind", [128, 4 * TL])}
    g2 = P.inp("g_bc", [128, D])
    w_up = P.inp("w_up", [D, DFF])
    w_down = P.inp("w_down", [DFF, D])
    ga = P.inp("ga_bc", [128, D])
    w_in = P.inp("w_in", [D, A_IN])
    d_in, d_out = attn_a1_io(P)
    y = P.out("y", [TL, D])
    with ExitStack() as es:
        c = load_consts(P, es)
        x_res = P.sb(es, "x_res", [128, NT, D], F32)
        load_x(P, x_res, x9)
        with ExitStack() as es1:
            gbc = P.sb(es1, "gbc", [128, D], F32)
            S.dma(gbc[:], gp, w=["gbc"])
            wk = norm_work(P, es1)
            hT9 = P.sb(es1, "hT9", [128, KC, 9 * 128], BF16)
            xt = P.sb(es1, "xt", [128, D], F32)
            S.dma(xt[:], x9[TL:TL + 128, :], w=["xt"])
            for t in range(NT):
                emit_norm_T(P, c, x_res[:, t, :], ("x", t), gbc[:], hT9, "hT", t * 128, wk)
            emit_norm_T(P, c, xt[:], "xt", gbc[:], hT9, "hT", TL, wk)
            emit_pool(P, c, es1, x_res, hT9, dp)
        emit_mlp(P, c, es, x_res, g2, w_up, w_down)
        keys = store_x(P, y, x_res)
        with ExitStack() as es2:
            gbc = P.sb(es2, "gbc", [128, D], F32)
            S.dma(gbc[:], ga, w=["gbc"])
            wk = norm_work(P, es2)
            hT = P.sb(es2, "hT", [128, KC, TL], BF16)
            for t in range(NT):
                emit_norm_T(P, c, x_res[:, t, :], ("x", t), gbc[:], hT, "hT", t * 128, wk)
            keys += emit_attn_inproj(P, c, es2, hT, w_in, d_in, d_out)
            S.barrier()
        nc = P.close(keys)
    return nc


def pa1_inputs(xg, pos, d, li, ja):
    xs = shard_tokens(xg)
    a1 = a1_inputs(xs, pos, d, ja, with_x=False)
    common = {"gp_bc": bc128(d["pool_norm"][0]), "pool_w": np.ascontiguousarray(d["pool_w"][0].reshape(D, 512)),
              "pool_b": bc128(d["pool_b"][0].reshape(-1)), "pool_s": bc128(d["pool_scale"][0]),
              "g_bc": bc128(d["mlp_norm"][li]), "w_up": np.ascontiguousarray(d["mlp_w_up"][li]),
              "w_down": np.ascontiguousarray(d["mlp_w_down"][li]), "ga_bc": bc128(d["attn_norm"][ja])}
    ins = []
    for c in range(NCORES):
        idx = (np.arange(NT).reshape(NT, 1) * 8 + c) * 128 + np.arange(128).reshape(1, 128)
        mind = np.stack([np.minimum(idx + 1, w) for w in POOL_W], 0).reshape(1, 4 * TL).astype(np.float32)
        m = dict(common, **a1[c])
        m["x9"] = np.concatenate([xs[c], halo_tile(xg, c)], 0)
        m["mind"] = np.ascontiguousarray(np.broadcast_to(mind, (128, 4 * TL)))
        ins.append(m)
    return ins


_CACHE = {}


def _prog(name, fn):
    if name not in _CACHE:
        _CACHE[name] = fn()
    return _CACHE[name]


def _run(nc, ins):
    return run_bass_kernel_spmd(nc, ins, core_ids=list(range(NCORES))).results


def kernel(**inp):
    d = {k: np.asarray(v) for k, v in inp.items()}
    xg = np.ascontiguousarray(d["x"].reshape(T, D).astype(np.float32, copy=False))
    pos = d["positions"].reshape(T)
    xs = shard_tokens(xg)
    a1 = _run(_prog("A1", build_A1), a1_inputs(xs, pos, d, 0))
    r = _run(_prog("A2", build_A2), a2_inputs(xs, a1, d, 0, 0))
    xg = unshard_tokens([r[c]["y"] for c in range(NCORES)])
    r1 = _run(_prog("R1", build_R1), r1_inputs(xg, d, 0))
    r = _run(_prog("R2", build_R2), r2_inputs(xg, r1, d, 0, 1))
    xg = unshard_tokens([r[c]["y"] for c in range(NCORES)])
    r = _run(_prog("PA1", build_PA1), pa1_inputs(xg, pos, d, 2, 1))
    xg = unshard_tokens([r[c]["y"] for c in range(NCORES)])
    xs = shard_tokens(xg)
    r = _run(_prog("A2", build_A2), a2_inputs(xs, r, d, 1, 3))
    out = unshard_tokens([r[c]["y"] for c in range(NCORES)])
    return out.reshape(1, T, D).astype(np.float32, copy=False)
```
